# Optimizing a Trainium2 kernel written in Bass

```python
import math
import jax, jax.numpy as jnp
from jax import lax
import numpy as np

D_MODEL = 1024
BATCH = 16
SEQ = 2048
DEPTH = 2

N_A_LAYERS = DEPTH // 2
N_B_LAYERS = DEPTH - N_A_LAYERS
N_DENSE_FFN = (DEPTH + 1) // 2
N_MOE_FFN = DEPTH // 2

S5_GROUP = 16
S5_GROUPS = D_MODEL // S5_GROUP
S5_STATE = 64
S5_DT_MIN = 1e-3
S5_DT_MAX = 1e-1

MLA_HEADS = 8
QK_NOPE = 128
QK_ROPE = 64
V_DIM = 128
Q_LORA = 256
KV_LORA = 128
ROPE_THETA = 10000.0
Q_BLOCK = 128

FFN_DIM = 2816
N_EXPERTS = 8
TOP_K = 2
EXPERT_DIM = 3584
ROUTE_BLOCK = 128

ALPHA = (2.0 * DEPTH) ** 0.25
BETA = (8.0 * DEPTH) ** -0.25
LN_EPS = 1e-5
RMS_EPS = 1e-6

kernel_name = 'yoco_s5_mla_moe_deepnorm'


def _layer_norm(x, g, b):
    xf = x.astype(jnp.float32)
    mu = jnp.mean(xf, axis=-1, keepdims=True)
    var = jnp.mean(jnp.square(xf - mu), axis=-1, keepdims=True)
    y = (xf - mu) * lax.rsqrt(var + LN_EPS) * g.astype(jnp.float32) + b.astype(jnp.float32)
    return y.astype(x.dtype)


def _rms_norm(x, g):
    xf = x.astype(jnp.float32)
    y = xf * lax.rsqrt(jnp.mean(jnp.square(xf), axis=-1, keepdims=True) + RMS_EPS) * g.astype(jnp.float32)
    return y.astype(x.dtype)


def _rope_tables(seq_len):
    pos = jnp.arange(seq_len, dtype=jnp.float32)
    inv_freq = ROPE_THETA ** (-jnp.arange(0, QK_ROPE, 2, dtype=jnp.float32) / QK_ROPE)
    ang = pos[:, None] * inv_freq[None, :]
    return jnp.cos(ang), jnp.sin(ang)


def _apply_rope(t, cos, sin):
    half = t.shape[-1] // 2
    t1 = t[..., :half].astype(jnp.float32)
    t2 = t[..., half:].astype(jnp.float32)
    out = jnp.concatenate([t1 * cos - t2 * sin, t1 * sin + t2 * cos], axis=-1)
    return out.astype(t.dtype)


def _s5_combine(left, right):
    a_re_l, a_im_l, b_re_l, b_im_l = left
    a_re_r, a_im_r, b_re_r, b_im_r = right
    return (a_re_r * a_re_l - a_im_r * a_im_l,
            a_re_r * a_im_l + a_im_r * a_re_l,
            a_re_r * b_re_l - a_im_r * b_im_l + b_re_r,
            a_re_r * b_im_l + a_im_r * b_re_l + b_im_r)


def _s5_mixer(u, lam_re, lam_im, log_step, b_re, b_im, c_re, c_im, d_skip, w_glu):
    bsz, seq, _ = u.shape
    f32 = jnp.float32
    uf = u.astype(f32)
    lr = lam_re.astype(f32)
    li = lam_im.astype(f32)
    dt = jnp.exp(log_step.astype(f32))[:, None]
    mag = jnp.exp(lr * dt)
    lb_re = mag * jnp.cos(li * dt)
    lb_im = mag * jnp.sin(li * dt)
    den = lr * lr + li * li
    f_re = ((lb_re - 1.0) * lr + lb_im * li) / den
    f_im = (lb_im * lr - (lb_re - 1.0) * li) / den
    br = b_re.astype(f32)
    bi = b_im.astype(f32)
    bb_re = f_re[..., None] * br - f_im[..., None] * bi
    bb_im = f_re[..., None] * bi + f_im[..., None] * br
    ug = uf.reshape(bsz, seq, S5_GROUPS, S5_GROUP)
    bu_re = jnp.einsum('bsgc,gpc->sbgp', ug, bb_re)
    bu_im = jnp.einsum('bsgc,gpc->sbgp', ug, bb_im)
    a_re = jnp.broadcast_to(lb_re, (seq, 1) + lb_re.shape)
    a_im = jnp.broadcast_to(lb_im, (seq, 1) + lb_im.shape)
    _, _, s_re, s_im = lax.associative_scan(_s5_combine, (a_re, a_im, bu_re, bu_im), axis=0)
    y = (jnp.einsum('sbgp,gcp->bsgc', s_re, c_re.astype(f32))
         - jnp.einsum('sbgp,gcp->bsgc', s_im, c_im.astype(f32)))
    y = y.reshape(bsz, seq, D_MODEL) + d_skip.astype(f32) * uf
    act = jax.nn.gelu(y).astype(u.dtype)
    val, gate = jnp.split(act @ w_glu, 2, axis=-1)
    return val * jax.nn.sigmoid(gate)


def _mla_shared_kv(h, kv_w_a, kv_norm, kv_w_b, cos, sin):
    bsz, seq, _ = h.shape
    kv_a = h @ kv_w_a
    c_kv = _rms_norm(kv_a[..., :KV_LORA], kv_norm)
    k_rope = _apply_rope(kv_a[..., KV_LORA:], cos[None], sin[None])
    kv = (c_kv @ kv_w_b).reshape(bsz, seq, MLA_HEADS, QK_NOPE + V_DIM)
    return kv[..., :QK_NOPE], k_rope, kv[..., QK_NOPE:]


def _mla_attention(x, q_w_a, q_norm, q_w_b, o_w, k_nope, k_rope, v, cos, sin):
    bsz, seq, _ = x.shape
    c_q = _rms_norm(x @ q_w_a, q_norm)
    q = (c_q @ q_w_b).reshape(bsz, seq, MLA_HEADS, QK_NOPE + QK_ROPE)
    q_nope = q[..., :QK_NOPE]
    q_rope = _apply_rope(q[..., QK_NOPE:], cos[None, :, None, :], sin[None, :, None, :])
    scale = 1.0 / math.sqrt(QK_NOPE + QK_ROPE)
    outs = []
    for q0 in range(0, seq, Q_BLOCK):
        kv_len = q0 + Q_BLOCK
        s = (jnp.einsum('bqhd,bkhd->bhqk', q_nope[:, q0:kv_len], k_nope[:, :kv_len])
             + jnp.einsum('bqhr,bkr->bhqk', q_rope[:, q0:kv_len], k_rope[:, :kv_len]))
        s = s.astype(jnp.float32) * scale
        q_pos = q0 + jnp.arange(Q_BLOCK)
        k_pos = jnp.arange(kv_len)
        s = jnp.where(k_pos[None, :] <= q_pos[:, None], s, -jnp.inf)
        p = jax.nn.softmax(s, axis=-1).astype(v.dtype)
        outs.append(jnp.einsum('bhqk,bkhd->bqhd', p, v[:, :kv_len]))
    o = jnp.concatenate(outs, axis=1).reshape(bsz, seq, MLA_HEADS * V_DIM)
    return o @ o_w


def _swiglu(x, w_in, w_out):
    g, u = jnp.split(x @ w_in, 2, axis=-1)
    return (jax.nn.silu(g) * u) @ w_out


def _moe_swiglu(x, w_router, w_in, w_out):
    bsz, seq, d = x.shape
    n_tok = bsz * seq
    n_assign = n_tok * TOP_K
    xf = x.reshape(n_tok, d)
    logits = (xf @ w_router).astype(jnp.float32)
    top_val, top_idx = lax.top_k(logits, TOP_K)
    gate = jax.nn.softmax(top_val, axis=-1)
    flat_e = top_idx.reshape(-1)
    flat_tok = jnp.repeat(jnp.arange(n_tok, dtype=jnp.int32), TOP_K)
    flat_w = gate.reshape(-1)
    order = jnp.argsort(flat_e)
    sorted_e = flat_e[order]
    counts = jnp.bincount(flat_e, length=N_EXPERTS)
    starts = jnp.cumsum(counts) - counts
    padded = ((counts + ROUTE_BLOCK - 1) // ROUTE_BLOCK) * ROUTE_BLOCK
    pad_ends = jnp.cumsum(padded)
    pad_starts = pad_ends - padded
    dest = pad_starts[sorted_e] + (jnp.arange(n_assign) - starts[sorted_e])
    n_rows = n_assign + N_EXPERTS * ROUTE_BLOCK
    n_blocks = n_rows // ROUTE_BLOCK
    row_tok = jnp.full((n_rows,), n_tok, jnp.int32).at[dest].set(flat_tok[order])
    row_w = jnp.zeros((n_rows,), jnp.float32).at[dest].set(flat_w[order])
    blk_e = jnp.minimum(jnp.searchsorted(pad_ends, jnp.arange(n_blocks) * ROUTE_BLOCK, side='right'),
                        N_EXPERTS - 1)
    x_pad = jnp.concatenate([xf, jnp.zeros((1, d), xf.dtype)], axis=0)
    xs = x_pad[row_tok].reshape(n_blocks, ROUTE_BLOCK, d)

    def _expert_block(args):
        xb, e = args
        return _swiglu(xb, w_in[e], w_out[e])

    ys = lax.map(_expert_block, (xs, blk_e)).reshape(n_rows, d)
    out = jnp.zeros((n_tok + 1, d), ys.dtype).at[row_tok].add(ys * row_w[:, None].astype(ys.dtype))
    return out[:n_tok].reshape(bsz, seq, d)


def _normal(key, shape, fan_in, scale=1.0):
    return jax.random.normal(key, shape, jnp.float32) * (scale * fan_in ** -0.5)


def setup_inputs(seed: int = 0) -> dict:
    key = jax.random.key(seed)
    k = jax.random.split(key, 28)
    f32 = jnp.float32
    G, P, C = S5_GROUPS, S5_STATE, S5_GROUP
    x = jax.random.normal(k[0], (BATCH, SEQ, D_MODEL), f32)
    s5_lam_re = -0.5 + 0.01 * jax.random.normal(k[1], (N_A_LAYERS, G, P), f32)
    s5_lam_im = (jnp.pi * jnp.arange(P, dtype=f32)[None, None, :]
                 + 0.01 * jax.random.normal(k[2], (N_A_LAYERS, G, P), f32))
    s5_log_step = jax.random.uniform(k[3], (N_A_LAYERS, G), f32,
                                     minval=math.log(S5_DT_MIN), maxval=math.log(S5_DT_MAX))
    s5_b_re = _normal(k[4], (N_A_LAYERS, G, P, C), 2 * C)
    s5_b_im = _normal(k[5], (N_A_LAYERS, G, P, C), 2 * C)
    s5_c_re = _normal(k[6], (N_A_LAYERS, G, C, P), 2 * P)
    s5_c_im = _normal(k[7], (N_A_LAYERS, G, C, P), 2 * P)
    s5_d = jax.random.normal(k[8], (N_A_LAYERS, D_MODEL), f32)
    s5_w_glu = jnp.concatenate([_normal(k[9], (N_A_LAYERS, D_MODEL, D_MODEL), D_MODEL, BETA),
                                _normal(k[10], (N_A_LAYERS, D_MODEL, D_MODEL), D_MODEL)], axis=-1)
    mla_q_w_a = _normal(k[11], (N_B_LAYERS, D_MODEL, Q_LORA), D_MODEL)
    mla_q_norm = 1.0 + 0.01 * jax.random.normal(k[12], (N_B_LAYERS, Q_LORA), f32)
    mla_q_w_b = _normal(k[13], (N_B_LAYERS, Q_LORA, MLA_HEADS * (QK_NOPE + QK_ROPE)), Q_LORA)
    mla_o_w = _normal(k[14], (N_B_LAYERS, MLA_HEADS * V_DIM, D_MODEL), MLA_HEADS * V_DIM, BETA)
    kv_w_a = _normal(k[15], (D_MODEL, KV_LORA + QK_ROPE), D_MODEL)
    kv_norm = 1.0 + 0.01 * jax.random.normal(k[16], (KV_LORA,), f32)
    kv_w_b = jnp.concatenate([_normal(k[17], (KV_LORA, MLA_HEADS, QK_NOPE), KV_LORA),
                              _normal(k[18], (KV_LORA, MLA_HEADS, V_DIM), KV_LORA, BETA)],
                             axis=-1).reshape(KV_LORA, MLA_HEADS * (QK_NOPE + V_DIM))
    ffn_w_in = _normal(k[19], (N_DENSE_FFN, D_MODEL, 2 * FFN_DIM), D_MODEL)
    ffn_w_out = _normal(k[20], (N_DENSE_FFN, FFN_DIM, D_MODEL), FFN_DIM, BETA)
    moe_router = _normal(k[21], (N_MOE_FFN, D_MODEL, N_EXPERTS), D_MODEL)
    moe_w_in = _normal(k[22], (N_MOE_FFN, N_EXPERTS, D_MODEL, 2 * EXPERT_DIM), D_MODEL)
    moe_w_out = _normal(k[23], (N_MOE_FFN, N_EXPERTS, EXPERT_DIM, D_MODEL), EXPERT_DIM, BETA)
    ln_g = 1.0 + 0.01 * jax.random.normal(k[24], (DEPTH, 2, D_MODEL), f32)
    ln_b = 0.01 * jax.random.normal(k[25], (DEPTH, 2, D_MODEL), f32)
    return {'x': x,
            's5_lam_re': s5_lam_re, 's5_lam_im': s5_lam_im, 's5_log_step': s5_log_step,
            's5_b_re': s5_b_re, 's5_b_im': s5_b_im, 's5_c_re': s5_c_re, 's5_c_im': s5_c_im,
            's5_d': s5_d, 's5_w_glu': s5_w_glu,
            'mla_q_w_a': mla_q_w_a, 'mla_q_norm': mla_q_norm, 'mla_q_w_b': mla_q_w_b, 'mla_o_w': mla_o_w,
            'kv_w_a': kv_w_a, 'kv_norm': kv_norm, 'kv_w_b': kv_w_b,
            'ffn_w_in': ffn_w_in, 'ffn_w_out': ffn_w_out,
            'moe_router': moe_router, 'moe_w_in': moe_w_in, 'moe_w_out': moe_w_out,
            'ln_g': ln_g, 'ln_b': ln_b}


def reference(x, s5_lam_re, s5_lam_im, s5_log_step, s5_b_re, s5_b_im, s5_c_re, s5_c_im,
              s5_d, s5_w_glu, mla_q_w_a, mla_q_norm, mla_q_w_b, mla_o_w,
              kv_w_a, kv_norm, kv_w_b, ffn_w_in, ffn_w_out,
              moe_router, moe_w_in, moe_w_out, ln_g, ln_b):
    cos, sin = _rope_tables(x.shape[1])
    k_nope = k_rope = v = None
    for layer in range(DEPTH):
        if layer < N_A_LAYERS:
            i = layer
            mix = _s5_mixer(x, s5_lam_re[i], s5_lam_im[i], s5_log_step[i], s5_b_re[i], s5_b_im[i],
                            s5_c_re[i], s5_c_im[i], s5_d[i], s5_w_glu[i])
        else:
            if layer == N_A_LAYERS:
                k_nope, k_rope, v = _mla_shared_kv(x, kv_w_a, kv_norm, kv_w_b, cos, sin)
            i = layer - N_A_LAYERS
            mix = _mla_attention(x, mla_q_w_a[i], mla_q_norm[i], mla_q_w_b[i], mla_o_w[i],
                                 k_nope, k_rope, v, cos, sin)
        x = _layer_norm(ALPHA * x + mix, ln_g[layer, 0], ln_b[layer, 0])
        if layer % 2 == 0:
            ffn = _swiglu(x, ffn_w_in[layer // 2], ffn_w_out[layer // 2])
        else:
            ffn = _moe_swiglu(x, moe_router[layer // 2], moe_w_in[layer // 2], moe_w_out[layer // 2])
        x = _layer_norm(ALPHA * x + ffn, ln_g[layer, 1], ln_b[layer, 1])
    return x
```

```python
import math
import numpy as np
import ml_dtypes
from contextlib import ExitStack
import concourse.bass as bass
import concourse.mybir as mybir
from concourse.bass_utils import run_bass_kernel_spmd

F32 = mybir.dt.float32
BF16 = mybir.dt.bfloat16
I32 = mybir.dt.int32
AF = mybir.ActivationFunctionType
ALU = mybir.AluOpType
AX = mybir.AxisListType

import os
SAME_ENG_SYNC = os.environ.get("KSES", "1") == "1"
ROUTED = True
RING = 12
NCORES = 8
D = 1024
ALPHA = (2.0 * 2) ** 0.25
LN_EPS = 1e-5
RMS_EPS = 1e-6
PI = math.pi
TWO_PI = 2.0 * math.pi
CAP = 1408
NEXP = 8
FFN = 2816
EDIM = 3584


class Op:
    __slots__ = ("eng", "fn", "deps", "sig", "dma", "semi", "semv")


class Prog:
    ENGS = ("pe", "act", "dve", "pool", "sp")
    COMPUTE = ("pe", "act", "dve", "pool")

    def __init__(self, nc):
        self.nc = nc
        self.q = {e: [] for e in self.ENGS}
        self.lastw = {}
        self.rd = {}
        self.ndma = {e: 0 for e in self.ENGS}
        self.dmaops = {e: [] for e in self.ENGS}

    def op(self, eng, fn, r=(), w=(), dma=False, extra=()):
        o = Op()
        o.eng = eng
        o.fn = fn
        o.dma = dma
        o.sig = dma
        o.semi = None
        o.semv = 0
        deps = set(extra)
        lastw = self.lastw
        rd = self.rd
        for k in r:
            lw = lastw.get(k)
            if lw is not None:
                deps.add(lw)
        for k in w:
            lw = lastw.get(k)
            if lw is not None:
                deps.add(lw)
            x = rd.get(k)
            if x:
                deps.update(x)
        for k in r:
            l = rd.get(k)
            if l is None:
                rd[k] = [o]
            elif not dma:
                for i, x in enumerate(l):
                    if x.eng == eng and not x.dma:
                        l[i] = o
                        break
                else:
                    l.append(o)
            else:
                l.append(o)
        for k in w:
            lastw[k] = o
            rd[k] = []
        deps.discard(o)
        o.deps = deps
        if dma:
            o.semi = self.ndma[eng] % RING
            o.semv = 16 * (self.ndma[eng] // RING + 1)
            self.ndma[eng] += 1
            self.dmaops[eng].append(o)
        self.q[eng].append(o)
        return o

    def dma(self, eng, out, in_, r=(), w=(), **kw):
        return self.op(eng, lambda e: e.dma_start(out=out, in_=in_, **kw), r=r, w=w, dma=True)

    def barrier(self):
        lastc = []
        for e in self.COMPUTE:
            for o in reversed(self.q[e]):
                if not o.dma and o.fn is not None:
                    lastc.append(o)
                    break
        lastd = []
        for e in self.ENGS:
            lastd.extend(self.dmaops[e][-RING:])
        for e in self.ENGS:
            self.op(e, None, extra=lastc + lastd)

    def mm(self, out, lhsT, rhs, start, stop, r, w):
        return self.op("pe", lambda e: e.matmul(out, lhsT=lhsT, rhs=rhs, start=start, stop=stop), r=r, w=w)

    def tr(self, out, in_, ident, r, w):
        return self.op("pe", lambda e: e.transpose(out=out, in_=in_, identity=ident), r=r, w=w)

    def act(self, out, in_, func, r, w, eng="act", **kw):
        return self.op(eng, lambda e: e.activation(out=out, in_=in_, func=func, **kw), r=r, w=w)

    def tt(self, eng, out, in0, in1, op, r, w):
        return self.op(eng, lambda e: e.tensor_tensor(out=out, in0=in0, in1=in1, op=op), r=r, w=w)

    def ts(self, eng, out, in0, s1, s2, op0, op1, r, w):
        if op1 is None:
            return self.op(eng, lambda e: e.tensor_scalar(out=out, in0=in0, scalar1=s1, scalar2=None, op0=op0), r=r, w=w)
        return self.op(eng, lambda e: e.tensor_scalar(out=out, in0=in0, scalar1=s1, scalar2=s2, op0=op0, op1=op1), r=r, w=w)

    def stt(self, eng, out, in0, scalar, in1, op0, op1, r, w):
        return self.op(eng, lambda e: e.scalar_tensor_tensor(out=out, in0=in0, scalar=scalar, in1=in1, op0=op0, op1=op1), r=r, w=w)

    def cp(self, eng, out, in_, r, w):
        if eng == "act":
            return self.op(eng, lambda e: e.copy(out=out, in_=in_), r=r, w=w)
        return self.op(eng, lambda e: e.tensor_copy(out=out, in_=in_), r=r, w=w)

    def emit(self):
        nc = self.nc
        es = ExitStack()
        with es:
            csem = {e: es.enter_context(nc.semaphore("c_" + e)) for e in self.COMPUTE}
            dsem = {}
            for e in self.ENGS:
                if self.ndma[e]:
                    dsem[e] = [es.enter_context(nc.semaphore("d_%s_%d" % (e, i))) for i in range(min(RING, self.ndma[e]))]

            def skip_same(d, ename):
                return d.eng == ename and (ename == "pe" or ename == "sp" or not SAME_ENG_SYNC)

            for e in self.ENGS:
                for o in self.q[e]:
                    for d in o.deps:
                        if not d.dma and not skip_same(d, e):
                            d.sig = True
            for e in self.COMPUTE:
                c = 0
                for o in self.q[e]:
                    if o.sig and not o.dma:
                        c += 1
                        o.semv = c
            self.stats = {}

            def run(ename, eng):
                waited = {}
                nw = 0
                for o in self.q[ename]:
                    waits = {}
                    for d in o.deps:
                        if d.dma:
                            s = dsem[d.eng][d.semi]
                        else:
                            if skip_same(d, ename):
                                continue
                            s = csem[d.eng]
                        if waits.get(s, 0) < d.semv:
                            waits[s] = d.semv
                    if o.dma and o.semv > 16:
                        s = dsem[ename][o.semi]
                        if waits.get(s, 0) < o.semv - 16:
                            waits[s] = o.semv - 16
                    for s, v in waits.items():
                        if waited.get(s, 0) >= v:
                            continue
                        waited[s] = v
                        eng.wait_ge(s, v)
                        nw += 1
                    if o.fn is None:
                        continue
                    ins = o.fn(eng)
                    if o.dma:
                        ins.then_inc(dsem[ename][o.semi], 16)
                    elif o.sig:
                        ins.then_inc(csem[ename], 1)
                self.stats[ename] = (len(self.q[ename]), nw)

            with nc.Block() as block:
                @block.tensor
                def _(eng):
                    run("pe", eng)

                @block.scalar
                def _(eng):
                    run("act", eng)

                @block.vector
                def _(eng):
                    run("dve", eng)

                @block.gpsimd
                def _(eng):
                    run("pool", eng)

                @block.sync
                def _(eng):
                    run("sp", eng)


def dap(t, offset, ap):
    return bass.AP(t.tensor, offset, [list(x) for x in ap])


class _Cut(Exception):
    pass


def build_program(nphase=99, debug=False, cut=None, mini=False):
    nc = bass.Bass("TRN2", target_bir_lowering=False)
    p = Prog(nc)
    S5IN = ("s5_lam_re", "s5_lam_im", "s5_log_step", "s5_b_re", "s5_b_im", "s5_c_re", "s5_c_im", "s5_d")

    def din(name, shape, dt=F32):
        if mini and not (name in S5IN or name.startswith("c_")):
            shape = [1, 2]
        return nc.dram_tensor(name, list(shape), dt, kind="ExternalInput").ap()

    def cutpoint(n):
        if cut is not None and n >= cut:
            raise _Cut()

    dbg = debug if isinstance(debug, (set, list, tuple)) else (("xa", "xb", "xc", "xe", "ye") if debug else ())

    def dscr(name, shape, dt=F32):
        return nc.dram_tensor(name, list(shape), dt, kind=("ExternalOutput" if name in dbg else "Internal")).ap()

    x_in = din("x", [4, 128, 8, D])
    dbgo = nc.dram_tensor("dbgo", [128, 4096], F32, kind="ExternalOutput").ap() if cut is not None else None
    lam_re = din("s5_lam_re", [64, 64]); lam_im = din("s5_lam_im", [64, 64]); log_step = din("s5_log_step", [1, 64])
    b_re = din("s5_b_re", [64, 64, 16]); b_im = din("s5_b_im", [64, 64, 16])
    c_re = din("s5_c_re", [64, 16, 64]); c_im = din("s5_c_im", [64, 16, 64])
    s5_d = din("s5_d", [1, D]); w_glu = din("s5_w_glu", [D, 2 * D])
    q_w_a = din("mla_q_w_a", [D, 256]); q_norm = din("mla_q_norm", [256, 1]); q_w_b = din("mla_q_w_b", [256, 1536])
    o_w = din("mla_o_w", [D, D]); kv_w_a = din("kv_w_a", [D, 192]); kv_norm = din("kv_norm", [128, 1]); kv_w_b = din("kv_w_b", [128, 2048])
    ffn_w_in = din("ffn_w_in", [D, 2 * FFN]); ffn_w_out = din("ffn_w_out", [FFN, D])
    moe_router = din("moe_router", [D, NEXP]); moe_w_in = din("moe_w_in", [NEXP, D, 2 * EDIM] if nphase >= 5 else [1, 1, 2]); moe_w_out = din("moe_w_out", [NEXP, EDIM, D] if nphase >= 5 else [1, 1, 2])
    ln_g = din("ln_g", [4, D]); ln_b = din("ln_b", [4, D])
    c_ident = din("c_ident", [128, 128]); c_evec = din("c_evec", [128, 24]); c_cmask = din("c_cmask", [128, 256])
    c_iota = din("c_iota", [128, 2, 128]); c_triu = din("c_triu", [128, 128]); c_ecol = din("c_ecol", [128, NEXP])
    c_dmask = din("c_dmask", [128, 128]); c_pos = din("c_pos", [64, 2048]); c_invf = din("c_invf", [64, 2])

    out = nc.dram_tensor("out", [4, 128, 8, D], F32, kind="ExternalOutput").ap()
    xa = dscr("xa", [4, 128, 8, D]); xb = dscr("xb", [4, 128, 8, D]); xc = dscr("xc", [4, 128, 8, D])
    XE = dscr("xe", [NEXP * CAP + 128, D], BF16)
    YE = dscr("ye", [NEXP * CAP + 128, D])

    top = ExitStack()
    with top:
        ar = {"peak": 0, "n": 0}

        def sbt(es, name, shape, dt=F32):
            ar["n"] += 1
            return es.enter_context(nc.sbuf_tensor("%s_%d" % (name, ar["n"]), list(shape), dt))

        PS = [top.enter_context(nc.psum_tensor("ps%d" % i, [128, 1024], F32)) for i in range(4)]

        def bank(i):
            return PS[i // 2][:, (i % 2) * 512:(i % 2) * 512 + 512]

        def bankb(i):
            return PS[i // 2][:, (i % 2) * 512:(i % 2) * 512 + 512].bitcast(BF16)

        identf = sbt(top, "identf", [128, 128]); identb = sbt(top, "identb", [128, 128], BF16)
        negpi = sbt(top, "negpi", [128, 1]); epsln = sbt(top, "epsln", [128, 1])
        p.dma("sp", identf[:], c_ident, w=["identf"])

        def load_ln(es, lnidx):
            g_ = sbt(es, "lng%d" % lnidx, [128, D]); b_ = sbt(es, "lnb%d" % lnidx, [128, D])
            p.dma("sp", g_[:], dap(ln_g, lnidx * D, [[0, 128], [1, D]]), w=[("lng", lnidx)])
            p.dma("sp", b_[:], dap(ln_b, lnidx * D, [[0, 128], [1, D]]), w=[("lnb", lnidx)])
            return (g_, b_, lnidx)
        p.cp("pool", identb[:], identf[:], r=["identf"], w=["identb"])
        p.op("pool", lambda e: e.memset(negpi[:], -PI), w=["negpi"])
        p.op("pool", lambda e: e.memset(epsln[:], LN_EPS), w=["epsln"])
        halfpi = sbt(top, "halfpi", [128, 1])
        p.op("pool", lambda e: e.memset(halfpi[:], PI / 2), w=["halfpi"])
        MAGIC = 12582912.0

        def sincos(eng, sin_out, cos_out, y, tmp, rk, wk, npart=128):
            p.ts(eng, tmp, y, MAGIC, MAGIC, ALU.add, ALU.subtract, r=rk, w=wk)
            p.tt(eng, tmp, y, tmp, ALU.subtract, r=rk + wk, w=wk)
            p.act(sin_out, tmp, AF.Sin, r=wk, w=wk, scale=TWO_PI)
            p.stt(eng, tmp, tmp, -1.0, tmp, ALU.mult, ALU.max, r=wk, w=wk)
            p.act(cos_out, tmp, AF.Sin, r=wk + ["halfpi"], w=wk, scale=-TWO_PI, bias=halfpi[0:npart, 0:1])


        def layer_norm(blk, lnp, keyblk, stat, aff="pool"):
            st6, mv, rstd = stat
            lng_, lnb_, lnidx = lnp
            for h in range(2):
                p.op("dve", (lambda hh: lambda e: e.bn_stats(out=st6[:, hh, :], in_=blk[:, hh * 512:(hh + 1) * 512]))(h), r=[keyblk], w=["st6"])
            p.op("dve", lambda e: e.bn_aggr(out=mv[:], in_=st6[:].rearrange("p a b -> p (a b)")), r=["st6"], w=["mv"])
            p.act(rstd[:], mv[:, 1:2], AF.Sqrt, r=["mv", "epsln"], w=["rstd"], bias=epsln[:, 0:1])
            p.op("dve", lambda e: e.reciprocal(out=rstd[:], in_=rstd[:]), r=["rstd"], w=["rstd"])
            p.ts("dve", blk, blk, mv[:, 0:1], rstd[:, 0:1], ALU.subtract, ALU.mult, r=[keyblk, "mv", "rstd"], w=[keyblk])
            p.tt(aff, blk, blk, lng_[:], ALU.mult, r=[keyblk, ("lng", lnidx)], w=[keyblk])
            p.tt(aff, blk, blk, lnb_[:], ALU.add, r=[keyblk, ("lnb", lnidx)], w=[keyblk])

        try:
            s5 = ExitStack()
            with s5:
                CtrlRe = sbt(s5, "CtrlRe", [128, 32, 128], BF16); CtrlIm = sbt(s5, "CtrlIm", [128, 32, 128], BF16)
                Toep = sbt(s5, "Toep", [128, 64, 128], BF16)
                ObRe = sbt(s5, "ObRe", [128, 64, 128], BF16); ObIm = sbt(s5, "ObIm", [128, 64, 128], BF16)
                rho = sbt(s5, "rho", [128, 32]); phi = sbt(s5, "phi", [128, 32])
                p0 = ExitStack()
                with p0:
                    lr = sbt(p0, "lr", [128, 32]); li = sbt(p0, "li", [128, 32]); ls = sbt(p0, "ls", [128, 32])
                    Bre = sbt(p0, "Bre", [128, 32, 16]); Bim = sbt(p0, "Bim", [128, 32, 16])
                    Cre = sbt(p0, "Cre", [128, 32, 16]); Cim = sbt(p0, "Cim", [128, 32, 16])
                    evec = sbt(p0, "evec", [128, 24]); cmask = sbt(p0, "cmask", [128, 256])
                    dt = sbt(p0, "dt", [128, 32]); lrdt = sbt(p0, "lrdt", [128, 32]); lidt = sbt(p0, "lidt", [128, 32])
                    PWre = sbt(p0, "PWre", [128, 32, 24]); PWim = sbt(p0, "PWim", [128, 32, 24])
                    sm = [sbt(p0, "sm%d" % i, [128, 32]) for i in range(6)]
                    fre = sbt(p0, "fre", [128, 32]); fim = sbt(p0, "fim", [128, 32])
                    Bbre = sbt(p0, "Bbre", [128, 32, 16]); Bbim = sbt(p0, "Bbim", [128, 32, 16]); tb = sbt(p0, "tb", [128, 32, 16])
                    XBre = sbt(p0, "XBre", [128, 32, 8, 16]); XBim = sbt(p0, "XBim", [128, 32, 8, 16])
                    Zre = sbt(p0, "Zre", [128, 32, 8, 16]); Zim = sbt(p0, "Zim", [128, 32, 8, 16])
                    p0b = ExitStack()
                    A = sbt(p0b, "A", [128, 32, 24]); ANG = sbt(p0b, "ANG", [128, 32, 24]); XS = sbt(p0b, "XS", [128, 32, 24])
                    T1 = sbt(p0b, "T1", [128, 32, 8, 16]); T2 = sbt(p0b, "T2", [128, 32, 8, 16])
                    for a in range(2):
                        sl = slice(a * 64, (a + 1) * 64)
                        p.dma("sp", lr[sl, :], dap(lam_re, a * 64, [[1, 64], [128, 32]]), w=["lr"])
                        p.dma("sp", li[sl, :], dap(lam_im, a * 64, [[1, 64], [128, 32]]), w=["li"])
                        p.dma("sp", ls[sl, :], dap(log_step, a, [[0, 64], [2, 32]]), w=["ls"])
                        p.dma("sp", Bre[sl], dap(b_re, a * 1024, [[16, 64], [2048, 32], [1, 16]]), w=["Bre"])
                        p.dma("sp", Bim[sl], dap(b_im, a * 1024, [[16, 64], [2048, 32], [1, 16]]), w=["Bim"])
                        for c_ in range(16):
                            p.dma("sp", Cre[sl, :, c_], dap(c_re, a * 1024 + c_ * 64, [[1, 64], [2048, 32]]), w=["Cre"])
                            p.dma("sp", Cim[sl, :, c_], dap(c_im, a * 1024 + c_ * 64, [[1, 64], [2048, 32]]), w=["Cim"])
                    p.dma("sp", evec[:], c_evec, w=["evec"])
                    p.dma("sp", cmask[:], c_cmask, w=["cmask"])
                    cutpoint(1)
                    V, M, Sb, AD = "dve", ALU.mult, ALU.subtract, ALU.add
                    p.act(dt[:], ls[:], AF.Exp, r=["ls"], w=["dt"])
                    p.tt(V, lrdt[:], lr[:], dt[:], M, r=["lr", "dt"], w=["lrdt"])
                    p.tt(V, lidt[:], li[:], dt[:], M, r=["li", "dt"], w=["lidt"])
                    b3 = lambda t2: t2[:].unsqueeze(2).to_broadcast([128, 32, 24])
                    ev3 = evec[:].unsqueeze(1).to_broadcast([128, 32, 24])
                    p.tt(V, A[:], b3(lrdt), ev3, M, r=["lrdt", "evec"], w=["A"])
                    p.act(A[:], A[:], AF.Exp, r=["A"], w=["A"])
                    p.ts(V, sm[0][:], lidt[:], 1.0 / TWO_PI, None, M, None, r=["lidt"], w=["sm0"])
                    p.tt(V, ANG[:], b3(sm[0]), ev3, M, r=["sm0", "evec"], w=["ANG"])
                    sincos(V, PWim[:], PWre[:], ANG[:], XS[:], ["ANG"], ["XS", "PWre", "PWim"])
                    p.tt(V, PWim[:], PWim[:], A[:], M, r=["A", "PWim", "XS"], w=["PWim"])
                    p.tt(V, PWre[:], PWre[:], A[:], M, r=["A", "PWre", "XS"], w=["PWre"])
                    if cut == 2:
                        p.dma("sp", dbgo[:, 0:768], PWre[:].rearrange("p a b -> p (a b)"), r=["PWre"], w=["dbgo"])
                        p.dma("sp", dbgo[:, 768:1536], PWim[:].rearrange("p a b -> p (a b)"), r=["PWim"], w=["dbgo"])
                    cutpoint(2)
                    lbre = PWre[:, :, 16]; lbim = PWim[:, :, 16]
                    p.tt(V, sm[0][:], lr[:], lr[:], M, r=["lr"], w=["sm0"])
                    p.tt(V, sm[1][:], li[:], li[:], M, r=["li"], w=["sm1"])
                    p.tt(V, sm[0][:], sm[0][:], sm[1][:], AD, r=["sm0", "sm1"], w=["sm0"])
                    p.op(V, lambda e: e.reciprocal(out=sm[0][:], in_=sm[0][:]), r=["sm0"], w=["sm0"])
                    p.ts(V, sm[1][:], lbre, -1.0, None, AD, None, r=["PWre", "sm0"], w=["sm1"])
                    p.tt(V, sm[2][:], sm[1][:], lr[:], M, r=["sm1", "lr"], w=["sm2"])
                    p.tt(V, sm[3][:], lbim, li[:], M, r=["PWim", "li"], w=["sm3"])
                    p.tt(V, sm[2][:], sm[2][:], sm[3][:], AD, r=["sm2", "sm3"], w=["sm2"])
                    p.tt(V, fre[:], sm[2][:], sm[0][:], M, r=["sm2", "sm0"], w=["fre"])
                    p.tt(V, sm[4][:], lbim, lr[:], M, r=["PWim", "lr"], w=["sm4"])
                    p.tt(V, sm[5][:], sm[1][:], li[:], M, r=["sm1", "li"], w=["sm5"])
                    p.tt(V, sm[4][:], sm[4][:], sm[5][:], Sb, r=["sm4", "sm5"], w=["sm4"])
                    p.tt(V, fim[:], sm[4][:], sm[0][:], M, r=["sm4", "sm0"], w=["fim"])
                    f3 = lambda t2: t2[:].unsqueeze(2).to_broadcast([128, 32, 16])
                    p.tt(V, Bbre[:], Bre[:], f3(fre), M, r=["Bre", "fre"], w=["Bbre"])
                    p.tt(V, tb[:], Bim[:], f3(fim), M, r=["Bim", "fim"], w=["tb"])
                    p.tt(V, Bbre[:], Bbre[:], tb[:], Sb, r=["Bbre", "tb"], w=["Bbre"])
                    p.tt(V, Bbim[:], Bim[:], f3(fre), M, r=["Bim", "fre"], w=["Bbim"])
                    p.tt(V, tb[:], Bre[:], f3(fim), M, r=["Bre", "fim", "Bbre"], w=["tb"])
                    p.tt(V, Bbim[:], Bbim[:], tb[:], AD, r=["Bbim", "tb"], w=["Bbim"])
                    pw4 = lambda t3, lo: t3[:, :, lo:lo + 8].unsqueeze(3).to_broadcast([128, 32, 8, 16])
                    v4 = lambda t3: t3[:].unsqueeze(2).to_broadcast([128, 32, 8, 16])

                    def cmul(outre, outim, pre, pim, lo, vre, vim, kre, kim, negim, obf=None):
                        p.tt(V, T1[:], pw4(pre, lo), v4(vre), M, r=["PWre", kre], w=["T1"])
                        p.tt(V, T2[:], pw4(pim, lo), v4(vim), M, r=["PWim", kim], w=["T2"])
                        p.tt(V, outre[0], T1[:], T2[:], Sb, r=["T1", "T2"], w=[outre[1]])
                        p.tt(V, T1[:], pw4(pre, lo), v4(vim), M, r=["PWre", kim, outre[1]], w=["T1"])
                        p.tt(V, T2[:], pw4(pim, lo), v4(vre), M, r=["PWim", kre, outre[1]], w=["T2"])
                        if negim:
                            p.stt(V, outim[0], T1[:], -1.0, T2[:], M, Sb, r=["T1", "T2"], w=[outim[1]])
                        else:
                            p.tt(V, outim[0], T1[:], T2[:], AD, r=["T1", "T2"], w=[outim[1]])

                    cmul((XBre[:], "XBre"), (XBim[:], "XBim"), PWre, PWim, 0, Bbre, Bbim, "Bbre", "Bbim", False)
                    cmul((Zre[:], "Zre"), (Zim[:], "Zim"), PWre, PWim, 8, Cre, Cim, "Cre", "Cim", True)
                    p.op("pool", lambda e: e.memset(ObRe[:], 0.0), w=["ObRe"])
                    p.op("pool", lambda e: e.memset(ObIm[:], 0.0), w=["ObIm"])
                    pw4 = lambda t3, lo: t3[:, :, lo:lo + 8].unsqueeze(3).to_broadcast([128, 32, 8, 16])
                    p.tt(V, T1[:], pw4(PWre, 16), v4(Cre), M, r=["PWre", "Cre"], w=["T1"])
                    p.tt(V, T2[:], pw4(PWim, 16), v4(Cim), M, r=["PWim", "Cim"], w=["T2"])
                    for a in range(2):
                        sl = slice(a * 64, (a + 1) * 64)
                        ov = ObRe[sl, :, :].rearrange("p (k a) (j c) -> p k a j c", a=2, c=16)[:, :, a, :, :]
                        p.tt(V, ov, T1[sl], T2[sl], Sb, r=["T1", "T2"], w=["ObRe"])
                    p.tt(V, T1[:], pw4(PWre, 16), v4(Cim), M, r=["PWre", "Cim", "ObRe"], w=["T1"])
                    p.tt(V, T2[:], pw4(PWim, 16), v4(Cre), M, r=["PWim", "Cre", "ObRe"], w=["T2"])
                    for a in range(2):
                        sl = slice(a * 64, (a + 1) * 64)
                        ov = ObIm[sl, :, :].rearrange("p (k a) (j c) -> p k a j c", a=2, c=16)[:, :, a, :, :]
                        p.stt(V, ov, T1[sl], -1.0, T2[sl], M, Sb, r=["T1", "T2"], w=["ObIm"])
                    p.act(rho[:], lrdt[:], AF.Exp, r=["lrdt"], w=["rho"], scale=8.0)
                    p.ts(V, phi[:], lidt[:], 8.0 / TWO_PI, None, M, None, r=["lidt"], w=["phi"])
                    if cut == 3:
                        p.dma("sp", dbgo[:, 0:4096], XBre[:].rearrange("p a b c -> p (a b c)"), r=["XBre"], w=["dbgo"])
                    cutpoint(3)
                    p0b.close()
                    HL = {}
                    for nm, src in (("XBre", XBre), ("XBim", XBim)):
                        hi = sbt(p0, nm + "_h", [128, 32, 128], BF16)
                        p.cp("pool", hi[:], src[:].rearrange("p k i c -> p k (i c)"), r=[nm], w=["HL", "T1", "T2", "A", "ANG", "XS"])
                        HL[nm] = hi
                    for nm, src in (("Zre", Zre), ("Zim", Zim)):
                        hi = sbt(p0, nm + "_bd", [128, 32, 2, 128], BF16)
                        p.op("pool", (lambda h_: lambda e: e.memset(h_[:], 0.0))(hi), w=["HL", "T1", "T2", "A", "ANG", "XS"])
                        for a in range(2):
                            sl = slice(a * 64, (a + 1) * 64)
                            p.cp("pool", hi[sl, :, a, :], src[sl].rearrange("p k i c -> p k (i c)"), r=[nm], w=["HL"])
                        HL[nm] = hi
                    for k in range(32):
                        bk = 2 * (k % 2)
                        xr = XBre[:, k, :, :].rearrange("p i c -> p (i c)"); xi = XBim[:, k, :, :].rearrange("p i c -> p (i c)")
                        kb0 = ("bank", bk); kb1 = ("bank", bk + 1)
                        import os
                        PEVAR = int(os.environ.get("PEVAR", "0"))
                        if PEVAR in (0, 1):
                            p.tr(bank(bk)[:, 0:128], xr, identf[:], r=["XBre", "identf"], w=[kb0])
                            p.tr(bank(bk)[:, 128:256], xi, identf[:], r=["XBim", "identf"], w=[kb0])
                            p.cp("act", CtrlRe[:, k, :], bank(bk)[:, 0:128], r=[kb0], w=[("Ctrl", k)])
                            p.cp("act", CtrlIm[:, k, :], bank(bk)[:, 128:256], r=[kb0], w=[("Ctrl", k)])
                        o_ = bank(bk + 1)[:, 0:256]
                        p.mm(o_, HL["XBre"][:, k, :], HL["Zre"][:, k, :, :].rearrange("p a m -> p (a m)"), True, False, r=["HL"], w=[kb1])
                        p.mm(o_, HL["XBim"][:, k, :], HL["Zim"][:, k, :, :].rearrange("p a m -> p (a m)"), False, True, r=["HL"], w=[kb1])
                        p.tt(V, Toep[:, 2 * k:2 * k + 2, :].rearrange("p g m -> p (g m)"), bank(bk + 1)[:, 0:256], cmask[:], M, r=[kb1, "cmask"], w=[("Toep", k)])
                    if cut == 4:
                        p.dma("sp", dbgo[:, 0:2048], Toep[:, 0:32, :].rearrange("p a b -> p (a b)").bitcast(F32), r=[("Toep", k) for k in range(32)], w=["dbgo"])
                    cutpoint(4)
                p.barrier()
                if nphase < 1:
                    pass
                p1 = ExitStack()
                with p1:
                    Wglu = sbt(p1, "Wglu", [128, 8, 2 * D], BF16)
                    Xc = sbt(p1, "Xc", [128, 8, D])
                    bfA = sbt(p1, "bfA", [128, 8192], BF16)
                    bfB = sbt(p1, "bfB", [128, 8192], BF16)
                    Ere = sbt(p1, "Ere", [128, 32, 129], BF16); Eim = sbt(p1, "Eim", [128, 32, 129], BF16)
                    Vc = sbt(p1, "Vc", [128, 32, 2])
                    iota = sbt(p1, "iota", [128, 2, 128]); Dt = sbt(p1, "Dt", [128, D])
                    sg = sbt(p1, "sg", [128, D])
                    st6 = sbt(p1, "st6", [128, 2, 6]); mv = sbt(p1, "mv", [128, 2]); rstd = sbt(p1, "rstd", [128, 1])
                    NB = 2
                    tabs = [[sbt(p1, "tab%d_%d" % (i, j), [128, 128]) for j in range(8)] for i in range(NB)]
                    du = [sbt(p1, "du%d" % i, [128, 4, 8, 16]) for i in range(2)]
                    ln0 = load_ln(p1, 0)
                    p.dma("sp", iota[:], c_iota, w=["iota"])
                    p.dma("sp", Dt[:], dap(s5_d, 0, [[0, 128], [1, D]]), w=["Dt"])
                    for kc in range(8):
                        p.dma("pool", Wglu[:, kc, :], w_glu[kc * 128:(kc + 1) * 128, :], w=[("Wglu", kc)])
                    Xg = bfA[:].rearrange("p (g i c) -> p g i c", g=64, i=8)
                    Ycb = bfA[:].rearrange("p (j f) -> p j f", j=8)
                    Ub = bfB[:].rearrange("p (g n) -> p g n", g=64)
                    actT = bfB[:].rearrange("p (fc j n) -> p fc j n", fc=8, j=8)
                    for t in range(4 if nphase >= 1 else 0):
                        tis = t % 2
                        p.dma("sp", Xc[:], x_in[t], w=["Xc"] + [("Xc", j) for j in range(8)])
                        p.cp("pool", Xg, Xc[:].rearrange("p i (g c) -> p g i c", c=16), r=["Xc"], w=["bfA"] + [("Ycb", gq) for gq in range(16)])
                        if tis == 0:
                            p.op("pool", lambda e: e.memset(Ere[:, :, 0:1], 0.0), w=["Ecar"])
                            p.op("pool", lambda e: e.memset(Eim[:, :, 0:1], 0.0), w=["Ecar"])
                            p.op("pool", lambda e: e.memset(Vc[:], 0.0), w=[("Vc", k) for k in range(32)])
                        else:
                            p.cp("pool", Ere[:, :, 0:1], Ere[:, :, 128:129], r=[("E", k) for k in range(32)], w=["Ecar"])
                            p.cp("pool", Eim[:, :, 0:1], Eim[:, :, 128:129], r=[("E", k) for k in range(32)], w=["Ecar"])
                        for gq in range(8):
                            bk = gq % 2
                            kb = ("bank", bk)
                            for gg in range(8):
                                g = gq * 8 + gg
                                p.tr(bankb(bk)[:, gg * 128:(gg + 1) * 128], Xg[:, g, :, :].rearrange("p i c -> p (i c)"), identb[:], r=["bfA", "identb"], w=[kb])
                            p.cp("act", Ub[:, gq * 8:(gq + 1) * 8, :].rearrange("p g n -> p (g n)"), bankb(bk), r=[kb], w=[("Ub", gq)] + [("actT", j) for j in range(8)])
                        for k in range(32):
                            bk = 2 + (k % 3)
                            kb = ("bank", bk)
                            T = tabs[k % NB]
                            tk = ("tab", k % NB)
                            for a in range(2):
                                g = 2 * k + a
                                p.mm(bank(bk)[:, a * 128:(a + 1) * 128], CtrlRe[:, k, :], Ub[:, g, :], True, True, r=[("Ctrl", k), ("Ub", g // 8)], w=[kb])
                                p.mm(bank(bk)[:, 256 + a * 128:256 + (a + 1) * 128], CtrlIm[:, k, :], Ub[:, g, :], True, True, r=[("Ctrl", k), ("Ub", g // 8)], w=[kb])
                            h0 = slice(0, 64); h1 = slice(64, 128)
                            p.ts("dve", T[2][:], iota[:, tis, :], phi[:, k:k + 1], None, ALU.mult, None, r=["iota", "phi"], w=[tk])
                            sincos("dve", T[0][:], T[1][:], T[2][:], T[3][:], [tk], [tk])
                            sn, cn = T[0], T[1]
                            for hs, ro in ((h0, 0), (h1, 128)):
                                Wr = bank(bk)[hs, ro:ro + 128]; Wi = bank(bk)[hs, 256 + ro:256 + ro + 128]
                                p.tt("dve", T[2][hs], cn[hs], Wr, ALU.mult, r=[tk, kb], w=[tk])
                                p.tt("dve", T[3][hs], sn[hs], Wi, ALU.mult, r=[tk, kb], w=[tk])
                                p.tt("dve", T[4][hs], cn[hs], Wi, ALU.mult, r=[tk, kb], w=[tk])
                                p.tt("dve", T[5][hs], sn[hs], Wr, ALU.mult, r=[tk, kb], w=[tk])
                            p.tt("dve", T[2][:], T[2][:], T[3][:], ALU.add, r=[tk], w=[tk])
                            p.tt("dve", T[4][:], T[4][:], T[5][:], ALU.subtract, r=[tk], w=[tk])
                            rb = rho[:, k:k + 1].to_broadcast([128, 128])
                            p.op("dve", (lambda o_, d1, ini: lambda e: e.tensor_tensor_scan(out=o_, data0=rb, data1=d1, initial=ini, op0=ALU.mult, op1=ALU.add))(T[6][:], T[2][:], Vc[:, k, 0:1]), r=[tk, "rho", ("Vc", k)], w=[tk])
                            p.op("dve", (lambda o_, d1, ini: lambda e: e.tensor_tensor_scan(out=o_, data0=rb, data1=d1, initial=ini, op0=ALU.mult, op1=ALU.add))(T[7][:], T[4][:], Vc[:, k, 1:2]), r=[tk, "rho", ("Vc", k)], w=[tk])
                            p.cp("dve", Vc[:, k, 0:1], T[6][:, 127:128], r=[tk], w=[("Vc", k)])
                            p.cp("dve", Vc[:, k, 1:2], T[7][:, 127:128], r=[tk], w=[("Vc", k)])
                            p.tt("pool", T[3][:], cn[:], T[6][:], ALU.mult, r=[tk], w=[tk])
                            p.tt("pool", T[5][:], sn[:], T[7][:], ALU.mult, r=[tk], w=[tk])
                            p.tt("pool", Ere[:, k, 1:129], T[3][:], T[5][:], ALU.subtract, r=[tk], w=[("E", k)])
                            p.tt("pool", T[3][:], sn[:], T[6][:], ALU.mult, r=[tk], w=[tk])
                            p.tt("pool", T[5][:], cn[:], T[7][:], ALU.mult, r=[tk], w=[tk])
                            p.tt("pool", Eim[:, k, 1:129], T[3][:], T[5][:], ALU.add, r=[tk], w=[("E", k)])
                        for gq in range(16):
                            bk = 5 + (gq % 2)
                            kb = ("bank", bk)
                            for gg in range(4):
                                g = gq * 4 + gg
                                k = g // 2; a = g % 2
                                sl = slice(a * 64, (a + 1) * 64)
                                o_ = bank(bk)[:, gg * 128:(gg + 1) * 128]
                                p.mm(o_, Ub[:, g, :], Toep[:, g, :], True, False, r=[("Ub", g // 8), ("Toep", k)], w=[kb])
                                p.mm(o_, Ere[:, k, 0:128], ObRe[:, g, :], False, False, r=[("E", k), "Ecar", "ObRe"], w=[kb])
                                p.mm(o_, Eim[:, k, 0:128], ObIm[:, g, :], False, True, r=[("E", k), "Ecar", "ObIm"], w=[kb])
                            dd = du[gq % 2]
                            kd = ("du", gq % 2)
                            xv = Xc[:, :, gq * 64:(gq + 1) * 64].rearrange("p i (g c) -> p g i c", c=16)
                            dv = Dt[:, gq * 64:(gq + 1) * 64].rearrange("p (g c) -> p g c", c=16).unsqueeze(2).to_broadcast([128, 4, 8, 16])
                            p.tt("pool", dd[:], xv, dv, ALU.mult, r=["Xc", "Dt"], w=[kd])
                            p.tt("dve", dd[:], dd[:], bank(bk).rearrange("p (g j c) -> p g j c", g=4, j=8), ALU.add, r=[kd, kb], w=[kd])
                            yv = Ycb[:, :, gq * 64:(gq + 1) * 64].rearrange("p j (g c) -> p g j c", c=16)
                            p.act(yv, dd[:], AF.Gelu_apprx_tanh, r=[kd, ("Ub", 0), ("Ub", 7)], w=[("Ycb", gq)])
                        ykeys = [("Ycb", gq) for gq in range(16)]
                        for j in range(8):
                            bk = j % 2
                            kb = ("bank", bk)
                            for fc in range(8):
                                p.tr(bankb(bk)[:, fc * 128:(fc + 1) * 128], Ycb[:, j, fc * 128:(fc + 1) * 128], identb[:], r=ykeys + ["identb"], w=[kb])
                            p.cp("act", actT[:, :, j, :], bankb(bk).rearrange("p (fc n) -> p fc n", fc=8), r=[kb], w=[("Ub", gq2) for gq2 in range(8)] + [("actT", j)])
                        for j in range(8):
                            if j % 2 == 0:
                                pv, pg, kv, kg = PS[1], PS[2], [("bank", 2), ("bank", 3)], [("bank", 4), ("bank", 5)]
                            else:
                                pv, pg, kv, kg = PS[3], PS[0], [("bank", 6), ("bank", 7)], [("bank", 0), ("bank", 1)]
                            for half in range(4):
                                tgt = (pv if half < 2 else pg)[:, (half % 2) * 512:(half % 2) * 512 + 512]
                                kk = (kv if half < 2 else kg)[half % 2]
                                for fc in range(8):
                                    p.mm(tgt, actT[:, fc, j, :], Wglu[:, fc, half * 512:(half + 1) * 512], fc == 0, fc == 7,
                                         r=[("actT", j), ("Wglu", fc)], w=[kk])
                            p.act(sg[:], pg[:], AF.Sigmoid, r=kg, w=["sg"])
                            p.tt("dve", sg[:], sg[:], pv[:], ALU.mult, r=["sg"] + kv, w=["sg"])
                            blk = Xc[:, j, :]
                            kx = ("Xc", j)
                            p.stt("dve", blk, blk, ALPHA, sg[:], ALU.mult, ALU.add, r=["Xc", kx, "sg"] + [("du", 0), ("du", 1)], w=[kx])
                            layer_norm(blk, ln0, kx, (st6, mv, rstd))
                        p.dma("sp", xa[t], Xc[:], r=["Xc"] + [("Xc", j) for j in range(8)], w=[("xa", t)])
                p.barrier()
            if nphase >= 2:
                phase2(nc, p, top, sbt, PS, bank, bankb, identf, layer_norm, load_ln, xa, xb, ffn_w_in, ffn_w_out)
        except _Cut:
            pass
        if nphase >= 3:
            phase_attn(nc, p, sbt, PS, bank, bankb, identf, identb, layer_norm, load_ln, sincos, xb, xc,
                       (q_w_a, q_norm, q_w_b, o_w, kv_w_a, kv_norm, kv_w_b), (c_pos, c_invf, c_dmask))
        if nphase >= 5:
            if ROUTED:
                phase_moe_routed(nc, p, sbt, PS, bank, bankb, identf, identb, layer_norm, load_ln, xc, out, XE, YE, moe_router, moe_w_in, moe_w_out, (c_triu, c_ecol))
            else:
                phase_moe(nc, p, sbt, PS, bank, identf, layer_norm, load_ln, xc, out, moe_router, moe_w_in, moe_w_out)
        final = [o for o in p.dmaops["sp"][-RING:]] + [o for o in p.dmaops["pool"][-RING:]]
        p.op("sp", None, extra=final)
        with nc.allow_non_contiguous_dma(reason="tiny strided parameter loads"):
            p.emit()
        p.peak = ar["peak"]
    return nc, p


def phase2(nc, p, top, sbt, PS, bank, bankb, identf, layer_norm, load_ln, xa, xb, ffn_w_in, ffn_w_out):
    es = ExitStack()
    with es:
        Win = sbt(es, "Win", [128, 8, 2 * FFN], BF16)
        Wout = sbt(es, "Wout", [128, 22, D], BF16)
        Xh = sbt(es, "Xh", [128, 4, D])
        xT = sbt(es, "xT", [128, 8, 4, 128], BF16)
        hT = sbt(es, "hT", [128, 22, 512], BF16)
        slt = [sbt(es, "slt%d" % i, [128, 512], BF16) for i in range(2)]
        st6 = sbt(es, "st6b", [128, 2, 6]); mv = sbt(es, "mvb", [128, 2]); rstd = sbt(es, "rstdb", [128, 1])
        ln1 = load_ln(es, 1)
        for kc in range(8):
            p.dma("pool", Win[:, kc, :], ffn_w_in[kc * 128:(kc + 1) * 128, :], w=[("Win", kc)])
        for fc in range(22):
            p.dma("pool", Wout[:, fc, :], ffn_w_out[fc * 128:(fc + 1) * 128, :], w=[("Wout", fc)])
        for t in range(4):
            for jh in range(2):
                p.dma("sp", Xh[:], xa[t][:, jh * 4:(jh + 1) * 4, :], r=[("xa", t)], w=["Xh"] + [("Xh", j) for j in range(4)])
                for j in range(4):
                    for dq in range(2):
                        bk = (j * 2 + dq) % 2
                        kb = ("bank", bk)
                        for dd in range(4):
                            dc = dq * 4 + dd
                            p.tr(bank(bk)[:, dd * 128:(dd + 1) * 128], Xh[:, j, dc * 128:(dc + 1) * 128], identf[:], r=["Xh", "identf"], w=[kb])
                        p.cp("act", xT[:, dq * 4:(dq + 1) * 4, j, :], bank(bk).rearrange("p (d n) -> p d n", d=4), r=[kb], w=[("xT", j)])
                xkeys = [("xT", j) for j in range(4)]
                for fc in range(22):
                    bg = 2 + 2 * (fc % 3); bu = bg + 1
                    kg = ("bank", bg); ku = ("bank", bu)
                    for dc in range(8):
                        p.mm(bank(bg), Win[:, dc, fc * 128:(fc + 1) * 128], xT[:, dc, :, :].rearrange("p j n -> p (j n)"), dc == 0, dc == 7, r=xkeys + [("Win", dc)], w=[kg])
                    for dc in range(8):
                        p.mm(bank(bu), Win[:, dc, FFN + fc * 128:FFN + (fc + 1) * 128], xT[:, dc, :, :].rearrange("p j n -> p (j n)"), dc == 0, dc == 7, r=xkeys + [("Win", dc)], w=[ku])
                    s_ = slt[fc % 2]
                    ks = ("slt", fc % 2)
                    p.act(s_[:], bank(bg), AF.Silu, r=[kg], w=[ks])
                    p.tt("dve", hT[:, fc, :], s_[:], bank(bu), ALU.mult, r=[ks, ku], w=[("hT", fc)])
                for j in range(4):
                    po = PS[0]
                    ko = [("bank", 0), ("bank", 1)]
                    for nh in range(2):
                        for fc in range(22):
                            p.mm(po[:, nh * 512:(nh + 1) * 512], hT[:, fc, j * 128:(j + 1) * 128], Wout[:, fc, nh * 512:(nh + 1) * 512], fc == 0, fc == 21,
                                 r=[("hT", fc), ("Wout", fc)], w=[ko[nh]])
                    blk = Xh[:, j, :]
                    kx = ("Xh", j)
                    p.stt("dve", blk, blk, ALPHA, po[:], ALU.mult, ALU.add, r=["Xh", kx] + ko, w=[kx])
                    layer_norm(blk, ln1, kx, (st6, mv, rstd))
                p.dma("sp", xb[t][:, jh * 4:(jh + 1) * 4, :], Xh[:], r=["Xh"] + [("Xh", j) for j in range(4)], w=[("xb", t)])
    p.barrier()


def make_consts():
    c = {}
    c["c_ident"] = np.eye(128, dtype=np.float32)
    ev = np.array([7, 6, 5, 4, 3, 2, 1, 0, -7, -6, -5, -4, -3, -2, -1, 0, 1, 2, 3, 4, 5, 6, 7, 8], np.float32)
    c["c_evec"] = np.tile(ev[None, :], (128, 1))
    i_idx = np.arange(128) // 16
    cm = (i_idx[None, :] >= i_idx[:, None]).astype(np.float32)
    c["c_cmask"] = np.concatenate([cm, cm], axis=1)
    io = np.arange(256, dtype=np.float32).reshape(2, 128)
    c["c_iota"] = np.tile(io[None], (128, 1, 1))
    c["c_triu"] = (np.arange(128)[:, None] < np.arange(128)[None, :]).astype(np.float32)
    c["c_ecol"] = np.tile((np.arange(NEXP, dtype=np.float32) * CAP)[None, :], (128, 1))
    c["c_dmask"] = (np.arange(128)[:, None] <= np.arange(128)[None, :]).astype(np.float32)
    c["c_pos"] = np.tile(np.arange(2048, dtype=np.float32)[None, :], (64, 1))
    invf = ((10000.0 ** (-np.arange(0, 64, 2, dtype=np.float64) / 64)) / (2 * np.pi)).astype(np.float32)
    sgn = np.concatenate([-np.ones(32, np.float32), np.ones(32, np.float32)])
    c["c_invf"] = np.stack([np.concatenate([invf, invf]), sgn], axis=1).astype(np.float32)
    return c


_CACHE = {}


def kernel(**inputs):
    x = np.ascontiguousarray(inputs["x"], dtype=np.float32)
    if "nc" not in _CACHE:
        _CACHE["nc"] = build_program()[0]
    nc = _CACHE["nc"]
    consts = make_consts()
    shared = dict(consts)
    f = lambda k: np.ascontiguousarray(inputs[k], dtype=np.float32)
    shared["s5_lam_re"] = f("s5_lam_re")[0]; shared["s5_lam_im"] = f("s5_lam_im")[0]; shared["s5_log_step"] = f("s5_log_step")[0].reshape(1, 64)
    shared["s5_b_re"] = f("s5_b_re")[0]; shared["s5_b_im"] = f("s5_b_im")[0]; shared["s5_c_re"] = f("s5_c_re")[0]; shared["s5_c_im"] = f("s5_c_im")[0]
    shared["s5_d"] = f("s5_d")[0].reshape(1, D); shared["s5_w_glu"] = f("s5_w_glu")[0]
    shared["mla_q_w_a"] = f("mla_q_w_a")[0]; shared["mla_q_norm"] = f("mla_q_norm")[0].reshape(256, 1); shared["mla_q_w_b"] = f("mla_q_w_b")[0]
    shared["mla_o_w"] = f("mla_o_w")[0]; shared["kv_w_a"] = f("kv_w_a"); shared["kv_norm"] = f("kv_norm").reshape(128, 1); shared["kv_w_b"] = f("kv_w_b")
    shared["ffn_w_in"] = f("ffn_w_in")[0]; shared["ffn_w_out"] = f("ffn_w_out")[0]
    shared["moe_router"] = f("moe_router")[0]; shared["moe_w_in"] = f("moe_w_in")[0]; shared["moe_w_out"] = f("moe_w_out")[0]
    if _CACHE.get("small_moe"):
        shared["moe_w_in"] = np.zeros((1, 1, 2), np.float32); shared["moe_w_out"] = np.zeros((1, 1, 2), np.float32)
    shared["ln_g"] = f("ln_g").reshape(4, D); shared["ln_b"] = f("ln_b").reshape(4, D)
    in_maps = []
    for c in range(NCORES):
        m = dict(shared)
        m["x"] = x[2 * c:2 * c + 2].reshape(4, 128, 8, D)
        in_maps.append(m)
    res = run_bass_kernel_spmd(nc, in_maps, core_ids=list(range(NCORES)))
    outs = [np.asarray(r["out"]).reshape(2, 2048, D) for r in res.results]
    return np.concatenate(outs, axis=0).astype(np.float32)


def phase_moe(nc, p, sbt, PS, bank, identf, layer_norm, load_ln, xc, out, moe_router, moe_w_in, moe_w_out):
    es = ExitStack()
    with es:
        G = sbt(es, "G", [128, 32, NEXP])
        g1 = ExitStack()
        with g1:
            wrb = sbt(g1, "wrb", [128, D, NEXP]); Xg_ = sbt(g1, "Xcg", [128, 8, D]); junk = sbt(g1, "junk", [128, D])
            lg = sbt(g1, "lg", [128, NEXP]); m8 = sbt(g1, "m8", [128, 8]); dd = sbt(g1, "dd", [128, 2])
            m1 = sbt(g1, "m1", [128, NEXP]); m2 = sbt(g1, "m2", [128, NEXP])
            p.dma("sp", wrb[:].rearrange("p d e -> p (d e)"), dap(moe_router, 0, [[0, 128], [1, D * NEXP]]), w=["wrb"])
            for t in range(4):
                p.dma("sp", Xg_[:], xc[t], r=[("xc", t)], w=["Xcg"])
                for j in range(8):
                    for e in range(NEXP):
                        p.tt("dve", junk[:], Xg_[:, j, :], wrb[:, :, e], ALU.mult, r=["Xcg", "wrb"], w=["junk"])
                        p.op("dve", (lambda e_: lambda en: en.reduce_sum(out=lg[:, e_:e_ + 1], in_=junk[:], axis=AX.X))(e), r=["junk"], w=["lg"])
                    p.op("dve", lambda en: en.max(out=m8[:], in_=lg[:]), r=["lg"], w=["m8"])
                    p.tt("dve", dd[:, 0:1], m8[:, 1:2], m8[:, 0:1], ALU.subtract, r=["m8"], w=["dd"])
                    p.act(dd[:, 1:2], dd[:, 0:1], AF.Sigmoid, r=["dd"], w=["dd"])
                    p.ts("dve", dd[:, 0:1], dd[:, 1:2], -1.0, 1.0, ALU.mult, ALU.add, r=["dd"], w=["dd"])
                    p.ts("dve", m1[:], lg[:], m8[:, 0:1], dd[:, 0:1], ALU.is_equal, ALU.mult, r=["lg", "m8", "dd"], w=["m1"])
                    p.ts("dve", m2[:], lg[:], m8[:, 1:2], dd[:, 1:2], ALU.is_equal, ALU.mult, r=["lg", "m8", "dd"], w=["m2"])
                    p.tt("dve", G[:, t * 8 + j, :], m1[:], m2[:], ALU.add, r=["m1", "m2"], w=[("G", t)])
        p.barrier()
        Xc = sbt(es, "Xcm", [128, 8, D]); acc = sbt(es, "acc", [128, 8, D]); xT = sbt(es, "xTm", [128, 8, 8, 128], BF16)
        Wi = [sbt(es, "Wi%d" % i, [128, 8, 2, 896], BF16) for i in range(2)]
        Wo2 = [sbt(es, "Wo%d" % i, [128, 7, D], BF16) for i in range(2)]
        hT = sbt(es, "hTm", [128, 7, 512], BF16)
        slt = [sbt(es, "sltm%d" % i, [128, 512], BF16) for i in range(2)]
        st6 = sbt(es, "st6m", [128, 2, 6]); mv = sbt(es, "mvm", [128, 2]); rstd = sbt(es, "rstdm", [128, 1])
        ln3 = load_ln(es, 3)
        idx = 0
        for t in range(4):
            p.dma("sp", Xc[:], xc[t], r=[("xc", t)], w=["Xcm"] + [("Xcm", j) for j in range(8)])
            p.op("pool", lambda en: en.memset(acc[:], 0.0), w=[("acc", j) for j in range(8)])
            for j in range(8):
                for dq in range(2):
                    bk = dq
                    kb = ("bank", bk)
                    for d4 in range(4):
                        dc = dq * 4 + d4
                        p.tr(bank(bk)[:, d4 * 128:(d4 + 1) * 128], Xc[:, j, dc * 128:(dc + 1) * 128], identf[:], r=["Xcm", "identf"], w=[kb])
                    p.cp("act", xT[:, dq * 4:(dq + 1) * 4, j, :], bank(bk).rearrange("p (d n) -> p d n", d=4), r=[kb], w=[("xTm", j // 4)])
            for e in range(NEXP):
                for qh in range(4):
                    wi = Wi[idx % 2]; wo = Wo2[idx % 2]
                    kwi = ("Wi", idx % 2); kwo = ("Wo", idx % 2)
                    idx += 1
                    for gu in range(2):
                        p.dma("pool", wi[:, :, gu, :], dap(moe_w_in, e * D * 2 * EDIM + gu * EDIM + qh * 896, [[2 * EDIM, 128], [2 * EDIM * 128, 8], [1, 896]]), w=[kwi])
                    p.dma("pool", wo[:], dap(moe_w_out, (e * EDIM + qh * 896) * D, [[D, 128], [128 * D, 7], [1, D]]), w=[kwo])
                    for half in range(2):
                        xk = [("xTm", half)]
                        for fc in range(7):
                            bg = 2 + 2 * (fc % 3); bu = bg + 1
                            kg = ("bank", bg); ku = ("bank", bu)
                            rhs_ = lambda dc: xT[:, dc, half * 4:(half + 1) * 4, :].rearrange("p j n -> p (j n)")
                            for dc in range(8):
                                p.mm(bank(bg), wi[:, dc, 0, fc * 128:(fc + 1) * 128], rhs_(dc), dc == 0, dc == 7, r=xk + [kwi], w=[kg])
                            for dc in range(8):
                                p.mm(bank(bu), wi[:, dc, 1, fc * 128:(fc + 1) * 128], rhs_(dc), dc == 0, dc == 7, r=xk + [kwi], w=[ku])
                            s_ = slt[fc % 2]; ks = ("sltm", fc % 2)
                            p.act(s_[:], bank(bg), AF.Silu, r=[kg], w=[ks])
                            p.tt("dve", hT[:, fc, :], s_[:], bank(bu), ALU.mult, r=[ks, ku], w=[("hTm", fc)])
                        for j in range(4):
                            jj = half * 4 + j
                            ko = [("bank", 0), ("bank", 1)]
                            for nh in range(2):
                                for fc in range(7):
                                    p.mm(PS[0][:, nh * 512:(nh + 1) * 512], hT[:, fc, j * 128:(j + 1) * 128], wo[:, fc, nh * 512:(nh + 1) * 512], fc == 0, fc == 6,
                                         r=[("hTm", fc), kwo], w=[ko[nh]])
                            p.stt("dve", acc[:, jj, :], PS[0][:], G[:, t * 8 + jj, e:e + 1], acc[:, jj, :], ALU.mult, ALU.add, r=ko + [("G", t), ("acc", jj)], w=[("acc", jj)])
            for j in range(8):
                blk = Xc[:, j, :]; kx = ("Xcm", j)
                p.stt("dve", blk, blk, ALPHA, acc[:, j, :], ALU.mult, ALU.add, r=["Xcm", kx, ("acc", j), ("xTm", 0), ("xTm", 1)], w=[kx])
                layer_norm(blk, ln3, kx, (st6, mv, rstd))
            p.dma("sp", out[t], Xc[:], r=["Xcm"] + [("Xcm", j) for j in range(8)], w=[("out", t)])
    p.barrier()


def phase_attn(nc, p, sbt, PS, bank, bankb, identf, identb, layer_norm, load_ln, sincos, xb, xc, W, consts):
    q_w_a, q_norm, q_w_b, o_w, kv_w_a, kv_norm, kv_w_b = W
    c_pos, c_invf, c_dmask = consts
    SCALE = 1.0 / math.sqrt(192.0)
    es = ExitStack()
    with es:
        Wqa = sbt(es, "Wqa", [128, 8, 256], BF16); Wkva = sbt(es, "Wkva", [128, 8, 192], BF16); WkvaB = sbt(es, "WkvaB", [128, 8, 64], BF16)
        Wqb = sbt(es, "Wqb", [128, 2, 1536], BF16); WqbB = sbt(es, "WqbB", [128, 2, 8, 64], BF16)
        Wkvb = sbt(es, "Wkvb", [128, 2, 8, 128], BF16); Wo = sbt(es, "Wo", [128, 8, D], BF16)
        qn = sbt(es, "qn", [128, 2]); kvn = sbt(es, "kvn", [128, 1]); invf = sbt(es, "invf", [128, 2])
        CC = sbt(es, "CC", [128, 2048]); SS = sbt(es, "SS", [128, 2048])
        dmask = sbt(es, "dmask", [128, 128], BF16); onesb = sbt(es, "onesb", [128, 128], BF16)
        epsr = sbt(es, "epsr", [128, 1])
        cqT = sbt(es, "cqT", [128, 2, 2048], BF16); ckvT = sbt(es, "ckvT", [128, 2048], BF16); krT = sbt(es, "krT", [128, 2048], BF16)
        OT = sbt(es, "OT", [128, 8, 2048], BF16)
        Xh = sbt(es, "Xha", [128, 4, D]); xT = sbt(es, "xTa", [128, 8, 4, 128], BF16)
        kTh = sbt(es, "kTh", [128, 2048], BF16); Vh = sbt(es, "Vh", [128, 16, 128], BF16)
        qTh = sbt(es, "qTh", [128, 2048], BF16); qrT = sbt(es, "qrT", [128, 2048], BF16)
        PT = [sbt(es, "PT%d" % i, [128, 512], BF16) for i in range(3)]
        rec = sbt(es, "rec", [128, 512]); t1 = sbt(es, "t1a", [128, 512]); t2 = sbt(es, "t2a", [128, 512])
        cqn = sbt(es, "cqn", [128, 256], BF16); ckvn = sbt(es, "ckvn", [128, 128], BF16)
        ss = sbt(es, "ss", [128, 4]); junk = sbt(es, "junka", [128, 256])
        st6 = sbt(es, "st6a", [128, 2, 6]); mv = sbt(es, "mva", [128, 2]); rstd = sbt(es, "rstda", [128, 1])
        ln2 = load_ln(es, 2)
        for dc in range(8):
            p.dma("pool", Wqa[:, dc, :], q_w_a[dc * 128:(dc + 1) * 128, :], w=["Wqa"])
            p.dma("pool", Wkva[:, dc, :], kv_w_a[dc * 128:(dc + 1) * 128, :], w=["Wkva"])
        p.dma("pool", WkvaB[:, :, 0:32], dap(kv_w_a, 160, [[192, 128], [192 * 128, 8], [1, 32]]), w=["WkvaB"])
        p.dma("pool", WkvaB[:, :, 32:64], dap(kv_w_a, 128, [[192, 128], [192 * 128, 8], [1, 32]]), w=["WkvaB"])
        for cc in range(2):
            p.dma("pool", Wqb[:, cc, :], q_w_b[cc * 128:(cc + 1) * 128, :], w=["Wqb"])
            p.dma("pool", WqbB[:, cc, :, 0:32], dap(q_w_b, cc * 128 * 1536 + 160, [[1536, 128], [192, 8], [1, 32]]), w=["WqbB"])
            p.dma("pool", WqbB[:, cc, :, 32:64], dap(q_w_b, cc * 128 * 1536 + 128, [[1536, 128], [192, 8], [1, 32]]), w=["WqbB"])
        for t_ in range(2):
            p.dma("pool", Wkvb[:, t_, :, :], dap(kv_w_b, t_ * 128, [[2048, 128], [256, 8], [1, 128]]), w=["Wkvb"])
        for h in range(8):
            p.dma("pool", Wo[:, h, :], o_w[h * 128:(h + 1) * 128, :], w=["Wo"])
        p.dma("sp", qn[:], dap(q_norm, 0, [[1, 128], [128, 2]]), w=["qn"])
        p.dma("sp", kvn[:], kv_norm, w=["kvn"])
        p.dma("sp", invf[0:64, :], c_invf, w=["invf"])
        p.dma("sp", CC[0:64, :], c_pos, w=["CC"])
        p.dma("pool", dmask[:], c_dmask, w=["dmask"])
        p.op("pool", lambda e: e.memset(onesb[:], 1.0), w=["onesb"])
        p.op("pool", lambda e: e.memset(epsr[:], RMS_EPS), w=["epsr"])
        p.op("pool", lambda e: e.memset(krT[:], 0.0), w=["krT"])
        p.op("pool", lambda e: e.memset(qrT[:], 0.0), w=["qrT"])
        for cc in range(2):
            p.ts("dve", Wqb[:, cc, :], Wqb[:, cc, :], qn[:, cc:cc + 1], None, ALU.mult, None, r=["Wqb", "qn"], w=["Wqb"])
            p.ts("dve", WqbB[:, cc, :, :], WqbB[:, cc, :, :], qn[:, cc:cc + 1], None, ALU.mult, None, r=["WqbB", "qn"], w=["WqbB"])
        p.ts("dve", Wkvb[:], Wkvb[:], kvn[:, 0:1], None, ALU.mult, None, r=["Wkvb", "kvn"], w=["Wkvb"])
        p.ts("dve", CC[0:64, :], CC[0:64, :], invf[0:64, 0:1], None, ALU.mult, None, r=["CC", "invf"], w=["CC"])
        p.cp("dve", SS[0:64, :], CC[0:64, :], r=["CC"], w=["SS"])
        for c0 in range(0, 2048, 512):
            sincos("dve", SS[0:64, c0:c0 + 512], CC[0:64, c0:c0 + 512], SS[0:64, c0:c0 + 512], t1[0:64, :], ["SS"], ["SS", "CC", "t1a"], npart=64)
        p.ts("dve", SS[0:64, :], SS[0:64, :], invf[0:64, 1:2], None, ALU.mult, None, r=["SS", "invf"], w=["SS"])
        for s_ in range(2):
            for tis in range(2):
                t = 2 * s_ + tis
                for jh in range(2):
                    p.dma("sp", Xh[:], xb[t][:, jh * 4:(jh + 1) * 4, :], r=[("xb", t)], w=["Xha"])
                    for j in range(4):
                        for dq in range(2):
                            bk = dq; kb = ("bank", bk)
                            for d4 in range(4):
                                dc = dq * 4 + d4
                                p.tr(bank(bk)[:, d4 * 128:(d4 + 1) * 128], Xh[:, j, dc * 128:(dc + 1) * 128], identf[:], r=["Xha", "identf"], w=[kb])
                            p.cp("act", xT[:, dq * 4:(dq + 1) * 4, j, :], bank(bk).rearrange("p (d n) -> p d n", d=4), r=[kb], w=["xTa"])
                    for j in range(4):
                        jg = jh * 4 + j
                        kb = ("bank", 2)
                        for dc in range(8):
                            p.mm(bank(2)[:, 0:256], xT[:, dc, j, :], Wqa[:, dc, :], dc == 0, dc == 7, r=["xTa", "Wqa"], w=[kb])
                        for dc in range(8):
                            p.mm(bank(2)[:, 256:384], xT[:, dc, j, :], Wkva[:, dc, 0:128], dc == 0, dc == 7, r=["xTa", "Wkva"], w=[kb])
                        p.act(junk[:], bank(2)[:, 0:256], AF.Square, r=[kb], w=["junka"], accum_out=ss[:, 0:1])
                        p.act(junk[:, 0:128], bank(2)[:, 256:384], AF.Square, r=[kb], w=["junka"], accum_out=ss[:, 1:2])
                        p.act(ss[:, 2:3], ss[:, 0:1], AF.Sqrt, r=["junka", "epsr"], w=["ss"], scale=1.0 / 256, bias=epsr[:, 0:1])
                        p.act(ss[:, 3:4], ss[:, 1:2], AF.Sqrt, r=["junka", "epsr"], w=["ss"], scale=1.0 / 128, bias=epsr[:, 0:1])
                        p.op("dve", lambda e: e.reciprocal(out=ss[:, 2:4], in_=ss[:, 2:4]), r=["ss"], w=["ss"])
                        p.ts("dve", cqn[:], bank(2)[:, 0:256], ss[:, 2:3], None, ALU.mult, None, r=[kb, "ss"], w=["cqn"])
                        p.ts("dve", ckvn[:], bank(2)[:, 256:384], ss[:, 3:4], None, ALU.mult, None, r=[kb, "ss"], w=["ckvn"])
                        kb3 = ("bank", 3)
                        for cc in range(2):
                            p.tr(bankb(3)[:, cc * 128:(cc + 1) * 128], cqn[:, cc * 128:(cc + 1) * 128], identb[:], r=["cqn", "identb"], w=[kb3])
                        p.tr(bankb(3)[:, 256:384], ckvn[:], identb[:], r=["ckvn", "identb"], w=[kb3])
                        for cc in range(2):
                            dst = cqT[:, cc, tis * 1024:(tis + 1) * 1024].rearrange("p (n j) -> p n j", j=8)[:, :, jg]
                            p.cp("act", dst, bankb(3)[:, cc * 128:(cc + 1) * 128], r=[kb3], w=["cqT"])
                        dst = ckvT[:, tis * 1024:(tis + 1) * 1024].rearrange("p (n j) -> p n j", j=8)[:, :, jg]
                        p.cp("act", dst, bankb(3)[:, 256:384], r=[kb3], w=["ckvT"])
                    kb4 = ("bank", 4); kb5 = ("bank", 5)
                    for dc in range(8):
                        p.mm(bank(4)[0:64, :], Wkva[:, dc, 128:192], xT[:, dc, :, :].rearrange("p j n -> p (j n)"), dc == 0, dc == 7, r=["xTa", "Wkva"], w=[kb4])
                    for dc in range(8):
                        p.mm(bank(5)[0:64, :], WkvaB[:, dc, :], xT[:, dc, :, :].rearrange("p j n -> p (j n)"), dc == 0, dc == 7, r=["xTa", "WkvaB"], w=[kb5])
                    vw = lambda T_: T_[0:64, tis * 1024:(tis + 1) * 1024].rearrange("p (n j) -> p j n", j=8)[:, jh * 4:(jh + 1) * 4, :]
                    p4 = lambda b_: b_[0:64, :].rearrange("p (j n) -> p j n", j=4)
                    t1v = t1[0:64, :].rearrange("p (j n) -> p j n", j=4); t2v = t2[0:64, :].rearrange("p (j n) -> p j n", j=4)
                    p.tt("dve", t1v, p4(bank(4)), vw(CC), ALU.mult, r=[kb4, "CC"], w=["t1a"])
                    p.tt("dve", t2v, p4(bank(5)), vw(SS), ALU.mult, r=[kb5, "SS"], w=["t2a"])
                    p.tt("dve", vw(krT), t1v, t2v, ALU.add, r=["t1a", "t2a"], w=["krT"])
            for h in range(8):
                for ch in range(4):
                    cs = slice(ch * 512, (ch + 1) * 512)
                    kb = ("bank", ch % 2)
                    p.mm(bank(ch % 2), Wkvb[:, 0, h, :], ckvT[:, cs], True, True, r=["Wkvb", "ckvT"], w=[kb])
                    p.cp("act", kTh[:, cs], bank(ch % 2), r=[kb], w=["kTh"])
                for pq in range(4):
                    kb = ("bank", 2 + pq % 2)
                    for pb in range(4):
                        pbk = pq * 4 + pb
                        p.mm(bank(2 + pq % 2)[:, pb * 128:(pb + 1) * 128], ckvT[:, pbk * 128:(pbk + 1) * 128], Wkvb[:, 1, h, :], True, True, r=["Wkvb", "ckvT"], w=[kb])
                    p.cp("act", Vh[:, pq * 4:(pq + 1) * 4, :].rearrange("p a d -> p (a d)"), bank(2 + pq % 2), r=[kb], w=["Vh"])
                for ch in range(4):
                    cs = slice(ch * 512, (ch + 1) * 512)
                    kb = ("bank", 4 + ch % 2)
                    for cc in range(2):
                        p.mm(bank(4 + ch % 2), Wqb[:, cc, h * 192:h * 192 + 128], cqT[:, cc, cs], cc == 0, cc == 1, r=["Wqb", "cqT"], w=[kb])
                    p.act(qTh[:, cs], bank(4 + ch % 2), AF.Copy, r=[kb], w=["qTh"], scale=SCALE)
                    kb6 = ("bank", 6); kb7 = ("bank", 7)
                    for cc in range(2):
                        p.mm(bank(6)[0:64, :], Wqb[:, cc, h * 192 + 128:h * 192 + 192], cqT[:, cc, cs], cc == 0, cc == 1, r=["Wqb", "cqT"], w=[kb6])
                    for cc in range(2):
                        p.mm(bank(7)[0:64, :], WqbB[:, cc, h, :], cqT[:, cc, cs], cc == 0, cc == 1, r=["WqbB", "cqT"], w=[kb7])
                    p.stt("dve", t1[0:64, :], bank(6)[0:64, :], SCALE, CC[0:64, cs], ALU.mult, ALU.mult, r=[kb6, "CC"], w=["t1a"])
                    p.stt("dve", t2[0:64, :], bank(7)[0:64, :], SCALE, SS[0:64, cs], ALU.mult, ALU.mult, r=[kb7, "SS"], w=["t2a"])
                    p.tt("dve", qrT[0:64, cs], t1[0:64, :], t2[0:64, :], ALU.add, r=["t1a", "t2a"], w=["qrT"])
                for qc in range(4):
                    nkb = 4 * (qc + 1)
                    ko = ("bank", 0); kd = ("bank", 1)
                    def geom(kbi):
                        q0 = max(kbi * 128, qc * 512)
                        return q0, (qc + 1) * 512 - q0, q0 - qc * 512

                    def emit_S(kbi):
                        q0, N, off = geom(kbi)
                        sb_ = 2 + (kbi % 4)
                        ksb = ("bank", sb_)
                        ks_ = slice(kbi * 128, (kbi + 1) * 128)
                        p.mm(bank(sb_)[:, 0:N], kTh[:, ks_], qTh[:, q0:q0 + N], True, False, r=["kTh", "qTh"], w=[ksb])
                        p.mm(bank(sb_)[:, 0:N], krT[:, ks_], qrT[:, q0:q0 + N], False, True, r=["krT", "qrT"], w=[ksb])

                    def emit_P(kbi):
                        q0, N, off = geom(kbi)
                        sb_ = 2 + (kbi % 4)
                        ksb = ("bank", sb_)
                        pt = PT[kbi % 3]; kpt = ("PT", kbi % 3)
                        p.act(pt[:, 0:N], bank(sb_)[:, 0:N], AF.Exp, r=[ksb], w=[kpt])
                        if kbi * 128 >= qc * 512:
                            p.tt("pool", pt[:, 0:128], pt[:, 0:128], dmask[:], ALU.mult, r=[kpt, "dmask"], w=[kpt])

                    def emit_V(kbi):
                        q0, N, off = geom(kbi)
                        pt = PT[kbi % 3]; kpt = ("PT", kbi % 3)
                        p.mm(bank(0)[:, off:off + N], Vh[:, kbi, :], pt[:, 0:N], kbi == 0, kbi == nkb - 1, r=["Vh", kpt], w=[ko])
                        p.mm(bank(1)[:, off:off + N], onesb[:], pt[:, 0:N], kbi == 0, kbi == nkb - 1, r=["onesb", kpt], w=[kd])

                    emit_S(0)
                    if nkb > 1:
                        emit_S(1)
                    for kbi in range(nkb):
                        emit_P(kbi)
                        if kbi + 2 < nkb:
                            emit_S(kbi + 2)
                        emit_V(kbi)
                    p.op("dve", lambda e: e.reciprocal(out=rec[:], in_=bank(1)), r=[kd], w=["rec"])
                    p.tt("dve", OT[:, h, qc * 512:(qc + 1) * 512], bank(0), rec[:], ALU.mult, r=[ko, "rec"], w=["OT"])
            for tis in range(2):
                t = 2 * s_ + tis
                for jh in range(2):
                    p.dma("sp", Xh[:], xb[t][:, jh * 4:(jh + 1) * 4, :], r=[("xb", t)], w=["Xha"] + [("Xha", j) for j in range(4)])
                    for j in range(4):
                        jg = jh * 4 + j
                        ko = [("bank", 6), ("bank", 7)]
                        for nh in range(2):
                            for h in range(8):
                                lh = OT[:, h, tis * 1024:(tis + 1) * 1024].rearrange("p (n j) -> p n j", j=8)[:, :, jg]
                                p.mm(PS[3][:, nh * 512:(nh + 1) * 512], lh, Wo[:, h, nh * 512:(nh + 1) * 512], h == 0, h == 7, r=["OT", "Wo"], w=[ko[nh]])
                        blk = Xh[:, j, :]; kx = ("Xha", j)
                        p.stt("dve", blk, blk, ALPHA, PS[3][:], ALU.mult, ALU.add, r=["Xha", kx] + ko, w=[kx])
                        layer_norm(blk, ln2, kx, (st6, mv, rstd))
                    p.dma("sp", xc[t][:, jh * 4:(jh + 1) * 4, :], Xh[:], r=["Xha"] + [("Xha", j) for j in range(4)], w=[("xc", t)])
    p.barrier()


def phase_moe_routed(nc, p, sbt, PS, bank, bankb, identf, identb, layer_norm, load_ln, xc, out, XE, YE, moe_router, moe_w_in, moe_w_out, consts):
    c_triu, c_ecol = consts
    NT = CAP // 128
    es = ExitStack()
    with es:
        S1 = [sbt(es, "S1_%d" % i, [128, 1], I32) for i in range(32)]; S2 = [sbt(es, "S2_%d" % i, [128, 1], I32) for i in range(32)]
        G1 = sbt(es, "G1", [128, 32]); G2 = sbt(es, "G2", [128, 32])
        g1 = ExitStack()
        with g1:
            wrb = sbt(g1, "wrb", [128, D, NEXP]); Xg_ = sbt(g1, "Xcg", [128, 8, D]); junk = sbt(g1, "junk", [128, D])
            Xb = [sbt(g1, "Xb%d" % i, [128, D], BF16) for i in range(2)]
            zt = sbt(g1, "zt", [128, NT, D], BF16)
            lg = sbt(g1, "lg", [128, NEXP]); m8 = sbt(g1, "m8", [128, 8]); dd = sbt(g1, "dd", [128, 1])
            mk = sbt(g1, "mk", [128, NEXP]); mkb = sbt(g1, "mkb", [128, NEXP], BF16)
            m1 = sbt(g1, "m1", [128, NEXP]); m2 = sbt(g1, "m2", [128, NEXP]); dest = sbt(g1, "dest", [128, NEXP]); ovf = sbt(g1, "ovf", [128, NEXP])
            base = sbt(g1, "base", [128, NEXP]); ecol = sbt(g1, "ecol", [128, NEXP]); sf = sbt(g1, "sf", [128, 2])
            triu = sbt(g1, "triu", [128, 128], BF16); onesb = sbt(g1, "onesb2", [128, 128], BF16)
            p.dma("sp", wrb[:].rearrange("p d e -> p (d e)"), dap(moe_router, 0, [[0, 128], [1, D * NEXP]]), w=["wrb"])
            p.dma("sp", ecol[:], c_ecol, w=["ecol"])
            p.dma("pool", triu[:], c_triu, w=["triu"])
            p.op("pool", lambda e: e.memset(onesb[:], 1.0), w=["onesb2"])
            p.op("pool", lambda e: e.memset(base[:], 0.0), w=["base"])
            p.op("pool", lambda e: e.memset(zt[:], 0.0), w=["zt"])
            for e in range(NEXP):
                p.dma("sp", XE[e * CAP:(e + 1) * CAP, :].rearrange("(t p) d -> p t d", p=128), zt[:], r=["zt"], w=["XE"])
            p.op("pool", lambda e: e.memset(junk[:], 0.0), w=["junk"])
            p.dma("sp", YE[NEXP * CAP:NEXP * CAP + 128, :], junk[:], r=["junk"], w=["YE"])
            for t in range(4):
                p.dma("sp", Xg_[:], xc[t], r=[("xc", t)], w=["Xcg"])
                for j in range(8):
                    b = t * 8 + j
                    xb_ = Xb[b % 2]; kxb = ("Xb", b % 2)
                    p.cp("act", xb_[:], Xg_[:, j, :], r=["Xcg"], w=[kxb])
                    for e in range(NEXP):
                        p.op("dve", (lambda e_, j_: lambda en: en.scalar_tensor_tensor(out=junk[:], in0=Xg_[:, j_, :], scalar=1.0, in1=wrb[:, :, e_],
                                                                                 op0=ALU.mult, op1=ALU.mult, accum_out=lg[:, e_:e_ + 1]))(e, j),
                             r=["Xcg", "wrb"], w=["junk", "lg"])
                    p.op("dve", lambda en: en.max(out=m8[:], in_=lg[:]), r=["lg"], w=["m8"])
                    p.tt("dve", dd[:], m8[:, 1:2], m8[:, 0:1], ALU.subtract, r=["m8"], w=["dd"])
                    p.act(G2[:, b:b + 1], dd[:], AF.Sigmoid, r=["dd"], w=[("G", b)])
                    p.ts("dve", G1[:, b:b + 1], G2[:, b:b + 1], -1.0, 1.0, ALU.mult, ALU.add, r=[("G", b)], w=[("G", b)])
                    p.ts("dve", mk[:], lg[:], m8[:, 1:2], None, ALU.is_ge, None, r=["lg", "m8"], w=["mk"])
                    p.cp("dve", mkb[:], mk[:], r=["mk"], w=["mkb"])
                    p.ts("dve", m1[:], lg[:], m8[:, 0:1], None, ALU.is_equal, None, r=["lg", "m8"], w=["m1"])
                    p.tt("dve", m2[:], mk[:], m1[:], ALU.subtract, r=["mk", "m1"], w=["m2"])
                    kb = ("bank", 2 + b % 2)
                    pb_ = bank(2 + b % 2)
                    p.mm(pb_[:, 0:NEXP], triu[:], mkb[:], True, True, r=["triu", "mkb"], w=[kb])
                    p.mm(pb_[:, 8:8 + NEXP], onesb[:], mkb[:], True, True, r=["onesb2", "mkb"], w=[kb])
                    p.tt("dve", dest[:], pb_[:, 0:NEXP], base[:], ALU.add, r=[kb, "base"], w=["dest"])
                    p.ts("dve", ovf[:], dest[:], float(CAP), 1.0e6, ALU.is_ge, ALU.mult, r=["dest"], w=["ovf"])
                    p.tt("dve", dest[:], dest[:], ecol[:], ALU.add, r=["dest", "ecol"], w=["dest"])
                    p.tt("dve", dest[:], dest[:], ovf[:], ALU.add, r=["dest", "ovf"], w=["dest"])
                    p.tt("dve", base[:], base[:], pb_[:, 8:8 + NEXP], ALU.add, r=[kb, "base", "dest"], w=["base"])
                    p.tt("dve", m1[:], m1[:], dest[:], ALU.mult, r=["m1", "dest"], w=["m1"])
                    p.tt("dve", m2[:], m2[:], dest[:], ALU.mult, r=["m2", "dest"], w=["m2"])
                    p.op("dve", lambda en: en.reduce_sum(out=sf[:, 0:1], in_=m1[:], axis=AX.X), r=["m1"], w=["sf"])
                    p.op("dve", lambda en: en.reduce_sum(out=sf[:, 1:2], in_=m2[:], axis=AX.X), r=["m2"], w=["sf"])
                    p.ts("dve", sf[:], sf[:], float(NEXP * CAP), None, ALU.min, None, r=["sf"], w=["sf"])
                    p.cp("dve", S1[b][:], sf[:, 0:1], r=["sf"], w=[("S", b)])
                    p.cp("dve", S2[b][:], sf[:, 1:2], r=["sf"], w=[("S", b)])
                    for S_ in (S1, S2):
                        p.op("pool", (lambda S__, b_, x_: lambda en: en.indirect_dma_start(
                            out=XE[:, :], out_offset=bass.IndirectOffsetOnAxis(ap=S__[b_][:, :], axis=0),
                            in_=x_[:, :], in_offset=None, oob_is_err=False))(S_, b, xb_),
                            r=[kxb, ("S", b), "XE"], w=[("XEs", b)], dma=True)
        p.barrier()
        ex = ExitStack()
        with ex:
            XEe = sbt(ex, "XEe", [128, 2, D], BF16); xeT = sbt(ex, "xeT", [128, 8, CAP], BF16)
            SCW = 512
            CHUNKS = [(c0, min(512, CAP - c0)) for c0 in range(0, CAP, 512)]
            Wi = [sbt(ex, "Wi%d" % i, [128, 8, 2, 896], BF16) for i in range(2)]
            Wo2 = [sbt(ex, "Wo%d" % i, [128, 7, D], BF16) for i in range(2)]
            hT = sbt(ex, "hTm", [128, 7, CAP], BF16)
            slt = [sbt(ex, "sltm%d" % i, [128, SCW], BF16) for i in range(2)]
            yacc = sbt(ex, "yacc", [128, NT, D])
            stg = [sbt(ex, "stg%d" % i, [128, 896]) for i in range(4)]
            si = [0]
            idx = 0
            for e in range(NEXP):
                for st in range(NT):
                    bk = st % 2; kb = ("bank", bk)
                    kxe = ("XEe", st % 2)
                    p.dma("sp", XEe[:, st % 2, :], XE[e * CAP + st * 128:e * CAP + (st + 1) * 128, :], r=[("XEs", b_) for b_ in range(32)] if (e == 0 and st < 2) else [], w=[kxe])
                    for dc in range(8):
                        p.tr(bankb(bk)[:, dc * 128:(dc + 1) * 128], XEe[:, st % 2, dc * 128:(dc + 1) * 128], identb[:], r=[kxe, "identb"], w=[kb])
                    p.cp("act", xeT[:, :, st * 128:(st + 1) * 128], bankb(bk).rearrange("p (d n) -> p d n", d=8), r=[kb], w=["xeT"])
                for qh in range(4):
                    wi = Wi[idx % 2]; wo = Wo2[idx % 2]
                    kwi = ("Wi", idx % 2); kwo = ("Wo", idx % 2)
                    idx += 1
                    ib = (idx - 1) % 2
                    for gu in range(2):
                        for dc in range(8):
                            st_ = stg[si[0] % 4]; kst = ("stg", si[0] % 4); si[0] += 1
                            p.dma("sp", st_[:], dap(moe_w_in, e * D * 2 * EDIM + dc * 128 * 2 * EDIM + gu * EDIM + qh * 896, [[2 * EDIM, 128], [1, 896]]), w=[kst])
                            p.cp("pool", wi[:, dc, gu, :], st_[:], r=[kst], w=[("Wi", ib, dc, gu)])
                    p.dma("pool", wo[:], dap(moe_w_out, (e * EDIM + qh * 896) * D, [[D, 128], [128 * D, 7], [1, D]]), w=[kwo])
                    for fc in range(7):
                        for sc, (c0_, cw_) in enumerate(CHUNKS):
                            ci = fc * len(CHUNKS) + sc
                            bg = 2 + 2 * (ci % 3); bu = bg + 1
                            kg = ("bank", bg); ku = ("bank", bu)
                            cs = slice(c0_, c0_ + cw_)
                            for dc in range(8):
                                p.mm(bank(bg)[:, 0:cw_], wi[:, dc, 0, fc * 128:(fc + 1) * 128], xeT[:, dc, cs], dc == 0, dc == 7, r=["xeT", ("Wi", ib, dc, 0)], w=[kg])
                            for dc in range(8):
                                p.mm(bank(bu)[:, 0:cw_], wi[:, dc, 1, fc * 128:(fc + 1) * 128], xeT[:, dc, cs], dc == 0, dc == 7, r=["xeT", ("Wi", ib, dc, 1)], w=[ku])
                            s_ = slt[ci % 2]; ks = ("sltm", ci % 2)
                            p.act(s_[:, 0:cw_], bank(bg)[:, 0:cw_], AF.Silu, r=[kg], w=[ks])
                            p.tt("dve", hT[:, fc, cs], s_[:, 0:cw_], bank(bu)[:, 0:cw_], ALU.mult, r=[ks, ku], w=[("hTm", fc)])
                    for st in range(NT):
                        ko = [("bank", 0), ("bank", 1)]
                        for nh in range(2):
                            for fc in range(7):
                                p.mm(PS[0][:, nh * 512:(nh + 1) * 512], hT[:, fc, st * 128:(st + 1) * 128], wo[:, fc, nh * 512:(nh + 1) * 512], fc == 0, fc == 6,
                                     r=[("hTm", fc), kwo], w=[ko[nh]])
                        if qh == 0:
                            p.cp("act", yacc[:, st, :], PS[0][:], r=ko, w=[("yacc", st)])
                        else:
                            p.tt("dve", yacc[:, st, :], yacc[:, st, :], PS[0][:], ALU.add, r=ko + [("yacc", st)], w=[("yacc", st)])
                p.dma("sp", YE[e * CAP:(e + 1) * CAP, :].rearrange("(t p) d -> p t d", p=128), yacc[:], r=[("yacc", st) for st in range(NT)], w=["YE"])
        p.barrier()
        cb = ExitStack()
        with cb:
            Xc = sbt(cb, "Xcm", [128, 8, D])
            Y1 = [sbt(cb, "Y1_%d" % i, [128, D]) for i in range(16)]; Y2 = [sbt(cb, "Y2_%d" % i, [128, D]) for i in range(16)]
            st6 = sbt(cb, "st6m", [128, 2, 6]); mv = sbt(cb, "mvm", [128, 2]); rstd = sbt(cb, "rstdm", [128, 1])
            ln3 = load_ln(cb, 3)

            def gathers(t):
                for j in range(8):
                    b = t * 8 + j
                    ky = ("Y", b % 16)
                    for S_, y_ in ((S1, Y1[b % 16]), (S2, Y2[b % 16])):
                        p.op("pool", (lambda S__, b_, yy: lambda en: en.indirect_dma_start(
                            out=yy[:, :], out_offset=None, in_=YE[:, :],
                            in_offset=bass.IndirectOffsetOnAxis(ap=S__[b_][:, :], axis=0),
                            oob_is_err=False))(S_, b, y_),
                            r=["YE", ("S", b)], w=[ky], dma=True)

            gathers(0)
            for t in range(4):
                if t + 1 < 4:
                    gathers(t + 1)
                p.dma("sp", Xc[:], xc[t], r=[("xc", t)], w=["Xcm"] + [("Xcm", j) for j in range(8)])
                for j in range(8):
                    b = t * 8 + j
                    y1 = Y1[b % 16]; y2 = Y2[b % 16]; ky = ("Y", b % 16)
                    p.ts("dve", y1[:], y1[:], G1[:, b:b + 1], None, ALU.mult, None, r=[ky, ("G", b)], w=[ky])
                    p.stt("dve", y1[:], y2[:], G2[:, b:b + 1], y1[:], ALU.mult, ALU.add, r=[ky, ("G", b)], w=[ky])
                    blk = Xc[:, j, :]; kx = ("Xcm", j)
                    p.stt("dve", blk, blk, ALPHA, y1[:], ALU.mult, ALU.add, r=["Xcm", kx, ky], w=[kx])
                    layer_norm(blk, ln3, kx, (st6, mv, rstd), aff="dve")
                p.dma("sp", out[t], Xc[:], r=["Xcm"] + [("Xcm", j) for j in range(8)], w=[("out", t)])
    p.barrier()
```

```python
import math
import numpy as np
import ml_dtypes
from contextlib import ExitStack
import concourse.bass as bass
import concourse.mybir as mybir
from concourse.bass_utils import run_bass_kernel_spmd

F32 = mybir.dt.float32
BF16 = mybir.dt.bfloat16
I32 = mybir.dt.int32
AF = mybir.ActivationFunctionType
ALU = mybir.AluOpType
AX = mybir.AxisListType

import os
SAME_ENG_SYNC = os.environ.get("KSES", "1") == "1"
ROUTED = True
RING = 12
NCORES = 8
D = 1024
ALPHA = (2.0 * 2) ** 0.25
LN_EPS = 1e-5
RMS_EPS = 1e-6
PI = math.pi
TWO_PI = 2.0 * math.pi
CAP = 1408
NEXP = 8
FFN = 2816
EDIM = 3584


class Op:
    __slots__ = ("eng", "fn", "deps", "sig", "dma", "semi", "semv")


class Prog:
    ENGS = ("pe", "act", "dve", "pool", "sp")
    COMPUTE = ("pe", "act", "dve", "pool")

    def __init__(self, nc):
        self.nc = nc
        self.q = {e: [] for e in self.ENGS}
        self.lastw = {}
        self.rd = {}
        self.ndma = {e: 0 for e in self.ENGS}
        self.dmaops = {e: [] for e in self.ENGS}

    def op(self, eng, fn, r=(), w=(), dma=False, extra=()):
        o = Op()
        o.eng = eng
        o.fn = fn
        o.dma = dma
        o.sig = dma
        o.semi = None
        o.semv = 0
        deps = set(extra)
        lastw = self.lastw
        rd = self.rd
        for k in r:
            lw = lastw.get(k)
            if lw is not None:
                deps.add(lw)
        for k in w:
            lw = lastw.get(k)
            if lw is not None:
                deps.add(lw)
            x = rd.get(k)
            if x:
                deps.update(x)
        for k in r:
            l = rd.get(k)
            if l is None:
                rd[k] = [o]
            elif not dma:
                for i, x in enumerate(l):
                    if x.eng == eng and not x.dma:
                        l[i] = o
                        break
                else:
                    l.append(o)
            else:
                l.append(o)
        for k in w:
            lastw[k] = o
            rd[k] = []
        deps.discard(o)
        o.deps = deps
        if dma:
            o.semi = self.ndma[eng] % RING
            o.semv = 16 * (self.ndma[eng] // RING + 1)
            self.ndma[eng] += 1
            self.dmaops[eng].append(o)
        self.q[eng].append(o)
        return o

    def dma(self, eng, out, in_, r=(), w=(), **kw):
        return self.op(eng, lambda e: e.dma_start(out=out, in_=in_, **kw), r=r, w=w, dma=True)

    def barrier(self):
        lastc = []
        for e in self.COMPUTE:
            for o in reversed(self.q[e]):
                if not o.dma and o.fn is not None:
                    lastc.append(o)
                    break
        lastd = []
        for e in self.ENGS:
            lastd.extend(self.dmaops[e][-RING:])
        for e in self.ENGS:
            self.op(e, None, extra=lastc + lastd)

    def mm(self, out, lhsT, rhs, start, stop, r, w):
        return self.op("pe", lambda e: e.matmul(out, lhsT=lhsT, rhs=rhs, start=start, stop=stop), r=r, w=w)

    def tr(self, out, in_, ident, r, w):
        return self.op("pe", lambda e: e.transpose(out=out, in_=in_, identity=ident), r=r, w=w)

    def act(self, out, in_, func, r, w, eng="act", **kw):
        return self.op(eng, lambda e: e.activation(out=out, in_=in_, func=func, **kw), r=r, w=w)

    def tt(self, eng, out, in0, in1, op, r, w):
        return self.op(eng, lambda e: e.tensor_tensor(out=out, in0=in0, in1=in1, op=op), r=r, w=w)

    def ts(self, eng, out, in0, s1, s2, op0, op1, r, w):
        if op1 is None:
            return self.op(eng, lambda e: e.tensor_scalar(out=out, in0=in0, scalar1=s1, scalar2=None, op0=op0), r=r, w=w)
        return self.op(eng, lambda e: e.tensor_scalar(out=out, in0=in0, scalar1=s1, scalar2=s2, op0=op0, op1=op1), r=r, w=w)

    def stt(self, eng, out, in0, scalar, in1, op0, op1, r, w):
        return self.op(eng, lambda e: e.scalar_tensor_tensor(out=out, in0=in0, scalar=scalar, in1=in1, op0=op0, op1=op1), r=r, w=w)

    def cp(self, eng, out, in_, r, w):
        if eng == "act":
            return self.op(eng, lambda e: e.copy(out=out, in_=in_), r=r, w=w)
        return self.op(eng, lambda e: e.tensor_copy(out=out, in_=in_), r=r, w=w)

    def emit(self):
        nc = self.nc
        es = ExitStack()
        with es:
            csem = {e: es.enter_context(nc.semaphore("c_" + e)) for e in self.COMPUTE}
            dsem = {}
            for e in self.ENGS:
                if self.ndma[e]:
                    dsem[e] = [es.enter_context(nc.semaphore("d_%s_%d" % (e, i))) for i in range(min(RING, self.ndma[e]))]

            def skip_same(d, ename):
                return d.eng == ename and (ename == "pe" or ename == "sp" or not SAME_ENG_SYNC)

            for e in self.ENGS:
                for o in self.q[e]:
                    for d in o.deps:
                        if not d.dma and not skip_same(d, e):
                            d.sig = True
            for e in self.COMPUTE:
                c = 0
                for o in self.q[e]:
                    if o.sig and not o.dma:
                        c += 1
                        o.semv = c
            self.stats = {}

            def run(ename, eng):
                waited = {}
                nw = 0
                for o in self.q[ename]:
                    waits = {}
                    for d in o.deps:
                        if d.dma:
                            s = dsem[d.eng][d.semi]
                        else:
                            if skip_same(d, ename):
                                continue
                            s = csem[d.eng]
                        if waits.get(s, 0) < d.semv:
                            waits[s] = d.semv
                    if o.dma and o.semv > 16:
                        s = dsem[ename][o.semi]
                        if waits.get(s, 0) < o.semv - 16:
                            waits[s] = o.semv - 16
                    for s, v in waits.items():
                        if waited.get(s, 0) >= v:
                            continue
                        waited[s] = v
                        eng.wait_ge(s, v)
                        nw += 1
                    if o.fn is None:
                        continue
                    ins = o.fn(eng)
                    if o.dma:
                        ins.then_inc(dsem[ename][o.semi], 16)
                    elif o.sig:
                        ins.then_inc(csem[ename], 1)
                self.stats[ename] = (len(self.q[ename]), nw)

            with nc.Block() as block:
                @block.tensor
                def _(eng):
                    run("pe", eng)

                @block.scalar
                def _(eng):
                    run("act", eng)

                @block.vector
                def _(eng):
                    run("dve", eng)

                @block.gpsimd
                def _(eng):
                    run("pool", eng)

                @block.sync
                def _(eng):
                    run("sp", eng)


def dap(t, offset, ap):
    return bass.AP(t.tensor, offset, [list(x) for x in ap])


class _Cut(Exception):
    pass


def build_program(nphase=99, debug=False, cut=None, mini=False):
    nc = bass.Bass("TRN2", target_bir_lowering=False)
    p = Prog(nc)
    S5IN = ("s5_lam_re", "s5_lam_im", "s5_log_step", "s5_b_re", "s5_b_im", "s5_c_re", "s5_c_im", "s5_d")

    def din(name, shape, dt=F32):
        if mini and not (name in S5IN or name.startswith("c_")):
            shape = [1, 2]
        return nc.dram_tensor(name, list(shape), dt, kind="ExternalInput").ap()

    def cutpoint(n):
        if cut is not None and n >= cut:
            raise _Cut()

    dbg = debug if isinstance(debug, (set, list, tuple)) else (("xa", "xb", "xc", "xe", "ye") if debug else ())

    def dscr(name, shape, dt=F32):
        return nc.dram_tensor(name, list(shape), dt, kind=("ExternalOutput" if name in dbg else "Internal")).ap()

    x_in = din("x", [4, 128, 8, D])
    dbgo = nc.dram_tensor("dbgo", [128, 4096], F32, kind="ExternalOutput").ap() if cut is not None else None
    lam_re = din("s5_lam_re", [64, 64]); lam_im = din("s5_lam_im", [64, 64]); log_step = din("s5_log_step", [1, 64])
    b_re = din("s5_b_re", [64, 64, 16]); b_im = din("s5_b_im", [64, 64, 16])
    c_re = din("s5_c_re", [64, 16, 64]); c_im = din("s5_c_im", [64, 16, 64])
    s5_d = din("s5_d", [1, D]); w_glu = din("s5_w_glu", [D, 2 * D])
    q_w_a = din("mla_q_w_a", [D, 256]); q_norm = din("mla_q_norm", [256, 1]); q_w_b = din("mla_q_w_b", [256, 1536])
    o_w = din("mla_o_w", [D, D]); kv_w_a = din("kv_w_a", [D, 192]); kv_norm = din("kv_norm", [128, 1]); kv_w_b = din("kv_w_b", [128, 2048])
    ffn_w_in = din("ffn_w_in", [D, 2 * FFN]); ffn_w_out = din("ffn_w_out", [FFN, D])
    moe_router = din("moe_router", [D, NEXP]); moe_w_in = din("moe_w_in", [NEXP, D, 2 * EDIM] if nphase >= 5 else [1, 1, 2]); moe_w_out = din("moe_w_out", [NEXP, EDIM, D] if nphase >= 5 else [1, 1, 2])
    ln_g = din("ln_g", [4, D]); ln_b = din("ln_b", [4, D])
    c_ident = din("c_ident", [128, 128]); c_evec = din("c_evec", [128, 24]); c_cmask = din("c_cmask", [128, 256])
    c_iota = din("c_iota", [128, 2, 128]); c_triu = din("c_triu", [128, 128]); c_ecol = din("c_ecol", [128, NEXP])
    c_dmask = din("c_dmask", [128, 128]); c_pos = din("c_pos", [64, 2048]); c_invf = din("c_invf", [64, 2])

    out = nc.dram_tensor("out", [4, 128, 8, D], F32, kind="ExternalOutput").ap()
    xa = dscr("xa", [4, 128, 8, D]); xb = dscr("xb", [4, 128, 8, D]); xc = dscr("xc", [4, 128, 8, D])
    XE = dscr("xe", [NEXP * CAP + 128, D], BF16)
    YE = dscr("ye", [NEXP * CAP + 128, D])

    top = ExitStack()
    with top:
        ar = {"peak": 0, "n": 0}

        def sbt(es, name, shape, dt=F32):
            ar["n"] += 1
            return es.enter_context(nc.sbuf_tensor("%s_%d" % (name, ar["n"]), list(shape), dt))

        PS = [top.enter_context(nc.psum_tensor("ps%d" % i, [128, 1024], F32)) for i in range(4)]

        def bank(i):
            return PS[i // 2][:, (i % 2) * 512:(i % 2) * 512 + 512]

        def bankb(i):
            return PS[i // 2][:, (i % 2) * 512:(i % 2) * 512 + 512].bitcast(BF16)

        identf = sbt(top, "identf", [128, 128]); identb = sbt(top, "identb", [128, 128], BF16)
        negpi = sbt(top, "negpi", [128, 1]); epsln = sbt(top, "epsln", [128, 1])
        p.dma("sp", identf[:], c_ident, w=["identf"])

        def load_ln(es, lnidx):
            g_ = sbt(es, "lng%d" % lnidx, [128, D]); b_ = sbt(es, "lnb%d" % lnidx, [128, D])
            p.dma("sp", g_[:], dap(ln_g, lnidx * D, [[0, 128], [1, D]]), w=[("lng", lnidx)])
            p.dma("sp", b_[:], dap(ln_b, lnidx * D, [[0, 128], [1, D]]), w=[("lnb", lnidx)])
            return (g_, b_, lnidx)
        p.cp("pool", identb[:], identf[:], r=["identf"], w=["identb"])
        p.op("pool", lambda e: e.memset(negpi[:], -PI), w=["negpi"])
        p.op("pool", lambda e: e.memset(epsln[:], LN_EPS), w=["epsln"])
        halfpi = sbt(top, "halfpi", [128, 1])
        p.op("pool", lambda e: e.memset(halfpi[:], PI / 2), w=["halfpi"])
        MAGIC = 12582912.0

        def sincos(eng, sin_out, cos_out, y, tmp, rk, wk, npart=128):
            p.ts(eng, tmp, y, MAGIC, MAGIC, ALU.add, ALU.subtract, r=rk, w=wk)
            p.tt(eng, tmp, y, tmp, ALU.subtract, r=rk + wk, w=wk)
            p.act(sin_out, tmp, AF.Sin, r=wk, w=wk, scale=TWO_PI)
            p.stt(eng, tmp, tmp, -1.0, tmp, ALU.mult, ALU.max, r=wk, w=wk)
            p.act(cos_out, tmp, AF.Sin, r=wk + ["halfpi"], w=wk, scale=-TWO_PI, bias=halfpi[0:npart, 0:1])


        def layer_norm(blk, lnp, keyblk, stat, aff="pool"):
            st6, mv, rstd = stat
            lng_, lnb_, lnidx = lnp
            for h in range(2):
                p.op("dve", (lambda hh: lambda e: e.bn_stats(out=st6[:, hh, :], in_=blk[:, hh * 512:(hh + 1) * 512]))(h), r=[keyblk], w=["st6"])
            p.op("dve", lambda e: e.bn_aggr(out=mv[:], in_=st6[:].rearrange("p a b -> p (a b)")), r=["st6"], w=["mv"])
            p.act(rstd[:], mv[:, 1:2], AF.Sqrt, r=["mv", "epsln"], w=["rstd"], bias=epsln[:, 0:1])
            p.op("dve", lambda e: e.reciprocal(out=rstd[:], in_=rstd[:]), r=["rstd"], w=["rstd"])
            p.ts("dve", blk, blk, mv[:, 0:1], rstd[:, 0:1], ALU.subtract, ALU.mult, r=[keyblk, "mv", "rstd"], w=[keyblk])
            p.tt(aff, blk, blk, lng_[:], ALU.mult, r=[keyblk, ("lng", lnidx)], w=[keyblk])
            p.tt(aff, blk, blk, lnb_[:], ALU.add, r=[keyblk, ("lnb", lnidx)], w=[keyblk])

        try:
            s5 = ExitStack()
            with s5:
                CtrlRe = sbt(s5, "CtrlRe", [128, 32, 128], BF16); CtrlIm = sbt(s5, "CtrlIm", [128, 32, 128], BF16)
                Toep = sbt(s5, "Toep", [128, 64, 128], BF16)
                ObRe = sbt(s5, "ObRe", [128, 64, 128], BF16); ObIm = sbt(s5, "ObIm", [128, 64, 128], BF16)
                rho = sbt(s5, "rho", [128, 32]); phi = sbt(s5, "phi", [128, 32])
                p0 = ExitStack()
                with p0:
                    lr = sbt(p0, "lr", [128, 32]); li = sbt(p0, "li", [128, 32]); ls = sbt(p0, "ls", [128, 32])
                    Bre = sbt(p0, "Bre", [128, 32, 16]); Bim = sbt(p0, "Bim", [128, 32, 16])
                    Cre = sbt(p0, "Cre", [128, 32, 16]); Cim = sbt(p0, "Cim", [128, 32, 16])
                    evec = sbt(p0, "evec", [128, 24]); cmask = sbt(p0, "cmask", [128, 256])
                    dt = sbt(p0, "dt", [128, 32]); lrdt = sbt(p0, "lrdt", [128, 32]); lidt = sbt(p0, "lidt", [128, 32])
                    PWre = sbt(p0, "PWre", [128, 32, 24]); PWim = sbt(p0, "PWim", [128, 32, 24])
                    sm = [sbt(p0, "sm%d" % i, [128, 32]) for i in range(6)]
                    fre = sbt(p0, "fre", [128, 32]); fim = sbt(p0, "fim", [128, 32])
                    Bbre = sbt(p0, "Bbre", [128, 32, 16]); Bbim = sbt(p0, "Bbim", [128, 32, 16]); tb = sbt(p0, "tb", [128, 32, 16])
                    XBre = sbt(p0, "XBre", [128, 32, 8, 16]); XBim = sbt(p0, "XBim", [128, 32, 8, 16])
                    Zre = sbt(p0, "Zre", [128, 32, 8, 16]); Zim = sbt(p0, "Zim", [128, 32, 8, 16])
                    p0b = ExitStack()
                    A = sbt(p0b, "A", [128, 32, 24]); ANG = sbt(p0b, "ANG", [128, 32, 24]); XS = sbt(p0b, "XS", [128, 32, 24])
                    T1 = sbt(p0b, "T1", [128, 32, 8, 16]); T2 = sbt(p0b, "T2", [128, 32, 8, 16])
                    for a in range(2):
                        sl = slice(a * 64, (a + 1) * 64)
                        p.dma("sp", lr[sl, :], dap(lam_re, a * 64, [[1, 64], [128, 32]]), w=["lr"])
                        p.dma("sp", li[sl, :], dap(lam_im, a * 64, [[1, 64], [128, 32]]), w=["li"])
                        p.dma("sp", ls[sl, :], dap(log_step, a, [[0, 64], [2, 32]]), w=["ls"])
                        p.dma("sp", Bre[sl], dap(b_re, a * 1024, [[16, 64], [2048, 32], [1, 16]]), w=["Bre"])
                        p.dma("sp", Bim[sl], dap(b_im, a * 1024, [[16, 64], [2048, 32], [1, 16]]), w=["Bim"])
                        for c_ in range(16):
                            p.dma("sp", Cre[sl, :, c_], dap(c_re, a * 1024 + c_ * 64, [[1, 64], [2048, 32]]), w=["Cre"])
                            p.dma("sp", Cim[sl, :, c_], dap(c_im, a * 1024 + c_ * 64, [[1, 64], [2048, 32]]), w=["Cim"])
                    p.dma("sp", evec[:], c_evec, w=["evec"])
                    p.dma("sp", cmask[:], c_cmask, w=["cmask"])
                    cutpoint(1)
                    V, M, Sb, AD = "dve", ALU.mult, ALU.subtract, ALU.add
                    p.act(dt[:], ls[:], AF.Exp, r=["ls"], w=["dt"])
                    p.tt(V, lrdt[:], lr[:], dt[:], M, r=["lr", "dt"], w=["lrdt"])
                    p.tt(V, lidt[:], li[:], dt[:], M, r=["li", "dt"], w=["lidt"])
                    b3 = lambda t2: t2[:].unsqueeze(2).to_broadcast([128, 32, 24])
                    ev3 = evec[:].unsqueeze(1).to_broadcast([128, 32, 24])
                    p.tt(V, A[:], b3(lrdt), ev3, M, r=["lrdt", "evec"], w=["A"])
                    p.act(A[:], A[:], AF.Exp, r=["A"], w=["A"])
                    p.ts(V, sm[0][:], lidt[:], 1.0 / TWO_PI, None, M, None, r=["lidt"], w=["sm0"])
                    p.tt(V, ANG[:], b3(sm[0]), ev3, M, r=["sm0", "evec"], w=["ANG"])
                    sincos(V, PWim[:], PWre[:], ANG[:], XS[:], ["ANG"], ["XS", "PWre", "PWim"])
                    p.tt(V, PWim[:], PWim[:], A[:], M, r=["A", "PWim", "XS"], w=["PWim"])
                    p.tt(V, PWre[:], PWre[:], A[:], M, r=["A", "PWre", "XS"], w=["PWre"])
                    if cut == 2:
                        p.dma("sp", dbgo[:, 0:768], PWre[:].rearrange("p a b -> p (a b)"), r=["PWre"], w=["dbgo"])
                        p.dma("sp", dbgo[:, 768:1536], PWim[:].rearrange("p a b -> p (a b)"), r=["PWim"], w=["dbgo"])
                    cutpoint(2)
                    lbre = PWre[:, :, 16]; lbim = PWim[:, :, 16]
                    p.tt(V, sm[0][:], lr[:], lr[:], M, r=["lr"], w=["sm0"])
                    p.tt(V, sm[1][:], li[:], li[:], M, r=["li"], w=["sm1"])
                    p.tt(V, sm[0][:], sm[0][:], sm[1][:], AD, r=["sm0", "sm1"], w=["sm0"])
                    p.op(V, lambda e: e.reciprocal(out=sm[0][:], in_=sm[0][:]), r=["sm0"], w=["sm0"])
                    p.ts(V, sm[1][:], lbre, -1.0, None, AD, None, r=["PWre", "sm0"], w=["sm1"])
                    p.tt(V, sm[2][:], sm[1][:], lr[:], M, r=["sm1", "lr"], w=["sm2"])
                    p.tt(V, sm[3][:], lbim, li[:], M, r=["PWim", "li"], w=["sm3"])
                    p.tt(V, sm[2][:], sm[2][:], sm[3][:], AD, r=["sm2", "sm3"], w=["sm2"])
                    p.tt(V, fre[:], sm[2][:], sm[0][:], M, r=["sm2", "sm0"], w=["fre"])
                    p.tt(V, sm[4][:], lbim, lr[:], M, r=["PWim", "lr"], w=["sm4"])
                    p.tt(V, sm[5][:], sm[1][:], li[:], M, r=["sm1", "li"], w=["sm5"])
                    p.tt(V, sm[4][:], sm[4][:], sm[5][:], Sb, r=["sm4", "sm5"], w=["sm4"])
                    p.tt(V, fim[:], sm[4][:], sm[0][:], M, r=["sm4", "sm0"], w=["fim"])
                    f3 = lambda t2: t2[:].unsqueeze(2).to_broadcast([128, 32, 16])
                    p.tt(V, Bbre[:], Bre[:], f3(fre), M, r=["Bre", "fre"], w=["Bbre"])
                    p.tt(V, tb[:], Bim[:], f3(fim), M, r=["Bim", "fim"], w=["tb"])
                    p.tt(V, Bbre[:], Bbre[:], tb[:], Sb, r=["Bbre", "tb"], w=["Bbre"])
                    p.tt(V, Bbim[:], Bim[:], f3(fre), M, r=["Bim", "fre"], w=["Bbim"])
                    p.tt(V, tb[:], Bre[:], f3(fim), M, r=["Bre", "fim", "Bbre"], w=["tb"])
                    p.tt(V, Bbim[:], Bbim[:], tb[:], AD, r=["Bbim", "tb"], w=["Bbim"])
                    pw4 = lambda t3, lo: t3[:, :, lo:lo + 8].unsqueeze(3).to_broadcast([128, 32, 8, 16])
                    v4 = lambda t3: t3[:].unsqueeze(2).to_broadcast([128, 32, 8, 16])

                    def cmul(outre, outim, pre, pim, lo, vre, vim, kre, kim, negim, obf=None):
                        p.tt(V, T1[:], pw4(pre, lo), v4(vre), M, r=["PWre", kre], w=["T1"])
                        p.tt(V, T2[:], pw4(pim, lo), v4(vim), M, r=["PWim", kim], w=["T2"])
                        p.tt(V, outre[0], T1[:], T2[:], Sb, r=["T1", "T2"], w=[outre[1]])
                        p.tt(V, T1[:], pw4(pre, lo), v4(vim), M, r=["PWre", kim, outre[1]], w=["T1"])
                        p.tt(V, T2[:], pw4(pim, lo), v4(vre), M, r=["PWim", kre, outre[1]], w=["T2"])
                        if negim:
                            p.stt(V, outim[0], T1[:], -1.0, T2[:], M, Sb, r=["T1", "T2"], w=[outim[1]])
                        else:
                            p.tt(V, outim[0], T1[:], T2[:], AD, r=["T1", "T2"], w=[outim[1]])

                    cmul((XBre[:], "XBre"), (XBim[:], "XBim"), PWre, PWim, 0, Bbre, Bbim, "Bbre", "Bbim", False)
                    cmul((Zre[:], "Zre"), (Zim[:], "Zim"), PWre, PWim, 8, Cre, Cim, "Cre", "Cim", True)
                    p.op("pool", lambda e: e.memset(ObRe[:], 0.0), w=["ObRe"])
                    p.op("pool", lambda e: e.memset(ObIm[:], 0.0), w=["ObIm"])
                    pw4 = lambda t3, lo: t3[:, :, lo:lo + 8].unsqueeze(3).to_broadcast([128, 32, 8, 16])
                    p.tt(V, T1[:], pw4(PWre, 16), v4(Cre), M, r=["PWre", "Cre"], w=["T1"])
                    p.tt(V, T2[:], pw4(PWim, 16), v4(Cim), M, r=["PWim", "Cim"], w=["T2"])
                    for a in range(2):
                        sl = slice(a * 64, (a + 1) * 64)
                        ov = ObRe[sl, :, :].rearrange("p (k a) (j c) -> p k a j c", a=2, c=16)[:, :, a, :, :]
                        p.tt(V, ov, T1[sl], T2[sl], Sb, r=["T1", "T2"], w=["ObRe"])
                    p.tt(V, T1[:], pw4(PWre, 16), v4(Cim), M, r=["PWre", "Cim", "ObRe"], w=["T1"])
                    p.tt(V, T2[:], pw4(PWim, 16), v4(Cre), M, r=["PWim", "Cre", "ObRe"], w=["T2"])
                    for a in range(2):
                        sl = slice(a * 64, (a + 1) * 64)
                        ov = ObIm[sl, :, :].rearrange("p (k a) (j c) -> p k a j c", a=2, c=16)[:, :, a, :, :]
                        p.stt(V, ov, T1[sl], -1.0, T2[sl], M, Sb, r=["T1", "T2"], w=["ObIm"])
                    p.act(rho[:], lrdt[:], AF.Exp, r=["lrdt"], w=["rho"], scale=8.0)
                    p.ts(V, phi[:], lidt[:], 8.0 / TWO_PI, None, M, None, r=["lidt"], w=["phi"])
                    if cut == 3:
                        p.dma("sp", dbgo[:, 0:4096], XBre[:].rearrange("p a b c -> p (a b c)"), r=["XBre"], w=["dbgo"])
                    cutpoint(3)
                    p0b.close()
                    HL = {}
                    for nm, src in (("XBre", XBre), ("XBim", XBim)):
                        hi = sbt(p0, nm + "_h", [128, 32, 128], BF16)
                        p.cp("pool", hi[:], src[:].rearrange("p k i c -> p k (i c)"), r=[nm], w=["HL", "T1", "T2", "A", "ANG", "XS"])
                        HL[nm] = hi
                    for nm, src in (("Zre", Zre), ("Zim", Zim)):
                        hi = sbt(p0, nm + "_bd", [128, 32, 2, 128], BF16)
                        p.op("pool", (lambda h_: lambda e: e.memset(h_[:], 0.0))(hi), w=["HL", "T1", "T2", "A", "ANG", "XS"])
                        for a in range(2):
                            sl = slice(a * 64, (a + 1) * 64)
                            p.cp("pool", hi[sl, :, a, :], src[sl].rearrange("p k i c -> p k (i c)"), r=[nm], w=["HL"])
                        HL[nm] = hi
                    for k in range(32):
                        bk = 2 * (k % 2)
                        xr = XBre[:, k, :, :].rearrange("p i c -> p (i c)"); xi = XBim[:, k, :, :].rearrange("p i c -> p (i c)")
                        kb0 = ("bank", bk); kb1 = ("bank", bk + 1)
                        import os
                        PEVAR = int(os.environ.get("PEVAR", "0"))
                        if PEVAR in (0, 1):
                            p.tr(bank(bk)[:, 0:128], xr, identf[:], r=["XBre", "identf"], w=[kb0])
                            p.tr(bank(bk)[:, 128:256], xi, identf[:], r=["XBim", "identf"], w=[kb0])
                            p.cp("act", CtrlRe[:, k, :], bank(bk)[:, 0:128], r=[kb0], w=[("Ctrl", k)])
                            p.cp("act", CtrlIm[:, k, :], bank(bk)[:, 128:256], r=[kb0], w=[("Ctrl", k)])
                        o_ = bank(bk + 1)[:, 0:256]
                        p.mm(o_, HL["XBre"][:, k, :], HL["Zre"][:, k, :, :].rearrange("p a m -> p (a m)"), True, False, r=["HL"], w=[kb1])
                        p.mm(o_, HL["XBim"][:, k, :], HL["Zim"][:, k, :, :].rearrange("p a m -> p (a m)"), False, True, r=["HL"], w=[kb1])
                        p.tt(V, Toep[:, 2 * k:2 * k + 2, :].rearrange("p g m -> p (g m)"), bank(bk + 1)[:, 0:256], cmask[:], M, r=[kb1, "cmask"], w=[("Toep", k)])
                    if cut == 4:
                        p.dma("sp", dbgo[:, 0:2048], Toep[:, 0:32, :].rearrange("p a b -> p (a b)").bitcast(F32), r=[("Toep", k) for k in range(32)], w=["dbgo"])
                    cutpoint(4)
                p.barrier()
                if nphase < 1:
                    pass
                p1 = ExitStack()
                with p1:
                    Wglu = sbt(p1, "Wglu", [128, 8, 2 * D], BF16)
                    Xc = sbt(p1, "Xc", [128, 8, D])
                    bfA = sbt(p1, "bfA", [128, 8192], BF16)
                    bfB = sbt(p1, "bfB", [128, 8192], BF16)
                    Ere = sbt(p1, "Ere", [128, 32, 129], BF16); Eim = sbt(p1, "Eim", [128, 32, 129], BF16)
                    Vc = sbt(p1, "Vc", [128, 32, 2])
                    iota = sbt(p1, "iota", [128, 2, 128]); Dt = sbt(p1, "Dt", [128, D])
                    sg = sbt(p1, "sg", [128, D])
                    st6 = sbt(p1, "st6", [128, 2, 6]); mv = sbt(p1, "mv", [128, 2]); rstd = sbt(p1, "rstd", [128, 1])
                    NB = 2
                    tabs = [[sbt(p1, "tab%d_%d" % (i, j), [128, 128]) for j in range(8)] for i in range(NB)]
                    du = [sbt(p1, "du%d" % i, [128, 4, 8, 16]) for i in range(2)]
                    ln0 = load_ln(p1, 0)
                    p.dma("sp", iota[:], c_iota, w=["iota"])
                    p.dma("sp", Dt[:], dap(s5_d, 0, [[0, 128], [1, D]]), w=["Dt"])
                    for kc in range(8):
                        p.dma("pool", Wglu[:, kc, :], w_glu[kc * 128:(kc + 1) * 128, :], w=[("Wglu", kc)])
                    Xg = bfA[:].rearrange("p (g i c) -> p g i c", g=64, i=8)
                    Ycb = bfA[:].rearrange("p (j f) -> p j f", j=8)
                    Ub = bfB[:].rearrange("p (g n) -> p g n", g=64)
                    actT = bfB[:].rearrange("p (fc j n) -> p fc j n", fc=8, j=8)
                    for t in range(4 if nphase >= 1 else 0):
                        tis = t % 2
                        p.dma("sp", Xc[:], x_in[t], w=["Xc"] + [("Xc", j) for j in range(8)])
                        p.cp("pool", Xg, Xc[:].rearrange("p i (g c) -> p g i c", c=16), r=["Xc"], w=["bfA"] + [("Ycb", gq) for gq in range(16)])
                        if tis == 0:
                            p.op("pool", lambda e: e.memset(Ere[:, :, 0:1], 0.0), w=["Ecar"])
                            p.op("pool", lambda e: e.memset(Eim[:, :, 0:1], 0.0), w=["Ecar"])
                            p.op("pool", lambda e: e.memset(Vc[:], 0.0), w=[("Vc", k) for k in range(32)])
                        else:
                            p.cp("pool", Ere[:, :, 0:1], Ere[:, :, 128:129], r=[("E", k) for k in range(32)], w=["Ecar"])
                            p.cp("pool", Eim[:, :, 0:1], Eim[:, :, 128:129], r=[("E", k) for k in range(32)], w=["Ecar"])
                        for gq in range(8):
                            bk = gq % 2
                            kb = ("bank", bk)
                            for gg in range(8):
                                g = gq * 8 + gg
                                p.tr(bankb(bk)[:, gg * 128:(gg + 1) * 128], Xg[:, g, :, :].rearrange("p i c -> p (i c)"), identb[:], r=["bfA", "identb"], w=[kb])
                            p.cp("act", Ub[:, gq * 8:(gq + 1) * 8, :].rearrange("p g n -> p (g n)"), bankb(bk), r=[kb], w=[("Ub", gq)] + [("actT", j) for j in range(8)])
                        for k in range(32):
                            bk = 2 + (k % 3)
                            kb = ("bank", bk)
                            T = tabs[k % NB]
                            tk = ("tab", k % NB)
                            for a in range(2):
                                g = 2 * k + a
                                p.mm(bank(bk)[:, a * 128:(a + 1) * 128], CtrlRe[:, k, :], Ub[:, g, :], True, True, r=[("Ctrl", k), ("Ub", g // 8)], w=[kb])
                                p.mm(bank(bk)[:, 256 + a * 128:256 + (a + 1) * 128], CtrlIm[:, k, :], Ub[:, g, :], True, True, r=[("Ctrl", k), ("Ub", g // 8)], w=[kb])
                            h0 = slice(0, 64); h1 = slice(64, 128)
                            p.ts("dve", T[2][:], iota[:, tis, :], phi[:, k:k + 1], None, ALU.mult, None, r=["iota", "phi"], w=[tk])
                            sincos("dve", T[0][:], T[1][:], T[2][:], T[3][:], [tk], [tk])
                            sn, cn = T[0], T[1]
                            for hs, ro in ((h0, 0), (h1, 128)):
                                Wr = bank(bk)[hs, ro:ro + 128]; Wi = bank(bk)[hs, 256 + ro:256 + ro + 128]
                                p.tt("dve", T[2][hs], cn[hs], Wr, ALU.mult, r=[tk, kb], w=[tk])
                                p.tt("dve", T[3][hs], sn[hs], Wi, ALU.mult, r=[tk, kb], w=[tk])
                                p.tt("dve", T[4][hs], cn[hs], Wi, ALU.mult, r=[tk, kb], w=[tk])
                                p.tt("dve", T[5][hs], sn[hs], Wr, ALU.mult, r=[tk, kb], w=[tk])
                            p.tt("dve", T[2][:], T[2][:], T[3][:], ALU.add, r=[tk], w=[tk])
                            p.tt("dve", T[4][:], T[4][:], T[5][:], ALU.subtract, r=[tk], w=[tk])
                            rb = rho[:, k:k + 1].to_broadcast([128, 128])
                            p.op("dve", (lambda o_, d1, ini: lambda e: e.tensor_tensor_scan(out=o_, data0=rb, data1=d1, initial=ini, op0=ALU.mult, op1=ALU.add))(T[6][:], T[2][:], Vc[:, k, 0:1]), r=[tk, "rho", ("Vc", k)], w=[tk])
                            p.op("dve", (lambda o_, d1, ini: lambda e: e.tensor_tensor_scan(out=o_, data0=rb, data1=d1, initial=ini, op0=ALU.mult, op1=ALU.add))(T[7][:], T[4][:], Vc[:, k, 1:2]), r=[tk, "rho", ("Vc", k)], w=[tk])
                            p.cp("dve", Vc[:, k, 0:1], T[6][:, 127:128], r=[tk], w=[("Vc", k)])
                            p.cp("dve", Vc[:, k, 1:2], T[7][:, 127:128], r=[tk], w=[("Vc", k)])
                            p.tt("pool", T[3][:], cn[:], T[6][:], ALU.mult, r=[tk], w=[tk])
                            p.tt("pool", T[5][:], sn[:], T[7][:], ALU.mult, r=[tk], w=[tk])
                            p.tt("pool", Ere[:, k, 1:129], T[3][:], T[5][:], ALU.subtract, r=[tk], w=[("E", k)])
                            p.tt("pool", T[3][:], sn[:], T[6][:], ALU.mult, r=[tk], w=[tk])
                            p.tt("pool", T[5][:], cn[:], T[7][:], ALU.mult, r=[tk], w=[tk])
                            p.tt("pool", Eim[:, k, 1:129], T[3][:], T[5][:], ALU.add, r=[tk], w=[("E", k)])
                        for gq in range(16):
                            bk = 5 + (gq % 2)
                            kb = ("bank", bk)
                            for gg in range(4):
                                g = gq * 4 + gg
                                k = g // 2; a = g % 2
                                sl = slice(a * 64, (a + 1) * 64)
                                o_ = bank(bk)[:, gg * 128:(gg + 1) * 128]
                                p.mm(o_, Ub[:, g, :], Toep[:, g, :], True, False, r=[("Ub", g // 8), ("Toep", k)], w=[kb])
                                p.mm(o_, Ere[:, k, 0:128], ObRe[:, g, :], False, False, r=[("E", k), "Ecar", "ObRe"], w=[kb])
                                p.mm(o_, Eim[:, k, 0:128], ObIm[:, g, :], False, True, r=[("E", k), "Ecar", "ObIm"], w=[kb])
                            dd = du[gq % 2]
                            kd = ("du", gq % 2)
                            xv = Xc[:, :, gq * 64:(gq + 1) * 64].rearrange("p i (g c) -> p g i c", c=16)
                            dv = Dt[:, gq * 64:(gq + 1) * 64].rearrange("p (g c) -> p g c", c=16).unsqueeze(2).to_broadcast([128, 4, 8, 16])
                            p.tt("pool", dd[:], xv, dv, ALU.mult, r=["Xc", "Dt"], w=[kd])
                            p.tt("dve", dd[:], dd[:], bank(bk).rearrange("p (g j c) -> p g j c", g=4, j=8), ALU.add, r=[kd, kb], w=[kd])
                            yv = Ycb[:, :, gq * 64:(gq + 1) * 64].rearrange("p j (g c) -> p g j c", c=16)
                            p.act(yv, dd[:], AF.Gelu_apprx_tanh, r=[kd, ("Ub", 0), ("Ub", 7)], w=[("Ycb", gq)])
                        ykeys = [("Ycb", gq) for gq in range(16)]
                        for j in range(8):
                            bk = j % 2
                            kb = ("bank", bk)
                            for fc in range(8):
                                p.tr(bankb(bk)[:, fc * 128:(fc + 1) * 128], Ycb[:, j, fc * 128:(fc + 1) * 128], identb[:], r=ykeys + ["identb"], w=[kb])
                            p.cp("act", actT[:, :, j, :], bankb(bk).rearrange("p (fc n) -> p fc n", fc=8), r=[kb], w=[("Ub", gq2) for gq2 in range(8)] + [("actT", j)])
                        for j in range(8):
                            if j % 2 == 0:
                                pv, pg, kv, kg = PS[1], PS[2], [("bank", 2), ("bank", 3)], [("bank", 4), ("bank", 5)]
                            else:
                                pv, pg, kv, kg = PS[3], PS[0], [("bank", 6), ("bank", 7)], [("bank", 0), ("bank", 1)]
                            for half in range(4):
                                tgt = (pv if half < 2 else pg)[:, (half % 2) * 512:(half % 2) * 512 + 512]
                                kk = (kv if half < 2 else kg)[half % 2]
                                for fc in range(8):
                                    p.mm(tgt, actT[:, fc, j, :], Wglu[:, fc, half * 512:(half + 1) * 512], fc == 0, fc == 7,
                                         r=[("actT", j), ("Wglu", fc)], w=[kk])
                            p.act(sg[:], pg[:], AF.Sigmoid, r=kg, w=["sg"])
                            p.tt("dve", sg[:], sg[:], pv[:], ALU.mult, r=["sg"] + kv, w=["sg"])
                            blk = Xc[:, j, :]
                            kx = ("Xc", j)
                            p.stt("dve", blk, blk, ALPHA, sg[:], ALU.mult, ALU.add, r=["Xc", kx, "sg"] + [("du", 0), ("du", 1)], w=[kx])
                            layer_norm(blk, ln0, kx, (st6, mv, rstd))
                        p.dma("sp", xa[t], Xc[:], r=["Xc"] + [("Xc", j) for j in range(8)], w=[("xa", t)])
                p.barrier()
            if nphase >= 2:
                phase2(nc, p, top, sbt, PS, bank, bankb, identf, layer_norm, load_ln, xa, xb, ffn_w_in, ffn_w_out)
        except _Cut:
            pass
        if nphase >= 3:
            phase_attn(nc, p, sbt, PS, bank, bankb, identf, identb, layer_norm, load_ln, sincos, xb, xc,
                       (q_w_a, q_norm, q_w_b, o_w, kv_w_a, kv_norm, kv_w_b), (c_pos, c_invf, c_dmask))
        if nphase >= 5:
            if ROUTED:
                phase_moe_routed(nc, p, sbt, PS, bank, bankb, identf, identb, layer_norm, load_ln, xc, out, XE, YE, moe_router, moe_w_in, moe_w_out, (c_triu, c_ecol))
            else:
                phase_moe(nc, p, sbt, PS, bank, identf, layer_norm, load_ln, xc, out, moe_router, moe_w_in, moe_w_out)
        final = [o for o in p.dmaops["sp"][-RING:]] + [o for o in p.dmaops["pool"][-RING:]]
        p.op("sp", None, extra=final)
        with nc.allow_non_contiguous_dma(reason="tiny strided parameter loads"):
            p.emit()
        p.peak = ar["peak"]
    return nc, p


def phase2(nc, p, top, sbt, PS, bank, bankb, identf, layer_norm, load_ln, xa, xb, ffn_w_in, ffn_w_out):
    es = ExitStack()
    with es:
        Win = sbt(es, "Win", [128, 8, 2 * FFN], BF16)
        Wout = sbt(es, "Wout", [128, 22, D], BF16)
        Xh = sbt(es, "Xh", [128, 4, D])
        xT = sbt(es, "xT", [128, 8, 4, 128], BF16)
        hT = sbt(es, "hT", [128, 22, 512], BF16)
        slt = [sbt(es, "slt%d" % i, [128, 512], BF16) for i in range(2)]
        st6 = sbt(es, "st6b", [128, 2, 6]); mv = sbt(es, "mvb", [128, 2]); rstd = sbt(es, "rstdb", [128, 1])
        ln1 = load_ln(es, 1)
        for kc in range(8):
            p.dma("pool", Win[:, kc, :], ffn_w_in[kc * 128:(kc + 1) * 128, :], w=[("Win", kc)])
        for fc in range(22):
            p.dma("pool", Wout[:, fc, :], ffn_w_out[fc * 128:(fc + 1) * 128, :], w=[("Wout", fc)])
        for t in range(4):
            for jh in range(2):
                p.dma("sp", Xh[:], xa[t][:, jh * 4:(jh + 1) * 4, :], r=[("xa", t)], w=["Xh"] + [("Xh", j) for j in range(4)])
                for j in range(4):
                    for dq in range(2):
                        bk = (j * 2 + dq) % 2
                        kb = ("bank", bk)
                        for dd in range(4):
                            dc = dq * 4 + dd
                            p.tr(bank(bk)[:, dd * 128:(dd + 1) * 128], Xh[:, j, dc * 128:(dc + 1) * 128], identf[:], r=["Xh", "identf"], w=[kb])
                        p.cp("act", xT[:, dq * 4:(dq + 1) * 4, j, :], bank(bk).rearrange("p (d n) -> p d n", d=4), r=[kb], w=[("xT", j)])
                xkeys = [("xT", j) for j in range(4)]
                for fc in range(22):
                    bg = 2 + 2 * (fc % 3); bu = bg + 1
                    kg = ("bank", bg); ku = ("bank", bu)
                    for dc in range(8):
                        p.mm(bank(bg), Win[:, dc, fc * 128:(fc + 1) * 128], xT[:, dc, :, :].rearrange("p j n -> p (j n)"), dc == 0, dc == 7, r=xkeys + [("Win", dc)], w=[kg])
                    for dc in range(8):
                        p.mm(bank(bu), Win[:, dc, FFN + fc * 128:FFN + (fc + 1) * 128], xT[:, dc, :, :].rearrange("p j n -> p (j n)"), dc == 0, dc == 7, r=xkeys + [("Win", dc)], w=[ku])
                    s_ = slt[fc % 2]
                    ks = ("slt", fc % 2)
                    p.act(s_[:], bank(bg), AF.Silu, r=[kg], w=[ks])
                    p.tt("dve", hT[:, fc, :], s_[:], bank(bu), ALU.mult, r=[ks, ku], w=[("hT", fc)])
                for j in range(4):
                    po = PS[0]
                    ko = [("bank", 0), ("bank", 1)]
                    for nh in range(2):
                        for fc in range(22):
                            p.mm(po[:, nh * 512:(nh + 1) * 512], hT[:, fc, j * 128:(j + 1) * 128], Wout[:, fc, nh * 512:(nh + 1) * 512], fc == 0, fc == 21,
                                 r=[("hT", fc), ("Wout", fc)], w=[ko[nh]])
                    blk = Xh[:, j, :]
                    kx = ("Xh", j)
                    p.stt("dve", blk, blk, ALPHA, po[:], ALU.mult, ALU.add, r=["Xh", kx] + ko, w=[kx])
                    layer_norm(blk, ln1, kx, (st6, mv, rstd))
                p.dma("sp", xb[t][:, jh * 4:(jh + 1) * 4, :], Xh[:], r=["Xh"] + [("Xh", j) for j in range(4)], w=[("xb", t)])
    p.barrier()


def make_consts():
    c = {}
    c["c_ident"] = np.eye(128, dtype=np.float32)
    ev = np.array([7, 6, 5, 4, 3, 2, 1, 0, -7, -6, -5, -4, -3, -2, -1, 0, 1, 2, 3, 4, 5, 6, 7, 8], np.float32)
    c["c_evec"] = np.tile(ev[None, :], (128, 1))
    i_idx = np.arange(128) // 16
    cm = (i_idx[None, :] >= i_idx[:, None]).astype(np.float32)
    c["c_cmask"] = np.concatenate([cm, cm], axis=1)
    io = np.arange(256, dtype=np.float32).reshape(2, 128)
    c["c_iota"] = np.tile(io[None], (128, 1, 1))
    c["c_triu"] = (np.arange(128)[:, None] < np.arange(128)[None, :]).astype(np.float32)
    c["c_ecol"] = np.tile((np.arange(NEXP, dtype=np.float32) * CAP)[None, :], (128, 1))
    c["c_dmask"] = (np.arange(128)[:, None] <= np.arange(128)[None, :]).astype(np.float32)
    c["c_pos"] = np.tile(np.arange(2048, dtype=np.float32)[None, :], (64, 1))
    invf = ((10000.0 ** (-np.arange(0, 64, 2, dtype=np.float64) / 64)) / (2 * np.pi)).astype(np.float32)
    sgn = np.concatenate([-np.ones(32, np.float32), np.ones(32, np.float32)])
    c["c_invf"] = np.stack([np.concatenate([invf, invf]), sgn], axis=1).astype(np.float32)
    return c


_CACHE = {}


def kernel(**inputs):
    x = np.ascontiguousarray(inputs["x"], dtype=np.float32)
    if "nc" not in _CACHE:
        _CACHE["nc"] = build_program()[0]
    nc = _CACHE["nc"]
    consts = make_consts()
    shared = dict(consts)
    f = lambda k: np.ascontiguousarray(inputs[k], dtype=np.float32)
    shared["s5_lam_re"] = f("s5_lam_re")[0]; shared["s5_lam_im"] = f("s5_lam_im")[0]; shared["s5_log_step"] = f("s5_log_step")[0].reshape(1, 64)
    shared["s5_b_re"] = f("s5_b_re")[0]; shared["s5_b_im"] = f("s5_b_im")[0]; shared["s5_c_re"] = f("s5_c_re")[0]; shared["s5_c_im"] = f("s5_c_im")[0]
    shared["s5_d"] = f("s5_d")[0].reshape(1, D); shared["s5_w_glu"] = f("s5_w_glu")[0]
    shared["mla_q_w_a"] = f("mla_q_w_a")[0]; shared["mla_q_norm"] = f("mla_q_norm")[0].reshape(256, 1); shared["mla_q_w_b"] = f("mla_q_w_b")[0]
    shared["mla_o_w"] = f("mla_o_w")[0]; shared["kv_w_a"] = f("kv_w_a"); shared["kv_norm"] = f("kv_norm").reshape(128, 1); shared["kv_w_b"] = f("kv_w_b")
    shared["ffn_w_in"] = f("ffn_w_in")[0]; shared["ffn_w_out"] = f("ffn_w_out")[0]
    shared["moe_router"] = f("moe_router")[0]; shared["moe_w_in"] = f("moe_w_in")[0]; shared["moe_w_out"] = f("moe_w_out")[0]
    if _CACHE.get("small_moe"):
        shared["moe_w_in"] = np.zeros((1, 1, 2), np.float32); shared["moe_w_out"] = np.zeros((1, 1, 2), np.float32)
    shared["ln_g"] = f("ln_g").reshape(4, D); shared["ln_b"] = f("ln_b").reshape(4, D)
    in_maps = []
    for c in range(NCORES):
        m = dict(shared)
        m["x"] = x[2 * c:2 * c + 2].reshape(4, 128, 8, D)
        in_maps.append(m)
    res = run_bass_kernel_spmd(nc, in_maps, core_ids=list(range(NCORES)))
    outs = [np.asarray(r["out"]).reshape(2, 2048, D) for r in res.results]
    return np.concatenate(outs, axis=0).astype(np.float32)


def phase_moe(nc, p, sbt, PS, bank, identf, layer_norm, load_ln, xc, out, moe_router, moe_w_in, moe_w_out):
    es = ExitStack()
    with es:
        G = sbt(es, "G", [128, 32, NEXP])
        g1 = ExitStack()
        with g1:
            wrb = sbt(g1, "wrb", [128, D, NEXP]); Xg_ = sbt(g1, "Xcg", [128, 8, D]); junk = sbt(g1, "junk", [128, D])
            lg = sbt(g1, "lg", [128, NEXP]); m8 = sbt(g1, "m8", [128, 8]); dd = sbt(g1, "dd", [128, 2])
            m1 = sbt(g1, "m1", [128, NEXP]); m2 = sbt(g1, "m2", [128, NEXP])
            p.dma("sp", wrb[:].rearrange("p d e -> p (d e)"), dap(moe_router, 0, [[0, 128], [1, D * NEXP]]), w=["wrb"])
            for t in range(4):
                p.dma("sp", Xg_[:], xc[t], r=[("xc", t)], w=["Xcg"])
                for j in range(8):
                    for e in range(NEXP):
                        p.tt("dve", junk[:], Xg_[:, j, :], wrb[:, :, e], ALU.mult, r=["Xcg", "wrb"], w=["junk"])
                        p.op("dve", (lambda e_: lambda en: en.reduce_sum(out=lg[:, e_:e_ + 1], in_=junk[:], axis=AX.X))(e), r=["junk"], w=["lg"])
                    p.op("dve", lambda en: en.max(out=m8[:], in_=lg[:]), r=["lg"], w=["m8"])
                    p.tt("dve", dd[:, 0:1], m8[:, 1:2], m8[:, 0:1], ALU.subtract, r=["m8"], w=["dd"])
                    p.act(dd[:, 1:2], dd[:, 0:1], AF.Sigmoid, r=["dd"], w=["dd"])
                    p.ts("dve", dd[:, 0:1], dd[:, 1:2], -1.0, 1.0, ALU.mult, ALU.add, r=["dd"], w=["dd"])
                    p.ts("dve", m1[:], lg[:], m8[:, 0:1], dd[:, 0:1], ALU.is_equal, ALU.mult, r=["lg", "m8", "dd"], w=["m1"])
                    p.ts("dve", m2[:], lg[:], m8[:, 1:2], dd[:, 1:2], ALU.is_equal, ALU.mult, r=["lg", "m8", "dd"], w=["m2"])
                    p.tt("dve", G[:, t * 8 + j, :], m1[:], m2[:], ALU.add, r=["m1", "m2"], w=[("G", t)])
        p.barrier()
        Xc = sbt(es, "Xcm", [128, 8, D]); acc = sbt(es, "acc", [128, 8, D]); xT = sbt(es, "xTm", [128, 8, 8, 128], BF16)
        Wi = [sbt(es, "Wi%d" % i, [128, 8, 2, 896], BF16) for i in range(2)]
        Wo2 = [sbt(es, "Wo%d" % i, [128, 7, D], BF16) for i in range(2)]
        hT = sbt(es, "hTm", [128, 7, 512], BF16)
        slt = [sbt(es, "sltm%d" % i, [128, 512], BF16) for i in range(2)]
        st6 = sbt(es, "st6m", [128, 2, 6]); mv = sbt(es, "mvm", [128, 2]); rstd = sbt(es, "rstdm", [128, 1])
        ln3 = load_ln(es, 3)
        idx = 0
        for t in range(4):
            p.dma("sp", Xc[:], xc[t], r=[("xc", t)], w=["Xcm"] + [("Xcm", j) for j in range(8)])
            p.op("pool", lambda en: en.memset(acc[:], 0.0), w=[("acc", j) for j in range(8)])
            for j in range(8):
                for dq in range(2):
                    bk = dq
                    kb = ("bank", bk)
                    for d4 in range(4):
                        dc = dq * 4 + d4
                        p.tr(bank(bk)[:, d4 * 128:(d4 + 1) * 128], Xc[:, j, dc * 128:(dc + 1) * 128], identf[:], r=["Xcm", "identf"], w=[kb])
                    p.cp("act", xT[:, dq * 4:(dq + 1) * 4, j, :], bank(bk).rearrange("p (d n) -> p d n", d=4), r=[kb], w=[("xTm", j // 4)])
            for e in range(NEXP):
                for qh in range(4):
                    wi = Wi[idx % 2]; wo = Wo2[idx % 2]
                    kwi = ("Wi", idx % 2); kwo = ("Wo", idx % 2)
                    idx += 1
                    for gu in range(2):
                        p.dma("pool", wi[:, :, gu, :], dap(moe_w_in, e * D * 2 * EDIM + gu * EDIM + qh * 896, [[2 * EDIM, 128], [2 * EDIM * 128, 8], [1, 896]]), w=[kwi])
                    p.dma("pool", wo[:], dap(moe_w_out, (e * EDIM + qh * 896) * D, [[D, 128], [128 * D, 7], [1, D]]), w=[kwo])
                    for half in range(2):
                        xk = [("xTm", half)]
                        for fc in range(7):
                            bg = 2 + 2 * (fc % 3); bu = bg + 1
                            kg = ("bank", bg); ku = ("bank", bu)
                            rhs_ = lambda dc: xT[:, dc, half * 4:(half + 1) * 4, :].rearrange("p j n -> p (j n)")
                            for dc in range(8):
                                p.mm(bank(bg), wi[:, dc, 0, fc * 128:(fc + 1) * 128], rhs_(dc), dc == 0, dc == 7, r=xk + [kwi], w=[kg])
                            for dc in range(8):
                                p.mm(bank(bu), wi[:, dc, 1, fc * 128:(fc + 1) * 128], rhs_(dc), dc == 0, dc == 7, r=xk + [kwi], w=[ku])
                            s_ = slt[fc % 2]; ks = ("sltm", fc % 2)
                            p.act(s_[:], bank(bg), AF.Silu, r=[kg], w=[ks])
                            p.tt("dve", hT[:, fc, :], s_[:], bank(bu), ALU.mult, r=[ks, ku], w=[("hTm", fc)])
                        for j in range(4):
                            jj = half * 4 + j
                            ko = [("bank", 0), ("bank", 1)]
                            for nh in range(2):
                                for fc in range(7):
                                    p.mm(PS[0][:, nh * 512:(nh + 1) * 512], hT[:, fc, j * 128:(j + 1) * 128], wo[:, fc, nh * 512:(nh + 1) * 512], fc == 0, fc == 6,
                                         r=[("hTm", fc), kwo], w=[ko[nh]])
                            p.stt("dve", acc[:, jj, :], PS[0][:], G[:, t * 8 + jj, e:e + 1], acc[:, jj, :], ALU.mult, ALU.add, r=ko + [("G", t), ("acc", jj)], w=[("acc", jj)])
            for j in range(8):
                blk = Xc[:, j, :]; kx = ("Xcm", j)
                p.stt("dve", blk, blk, ALPHA, acc[:, j, :], ALU.mult, ALU.add, r=["Xcm", kx, ("acc", j), ("xTm", 0), ("xTm", 1)], w=[kx])
                layer_norm(blk, ln3, kx, (st6, mv, rstd))
            p.dma("sp", out[t], Xc[:], r=["Xcm"] + [("Xcm", j) for j in range(8)], w=[("out", t)])
    p.barrier()


def phase_attn(nc, p, sbt, PS, bank, bankb, identf, identb, layer_norm, load_ln, sincos, xb, xc, W, consts):
    q_w_a, q_norm, q_w_b, o_w, kv_w_a, kv_norm, kv_w_b = W
    c_pos, c_invf, c_dmask = consts
    SCALE = 1.0 / math.sqrt(192.0)
    es = ExitStack()
    with es:
        Wqa = sbt(es, "Wqa", [128, 8, 256], BF16); Wkva = sbt(es, "Wkva", [128, 8, 192], BF16); WkvaB = sbt(es, "WkvaB", [128, 8, 64], BF16)
        Wqb = sbt(es, "Wqb", [128, 2, 1536], BF16); WqbB = sbt(es, "WqbB", [128, 2, 8, 64], BF16)
        Wkvb = sbt(es, "Wkvb", [128, 2, 8, 128], BF16); Wo = sbt(es, "Wo", [128, 8, D], BF16)
        qn = sbt(es, "qn", [128, 2]); kvn = sbt(es, "kvn", [128, 1]); invf = sbt(es, "invf", [128, 2])
        CC = sbt(es, "CC", [128, 2048]); SS = sbt(es, "SS", [128, 2048])
        dmask = sbt(es, "dmask", [128, 128], BF16); onesb = sbt(es, "onesb", [128, 128], BF16)
        epsr = sbt(es, "epsr", [128, 1])
        cqT = sbt(es, "cqT", [128, 2, 2048], BF16); ckvT = sbt(es, "ckvT", [128, 2048], BF16); krT = sbt(es, "krT", [128, 2048], BF16)
        OT = sbt(es, "OT", [128, 8, 2048], BF16)
        Xh = sbt(es, "Xha", [128, 4, D]); xT = sbt(es, "xTa", [128, 8, 4, 128], BF16)
        kTh = sbt(es, "kTh", [128, 2048], BF16); Vh = sbt(es, "Vh", [128, 16, 128], BF16)
        qTh = sbt(es, "qTh", [128, 2048], BF16); qrT = sbt(es, "qrT", [128, 2048], BF16)
        PT = [sbt(es, "PT%d" % i, [128, 512], BF16) for i in range(3)]
        rec = sbt(es, "rec", [128, 512]); t1 = sbt(es, "t1a", [128, 512]); t2 = sbt(es, "t2a", [128, 512])
        cqn = sbt(es, "cqn", [128, 256], BF16); ckvn = sbt(es, "ckvn", [128, 128], BF16)
        ss = sbt(es, "ss", [128, 4]); junk = sbt(es, "junka", [128, 256])
        st6 = sbt(es, "st6a", [128, 2, 6]); mv = sbt(es, "mva", [128, 2]); rstd = sbt(es, "rstda", [128, 1])
        ln2 = load_ln(es, 2)
        for dc in range(8):
            p.dma("pool", Wqa[:, dc, :], q_w_a[dc * 128:(dc + 1) * 128, :], w=["Wqa"])
            p.dma("pool", Wkva[:, dc, :], kv_w_a[dc * 128:(dc + 1) * 128, :], w=["Wkva"])
        p.dma("pool", WkvaB[:, :, 0:32], dap(kv_w_a, 160, [[192, 128], [192 * 128, 8], [1, 32]]), w=["WkvaB"])
        p.dma("pool", WkvaB[:, :, 32:64], dap(kv_w_a, 128, [[192, 128], [192 * 128, 8], [1, 32]]), w=["WkvaB"])
        for cc in range(2):
            p.dma("pool", Wqb[:, cc, :], q_w_b[cc * 128:(cc + 1) * 128, :], w=["Wqb"])
            p.dma("pool", WqbB[:, cc, :, 0:32], dap(q_w_b, cc * 128 * 1536 + 160, [[1536, 128], [192, 8], [1, 32]]), w=["WqbB"])
            p.dma("pool", WqbB[:, cc, :, 32:64], dap(q_w_b, cc * 128 * 1536 + 128, [[1536, 128], [192, 8], [1, 32]]), w=["WqbB"])
        for t_ in range(2):
            p.dma("pool", Wkvb[:, t_, :, :], dap(kv_w_b, t_ * 128, [[2048, 128], [256, 8], [1, 128]]), w=["Wkvb"])
        for h in range(8):
            p.dma("pool", Wo[:, h, :], o_w[h * 128:(h + 1) * 128, :], w=["Wo"])
        p.dma("sp", qn[:], dap(q_norm, 0, [[1, 128], [128, 2]]), w=["qn"])
        p.dma("sp", kvn[:], kv_norm, w=["kvn"])
        p.dma("sp", invf[0:64, :], c_invf, w=["invf"])
        p.dma("sp", CC[0:64, :], c_pos, w=["CC"])
        p.dma("pool", dmask[:], c_dmask, w=["dmask"])
        p.op("pool", lambda e: e.memset(onesb[:], 1.0), w=["onesb"])
        p.op("pool", lambda e: e.memset(epsr[:], RMS_EPS), w=["epsr"])
        p.op("pool", lambda e: e.memset(krT[:], 0.0), w=["krT"])
        p.op("pool", lambda e: e.memset(qrT[:], 0.0), w=["qrT"])
        for cc in range(2):
            p.ts("dve", Wqb[:, cc, :], Wqb[:, cc, :], qn[:, cc:cc + 1], None, ALU.mult, None, r=["Wqb", "qn"], w=["Wqb"])
            p.ts("dve", WqbB[:, cc, :, :], WqbB[:, cc, :, :], qn[:, cc:cc + 1], None, ALU.mult, None, r=["WqbB", "qn"], w=["WqbB"])
        p.ts("dve", Wkvb[:], Wkvb[:], kvn[:, 0:1], None, ALU.mult, None, r=["Wkvb", "kvn"], w=["Wkvb"])
        p.ts("dve", CC[0:64, :], CC[0:64, :], invf[0:64, 0:1], None, ALU.mult, None, r=["CC", "invf"], w=["CC"])
        p.cp("dve", SS[0:64, :], CC[0:64, :], r=["CC"], w=["SS"])
        for c0 in range(0, 2048, 512):
            sincos("dve", SS[0:64, c0:c0 + 512], CC[0:64, c0:c0 + 512], SS[0:64, c0:c0 + 512], t1[0:64, :], ["SS"], ["SS", "CC", "t1a"], npart=64)
        p.ts("dve", SS[0:64, :], SS[0:64, :], invf[0:64, 1:2], None, ALU.mult, None, r=["SS", "invf"], w=["SS"])
        for s_ in range(2):
            for tis in range(2):
                t = 2 * s_ + tis
                for jh in range(2):
                    p.dma("sp", Xh[:], xb[t][:, jh * 4:(jh + 1) * 4, :], r=[("xb", t)], w=["Xha"])
                    for j in range(4):
                        for dq in range(2):
                            bk = dq; kb = ("bank", bk)
                            for d4 in range(4):
                                dc = dq * 4 + d4
                                p.tr(bank(bk)[:, d4 * 128:(d4 + 1) * 128], Xh[:, j, dc * 128:(dc + 1) * 128], identf[:], r=["Xha", "identf"], w=[kb])
                            p.cp("act", xT[:, dq * 4:(dq + 1) * 4, j, :], bank(bk).rearrange("p (d n) -> p d n", d=4), r=[kb], w=["xTa"])
                    for j in range(4):
                        jg = jh * 4 + j
                        kb = ("bank", 2)
                        for dc in range(8):
                            p.mm(bank(2)[:, 0:256], xT[:, dc, j, :], Wqa[:, dc, :], dc == 0, dc == 7, r=["xTa", "Wqa"], w=[kb])
                        for dc in range(8):
                            p.mm(bank(2)[:, 256:384], xT[:, dc, j, :], Wkva[:, dc, 0:128], dc == 0, dc == 7, r=["xTa", "Wkva"], w=[kb])
                        p.act(junk[:], bank(2)[:, 0:256], AF.Square, r=[kb], w=["junka"], accum_out=ss[:, 0:1])
                        p.act(junk[:, 0:128], bank(2)[:, 256:384], AF.Square, r=[kb], w=["junka"], accum_out=ss[:, 1:2])
                        p.act(ss[:, 2:3], ss[:, 0:1], AF.Sqrt, r=["junka", "epsr"], w=["ss"], scale=1.0 / 256, bias=epsr[:, 0:1])
                        p.act(ss[:, 3:4], ss[:, 1:2], AF.Sqrt, r=["junka", "epsr"], w=["ss"], scale=1.0 / 128, bias=epsr[:, 0:1])
                        p.op("dve", lambda e: e.reciprocal(out=ss[:, 2:4], in_=ss[:, 2:4]), r=["ss"], w=["ss"])
                        p.ts("dve", cqn[:], bank(2)[:, 0:256], ss[:, 2:3], None, ALU.mult, None, r=[kb, "ss"], w=["cqn"])
                        p.ts("dve", ckvn[:], bank(2)[:, 256:384], ss[:, 3:4], None, ALU.mult, None, r=[kb, "ss"], w=["ckvn"])
                        kb3 = ("bank", 3)
                        for cc in range(2):
                            p.tr(bankb(3)[:, cc * 128:(cc + 1) * 128], cqn[:, cc * 128:(cc + 1) * 128], identb[:], r=["cqn", "identb"], w=[kb3])
                        p.tr(bankb(3)[:, 256:384], ckvn[:], identb[:], r=["ckvn", "identb"], w=[kb3])
                        for cc in range(2):
                            dst = cqT[:, cc, tis * 1024:(tis + 1) * 1024].rearrange("p (n j) -> p n j", j=8)[:, :, jg]
                            p.cp("act", dst, bankb(3)[:, cc * 128:(cc + 1) * 128], r=[kb3], w=["cqT"])
                        dst = ckvT[:, tis * 1024:(tis + 1) * 1024].rearrange("p (n j) -> p n j", j=8)[:, :, jg]
                        p.cp("act", dst, bankb(3)[:, 256:384], r=[kb3], w=["ckvT"])
                    kb4 = ("bank", 4); kb5 = ("bank", 5)
                    for dc in range(8):
                        p.mm(bank(4)[0:64, :], Wkva[:, dc, 128:192], xT[:, dc, :, :].rearrange("p j n -> p (j n)"), dc == 0, dc == 7, r=["xTa", "Wkva"], w=[kb4])
                    for dc in range(8):
                        p.mm(bank(5)[0:64, :], WkvaB[:, dc, :], xT[:, dc, :, :].rearrange("p j n -> p (j n)"), dc == 0, dc == 7, r=["xTa", "WkvaB"], w=[kb5])
                    vw = lambda T_: T_[0:64, tis * 1024:(tis + 1) * 1024].rearrange("p (n j) -> p j n", j=8)[:, jh * 4:(jh + 1) * 4, :]
                    p4 = lambda b_: b_[0:64, :].rearrange("p (j n) -> p j n", j=4)
                    t1v = t1[0:64, :].rearrange("p (j n) -> p j n", j=4); t2v = t2[0:64, :].rearrange("p (j n) -> p j n", j=4)
                    p.tt("dve", t1v, p4(bank(4)), vw(CC), ALU.mult, r=[kb4, "CC"], w=["t1a"])
                    p.tt("dve", t2v, p4(bank(5)), vw(SS), ALU.mult, r=[kb5, "SS"], w=["t2a"])
                    p.tt("dve", vw(krT), t1v, t2v, ALU.add, r=["t1a", "t2a"], w=["krT"])
            for h in range(8):
                for ch in range(4):
                    cs = slice(ch * 512, (ch + 1) * 512)
                    kb = ("bank", ch % 2)
                    p.mm(bank(ch % 2), Wkvb[:, 0, h, :], ckvT[:, cs], True, True, r=["Wkvb", "ckvT"], w=[kb])
                    p.cp("act", kTh[:, cs], bank(ch % 2), r=[kb], w=["kTh"])
                for pq in range(4):
                    kb = ("bank", 2 + pq % 2)
                    for pb in range(4):
                        pbk = pq * 4 + pb
                        p.mm(bank(2 + pq % 2)[:, pb * 128:(pb + 1) * 128], ckvT[:, pbk * 128:(pbk + 1) * 128], Wkvb[:, 1, h, :], True, True, r=["Wkvb", "ckvT"], w=[kb])
                    p.cp("act", Vh[:, pq * 4:(pq + 1) * 4, :].rearrange("p a d -> p (a d)"), bank(2 + pq % 2), r=[kb], w=["Vh"])
                for ch in range(4):
                    cs = slice(ch * 512, (ch + 1) * 512)
                    kb = ("bank", 4 + ch % 2)
                    for cc in range(2):
                        p.mm(bank(4 + ch % 2), Wqb[:, cc, h * 192:h * 192 + 128], cqT[:, cc, cs], cc == 0, cc == 1, r=["Wqb", "cqT"], w=[kb])
                    p.act(qTh[:, cs], bank(4 + ch % 2), AF.Copy, r=[kb], w=["qTh"], scale=SCALE)
                    kb6 = ("bank", 6); kb7 = ("bank", 7)
                    for cc in range(2):
                        p.mm(bank(6)[0:64, :], Wqb[:, cc, h * 192 + 128:h * 192 + 192], cqT[:, cc, cs], cc == 0, cc == 1, r=["Wqb", "cqT"], w=[kb6])
                    for cc in range(2):
                        p.mm(bank(7)[0:64, :], WqbB[:, cc, h, :], cqT[:, cc, cs], cc == 0, cc == 1, r=["WqbB", "cqT"], w=[kb7])
                    p.stt("dve", t1[0:64, :], bank(6)[0:64, :], SCALE, CC[0:64, cs], ALU.mult, ALU.mult, r=[kb6, "CC"], w=["t1a"])
                    p.stt("dve", t2[0:64, :], bank(7)[0:64, :], SCALE, SS[0:64, cs], ALU.mult, ALU.mult, r=[kb7, "SS"], w=["t2a"])
                    p.tt("dve", qrT[0:64, cs], t1[0:64, :], t2[0:64, :], ALU.add, r=["t1a", "t2a"], w=["qrT"])
                for qc in range(4):
                    nkb = 4 * (qc + 1)
                    ko = ("bank", 0); kd = ("bank", 1)
                    def geom(kbi):
                        q0 = max(kbi * 128, qc * 512)
                        return q0, (qc + 1) * 512 - q0, q0 - qc * 512

                    def emit_S(kbi):
                        q0, N, off = geom(kbi)
                        sb_ = 2 + (kbi % 4)
                        ksb = ("bank", sb_)
                        ks_ = slice(kbi * 128, (kbi + 1) * 128)
                        p.mm(bank(sb_)[:, 0:N], kTh[:, ks_], qTh[:, q0:q0 + N], True, False, r=["kTh", "qTh"], w=[ksb])
                        p.mm(bank(sb_)[:, 0:N], krT[:, ks_], qrT[:, q0:q0 + N], False, True, r=["krT", "qrT"], w=[ksb])

                    def emit_P(kbi):
                        q0, N, off = geom(kbi)
                        sb_ = 2 + (kbi % 4)
                        ksb = ("bank", sb_)
                        pt = PT[kbi % 3]; kpt = ("PT", kbi % 3)
                        p.act(pt[:, 0:N], bank(sb_)[:, 0:N], AF.Exp, r=[ksb], w=[kpt])
                        if kbi * 128 >= qc * 512:
                            p.tt("pool", pt[:, 0:128], pt[:, 0:128], dmask[:], ALU.mult, r=[kpt, "dmask"], w=[kpt])

                    def emit_V(kbi):
                        q0, N, off = geom(kbi)
                        pt = PT[kbi % 3]; kpt = ("PT", kbi % 3)
                        p.mm(bank(0)[:, off:off + N], Vh[:, kbi, :], pt[:, 0:N], kbi == 0, kbi == nkb - 1, r=["Vh", kpt], w=[ko])
                        p.mm(bank(1)[:, off:off + N], onesb[:], pt[:, 0:N], kbi == 0, kbi == nkb - 1, r=["onesb", kpt], w=[kd])

                    emit_S(0)
                    if nkb > 1:
                        emit_S(1)
                    for kbi in range(nkb):
                        emit_P(kbi)
                        if kbi + 2 < nkb:
                            emit_S(kbi + 2)
                        emit_V(kbi)
                    p.op("dve", lambda e: e.reciprocal(out=rec[:], in_=bank(1)), r=[kd], w=["rec"])
                    p.tt("dve", OT[:, h, qc * 512:(qc + 1) * 512], bank(0), rec[:], ALU.mult, r=[ko, "rec"], w=["OT"])
            for tis in range(2):
                t = 2 * s_ + tis
                for jh in range(2):
                    p.dma("sp", Xh[:], xb[t][:, jh * 4:(jh + 1) * 4, :], r=[("xb", t)], w=["Xha"] + [("Xha", j) for j in range(4)])
                    for j in range(4):
                        jg = jh * 4 + j
                        ko = [("bank", 6), ("bank", 7)]
                        for nh in range(2):
                            for h in range(8):
                                lh = OT[:, h, tis * 1024:(tis + 1) * 1024].rearrange("p (n j) -> p n j", j=8)[:, :, jg]
                                p.mm(PS[3][:, nh * 512:(nh + 1) * 512], lh, Wo[:, h, nh * 512:(nh + 1) * 512], h == 0, h == 7, r=["OT", "Wo"], w=[ko[nh]])
                        blk = Xh[:, j, :]; kx = ("Xha", j)
                        p.stt("dve", blk, blk, ALPHA, PS[3][:], ALU.mult, ALU.add, r=["Xha", kx] + ko, w=[kx])
                        layer_norm(blk, ln2, kx, (st6, mv, rstd))
                    p.dma("sp", xc[t][:, jh * 4:(jh + 1) * 4, :], Xh[:], r=["Xha"] + [("Xha", j) for j in range(4)], w=[("xc", t)])
    p.barrier()


def phase_moe_routed(nc, p, sbt, PS, bank, bankb, identf, identb, layer_norm, load_ln, xc, out, XE, YE, moe_router, moe_w_in, moe_w_out, consts):
    c_triu, c_ecol = consts
    NT = CAP // 128
    es = ExitStack()
    with es:
        S1 = [sbt(es, "S1_%d" % i, [128, 1], I32) for i in range(32)]; S2 = [sbt(es, "S2_%d" % i, [128, 1], I32) for i in range(32)]
        G1 = sbt(es, "G1", [128, 32]); G2 = sbt(es, "G2", [128, 32])
        g1 = ExitStack()
        with g1:
            wrb = sbt(g1, "wrb", [128, D, NEXP]); Xg_ = sbt(g1, "Xcg", [128, 8, D]); junk = sbt(g1, "junk", [128, D])
            Xb = [sbt(g1, "Xb%d" % i, [128, D], BF16) for i in range(2)]
            zt = sbt(g1, "zt", [128, NT, D], BF16)
            lg = sbt(g1, "lg", [128, NEXP]); m8 = sbt(g1, "m8", [128, 8]); dd = sbt(g1, "dd", [128, 1])
            mk = sbt(g1, "mk", [128, NEXP]); mkb = sbt(g1, "mkb", [128, NEXP], BF16)
            m1 = sbt(g1, "m1", [128, NEXP]); m2 = sbt(g1, "m2", [128, NEXP]); dest = sbt(g1, "dest", [128, NEXP]); ovf = sbt(g1, "ovf", [128, NEXP])
            base = sbt(g1, "base", [128, NEXP]); ecol = sbt(g1, "ecol", [128, NEXP]); sf = sbt(g1, "sf", [128, 2])
            triu = sbt(g1, "triu", [128, 128], BF16); onesb = sbt(g1, "onesb2", [128, 128], BF16)
            p.dma("sp", wrb[:].rearrange("p d e -> p (d e)"), dap(moe_router, 0, [[0, 128], [1, D * NEXP]]), w=["wrb"])
            p.dma("sp", ecol[:], c_ecol, w=["ecol"])
            p.dma("pool", triu[:], c_triu, w=["triu"])
            p.op("pool", lambda e: e.memset(onesb[:], 1.0), w=["onesb2"])
            p.op("pool", lambda e: e.memset(base[:], 0.0), w=["base"])
            p.op("pool", lambda e: e.memset(zt[:], 0.0), w=["zt"])
            for e in range(NEXP):
                p.dma("sp", XE[e * CAP:(e + 1) * CAP, :].rearrange("(t p) d -> p t d", p=128), zt[:], r=["zt"], w=["XE"])
            p.op("pool", lambda e: e.memset(junk[:], 0.0), w=["junk"])
            p.dma("sp", YE[NEXP * CAP:NEXP * CAP + 128, :], junk[:], r=["junk"], w=["YE"])
            for t in range(4):
                p.dma("sp", Xg_[:], xc[t], r=[("xc", t)], w=["Xcg"])
                for j in range(8):
                    b = t * 8 + j
                    xb_ = Xb[b % 2]; kxb = ("Xb", b % 2)
                    p.cp("act", xb_[:], Xg_[:, j, :], r=["Xcg"], w=[kxb])
                    for e in range(NEXP):
                        p.op("dve", (lambda e_, j_: lambda en: en.scalar_tensor_tensor(out=junk[:], in0=Xg_[:, j_, :], scalar=1.0, in1=wrb[:, :, e_],
                                                                                 op0=ALU.mult, op1=ALU.mult, accum_out=lg[:, e_:e_ + 1]))(e, j),
                             r=["Xcg", "wrb"], w=["junk", "lg"])
                    p.op("dve", lambda en: en.max(out=m8[:], in_=lg[:]), r=["lg"], w=["m8"])
                    p.tt("dve", dd[:], m8[:, 1:2], m8[:, 0:1], ALU.subtract, r=["m8"], w=["dd"])
                    p.act(G2[:, b:b + 1], dd[:], AF.Sigmoid, r=["dd"], w=[("G", b)])
                    p.ts("dve", G1[:, b:b + 1], G2[:, b:b + 1], -1.0, 1.0, ALU.mult, ALU.add, r=[("G", b)], w=[("G", b)])
                    p.ts("dve", mk[:], lg[:], m8[:, 1:2], None, ALU.is_ge, None, r=["lg", "m8"], w=["mk"])
                    p.cp("dve", mkb[:], mk[:], r=["mk"], w=["mkb"])
                    p.ts("dve", m1[:], lg[:], m8[:, 0:1], None, ALU.is_equal, None, r=["lg", "m8"], w=["m1"])
                    p.tt("dve", m2[:], mk[:], m1[:], ALU.subtract, r=["mk", "m1"], w=["m2"])
                    kb = ("bank", 2 + b % 2)
                    pb_ = bank(2 + b % 2)
                    p.mm(pb_[:, 0:NEXP], triu[:], mkb[:], True, True, r=["triu", "mkb"], w=[kb])
                    p.mm(pb_[:, 8:8 + NEXP], onesb[:], mkb[:], True, True, r=["onesb2", "mkb"], w=[kb])
                    p.tt("dve", dest[:], pb_[:, 0:NEXP], base[:], ALU.add, r=[kb, "base"], w=["dest"])
                    p.ts("dve", ovf[:], dest[:], float(CAP), 1.0e6, ALU.is_ge, ALU.mult, r=["dest"], w=["ovf"])
                    p.tt("dve", dest[:], dest[:], ecol[:], ALU.add, r=["dest", "ecol"], w=["dest"])
                    p.tt("dve", dest[:], dest[:], ovf[:], ALU.add, r=["dest", "ovf"], w=["dest"])
                    p.tt("dve", base[:], base[:], pb_[:, 8:8 + NEXP], ALU.add, r=[kb, "base", "dest"], w=["base"])
                    p.tt("dve", m1[:], m1[:], dest[:], ALU.mult, r=["m1", "dest"], w=["m1"])
                    p.tt("dve", m2[:], m2[:], dest[:], ALU.mult, r=["m2", "dest"], w=["m2"])
                    p.op("dve", lambda en: en.reduce_sum(out=sf[:, 0:1], in_=m1[:], axis=AX.X), r=["m1"], w=["sf"])
                    p.op("dve", lambda en: en.reduce_sum(out=sf[:, 1:2], in_=m2[:], axis=AX.X), r=["m2"], w=["sf"])
                    p.ts("dve", sf[:], sf[:], float(NEXP * CAP), None, ALU.min, None, r=["sf"], w=["sf"])
                    p.cp("dve", S1[b][:], sf[:, 0:1], r=["sf"], w=[("S", b)])
                    p.cp("dve", S2[b][:], sf[:, 1:2], r=["sf"], w=[("S", b)])
                    for S_ in (S1, S2):
                        p.op("pool", (lambda S__, b_, x_: lambda en: en.indirect_dma_start(
                            out=XE[:, :], out_offset=bass.IndirectOffsetOnAxis(ap=S__[b_][:, :], axis=0),
                            in_=x_[:, :], in_offset=None, oob_is_err=False))(S_, b, xb_),
                            r=[kxb, ("S", b), "XE"], w=[("XEs", b)], dma=True)
        p.barrier()
        ex = ExitStack()
        with ex:
            XEe = sbt(ex, "XEe", [128, 2, D], BF16); xeT = sbt(ex, "xeT", [128, 8, CAP], BF16)
            SCW = 512
            CHUNKS = [(c0, min(512, CAP - c0)) for c0 in range(0, CAP, 512)]
            Wi = [sbt(ex, "Wi%d" % i, [128, 8, 2, 896], BF16) for i in range(2)]
            Wo2 = [sbt(ex, "Wo%d" % i, [128, 7, D], BF16) for i in range(2)]
            hT = sbt(ex, "hTm", [128, 7, CAP], BF16)
            slt = [sbt(ex, "sltm%d" % i, [128, SCW], BF16) for i in range(2)]
            yacc = sbt(ex, "yacc", [128, NT, D])
            idx = 0
            for e in range(NEXP):
                for st in range(NT):
                    bk = st % 2; kb = ("bank", bk)
                    kxe = ("XEe", st % 2)
                    p.dma("sp", XEe[:, st % 2, :], XE[e * CAP + st * 128:e * CAP + (st + 1) * 128, :], r=[("XEs", b_) for b_ in range(32)] if (e == 0 and st < 2) else [], w=[kxe])
                    for dc in range(8):
                        p.tr(bankb(bk)[:, dc * 128:(dc + 1) * 128], XEe[:, st % 2, dc * 128:(dc + 1) * 128], identb[:], r=[kxe, "identb"], w=[kb])
                    p.cp("act", xeT[:, :, st * 128:(st + 1) * 128], bankb(bk).rearrange("p (d n) -> p d n", d=8), r=[kb], w=["xeT"])
                for qh in range(4):
                    wi = Wi[idx % 2]; wo = Wo2[idx % 2]
                    kwi = ("Wi", idx % 2); kwo = ("Wo", idx % 2)
                    idx += 1
                    for gu in range(2):
                        p.dma("pool", wi[:, :, gu, :], dap(moe_w_in, e * D * 2 * EDIM + gu * EDIM + qh * 896, [[2 * EDIM, 128], [2 * EDIM * 128, 8], [1, 896]]), w=[kwi])
                    p.dma("pool", wo[:], dap(moe_w_out, (e * EDIM + qh * 896) * D, [[D, 128], [128 * D, 7], [1, D]]), w=[kwo])
                    for fc in range(7):
                        for sc, (c0_, cw_) in enumerate(CHUNKS):
                            ci = fc * len(CHUNKS) + sc
                            bg = 2 + 2 * (ci % 2); bu = bg + 1
                            kg = ("bank", bg); ku = ("bank", bu)
                            cs = slice(c0_, c0_ + cw_)
                            for dc in range(8):
                                p.mm(bank(bg)[:, 0:cw_], wi[:, dc, 0, fc * 128:(fc + 1) * 128], xeT[:, dc, cs], dc == 0, dc == 7, r=["xeT", kwi], w=[kg])
                            for dc in range(8):
                                p.mm(bank(bu)[:, 0:cw_], wi[:, dc, 1, fc * 128:(fc + 1) * 128], xeT[:, dc, cs], dc == 0, dc == 7, r=["xeT", kwi], w=[ku])
                            s_ = slt[ci % 2]; ks = ("sltm", ci % 2)
                            p.act(s_[:, 0:cw_], bank(bg)[:, 0:cw_], AF.Silu, r=[kg], w=[ks])
                            p.tt("dve", hT[:, fc, cs], s_[:, 0:cw_], bank(bu)[:, 0:cw_], ALU.mult, r=[ks, ku], w=[("hTm", fc)])
                    for st in range(NT):
                        if st % 2 == 0:
                            po_, ko = PS[0], [("bank", 0), ("bank", 1)]
                        else:
                            po_, ko = PS[3], [("bank", 6), ("bank", 7)]
                        for nh in range(2):
                            for fc in range(7):
                                p.mm(po_[:, nh * 512:(nh + 1) * 512], hT[:, fc, st * 128:(st + 1) * 128], wo[:, fc, nh * 512:(nh + 1) * 512], fc == 0, fc == 6,
                                     r=[("hTm", fc), kwo], w=[ko[nh]])
                        if qh == 0:
                            p.cp("act", yacc[:, st, :], po_[:], r=ko, w=[("yacc", st)])
                        else:
                            p.tt("dve", yacc[:, st, :], yacc[:, st, :], po_[:], ALU.add, r=ko + [("yacc", st)], w=[("yacc", st)])
                p.dma("sp", YE[e * CAP:(e + 1) * CAP, :].rearrange("(t p) d -> p t d", p=128), yacc[:], r=[("yacc", st) for st in range(NT)], w=["YE"])
        p.barrier()
        cb = ExitStack()
        with cb:
            Xc = sbt(cb, "Xcm", [128, 8, D])
            Y1 = [sbt(cb, "Y1_%d" % i, [128, D]) for i in range(16)]; Y2 = [sbt(cb, "Y2_%d" % i, [128, D]) for i in range(16)]
            st6 = sbt(cb, "st6m", [128, 2, 6]); mv = sbt(cb, "mvm", [128, 2]); rstd = sbt(cb, "rstdm", [128, 1])
            ln3 = load_ln(cb, 3)

            def gathers(t):
                for j in range(8):
                    b = t * 8 + j
                    ky = ("Y", b % 16)
                    for S_, y_ in ((S1, Y1[b % 16]), (S2, Y2[b % 16])):
                        p.op("pool", (lambda S__, b_, yy: lambda en: en.indirect_dma_start(
                            out=yy[:, :], out_offset=None, in_=YE[:, :],
                            in_offset=bass.IndirectOffsetOnAxis(ap=S__[b_][:, :], axis=0),
                            oob_is_err=False))(S_, b, y_),
                            r=["YE", ("S", b)], w=[ky], dma=True)

            gathers(0)
            for t in range(4):
                if t + 1 < 4:
                    gathers(t + 1)
                p.dma("sp", Xc[:], xc[t], r=[("xc", t)], w=["Xcm"] + [("Xcm", j) for j in range(8)])
                for j in range(8):
                    b = t * 8 + j
                    y1 = Y1[b % 16]; y2 = Y2[b % 16]; ky = ("Y", b % 16)
                    p.ts("dve", y1[:], y1[:], G1[:, b:b + 1], None, ALU.mult, None, r=[ky, ("G", b)], w=[ky])
                    p.stt("dve", y1[:], y2[:], G2[:, b:b + 1], y1[:], ALU.mult, ALU.add, r=[ky, ("G", b)], w=[ky])
                    blk = Xc[:, j, :]; kx = ("Xcm", j)
                    p.stt("dve", blk, blk, ALPHA, y1[:], ALU.mult, ALU.add, r=["Xcm", kx, ky], w=[kx])
                    layer_norm(blk, ln3, kx, (st6, mv, rstd), aff="dve")
                p.dma("sp", out[t], Xc[:], r=["Xcm"] + [("Xcm", j) for j in range(8)], w=[("out", t)])
    p.barrier()
```

```python
import math
import numpy as np
import ml_dtypes
from contextlib import ExitStack
import concourse.bass as bass
import concourse.mybir as mybir
from concourse.bass_utils import run_bass_kernel_spmd

F32 = mybir.dt.float32
BF16 = mybir.dt.bfloat16
I32 = mybir.dt.int32
AF = mybir.ActivationFunctionType
ALU = mybir.AluOpType
AX = mybir.AxisListType

import os
SAME_ENG_SYNC = os.environ.get("KSES", "1") == "1"
ROUTED = True
RING = 12
NCORES = 8
D = 1024
ALPHA = (2.0 * 2) ** 0.25
LN_EPS = 1e-5
RMS_EPS = 1e-6
PI = math.pi
TWO_PI = 2.0 * math.pi
CAP = 1408
NEXP = 8
FFN = 2816
EDIM = 3584


class Op:
    __slots__ = ("eng", "fn", "deps", "sig", "dma", "semi", "semv")


class Prog:
    ENGS = ("pe", "act", "dve", "pool", "sp")
    COMPUTE = ("pe", "act", "dve", "pool")

    def __init__(self, nc):
        self.nc = nc
        self.q = {e: [] for e in self.ENGS}
        self.lastw = {}
        self.rd = {}
        self.ndma = {e: 0 for e in self.ENGS}
        self.dmaops = {e: [] for e in self.ENGS}

    def op(self, eng, fn, r=(), w=(), dma=False, extra=()):
        o = Op()
        o.eng = eng
        o.fn = fn
        o.dma = dma
        o.sig = dma
        o.semi = None
        o.semv = 0
        deps = set(extra)
        lastw = self.lastw
        rd = self.rd
        for k in r:
            lw = lastw.get(k)
            if lw is not None:
                deps.add(lw)
        for k in w:
            lw = lastw.get(k)
            if lw is not None:
                deps.add(lw)
            x = rd.get(k)
            if x:
                deps.update(x)
        for k in r:
            l = rd.get(k)
            if l is None:
                rd[k] = [o]
            elif not dma:
                for i, x in enumerate(l):
                    if x.eng == eng and not x.dma:
                        l[i] = o
                        break
                else:
                    l.append(o)
            else:
                l.append(o)
        for k in w:
            lastw[k] = o
            rd[k] = []
        deps.discard(o)
        o.deps = deps
        if dma:
            o.semi = self.ndma[eng] % RING
            o.semv = 16 * (self.ndma[eng] // RING + 1)
            self.ndma[eng] += 1
            self.dmaops[eng].append(o)
        self.q[eng].append(o)
        return o

    def dma(self, eng, out, in_, r=(), w=(), **kw):
        return self.op(eng, lambda e: e.dma_start(out=out, in_=in_, **kw), r=r, w=w, dma=True)

    def barrier(self):
        lastc = []
        for e in self.COMPUTE:
            for o in reversed(self.q[e]):
                if not o.dma and o.fn is not None:
                    lastc.append(o)
                    break
        lastd = []
        for e in self.ENGS:
            lastd.extend(self.dmaops[e][-RING:])
        for e in self.ENGS:
            self.op(e, None, extra=lastc + lastd)

    def mm(self, out, lhsT, rhs, start, stop, r, w):
        return self.op("pe", lambda e: e.matmul(out, lhsT=lhsT, rhs=rhs, start=start, stop=stop), r=r, w=w)

    def tr(self, out, in_, ident, r, w):
        return self.op("pe", lambda e: e.transpose(out=out, in_=in_, identity=ident), r=r, w=w)

    def act(self, out, in_, func, r, w, eng="act", **kw):
        return self.op(eng, lambda e: e.activation(out=out, in_=in_, func=func, **kw), r=r, w=w)

    def tt(self, eng, out, in0, in1, op, r, w):
        return self.op(eng, lambda e: e.tensor_tensor(out=out, in0=in0, in1=in1, op=op), r=r, w=w)

    def ts(self, eng, out, in0, s1, s2, op0, op1, r, w):
        if op1 is None:
            return self.op(eng, lambda e: e.tensor_scalar(out=out, in0=in0, scalar1=s1, scalar2=None, op0=op0), r=r, w=w)
        return self.op(eng, lambda e: e.tensor_scalar(out=out, in0=in0, scalar1=s1, scalar2=s2, op0=op0, op1=op1), r=r, w=w)

    def stt(self, eng, out, in0, scalar, in1, op0, op1, r, w):
        return self.op(eng, lambda e: e.scalar_tensor_tensor(out=out, in0=in0, scalar=scalar, in1=in1, op0=op0, op1=op1), r=r, w=w)

    def cp(self, eng, out, in_, r, w):
        if eng == "act":
            return self.op(eng, lambda e: e.copy(out=out, in_=in_), r=r, w=w)
        return self.op(eng, lambda e: e.tensor_copy(out=out, in_=in_), r=r, w=w)

    def emit(self):
        nc = self.nc
        es = ExitStack()
        with es:
            csem = {e: es.enter_context(nc.semaphore("c_" + e)) for e in self.COMPUTE}
            dsem = {}
            for e in self.ENGS:
                if self.ndma[e]:
                    dsem[e] = [es.enter_context(nc.semaphore("d_%s_%d" % (e, i))) for i in range(min(RING, self.ndma[e]))]

            def skip_same(d, ename):
                return d.eng == ename and (ename == "pe" or ename == "sp" or not SAME_ENG_SYNC)

            for e in self.ENGS:
                for o in self.q[e]:
                    for d in o.deps:
                        if not d.dma and not skip_same(d, e):
                            d.sig = True
            for e in self.COMPUTE:
                c = 0
                for o in self.q[e]:
                    if o.sig and not o.dma:
                        c += 1
                        o.semv = c
            self.stats = {}

            def run(ename, eng):
                waited = {}
                nw = 0
                for o in self.q[ename]:
                    waits = {}
                    for d in o.deps:
                        if d.dma:
                            s = dsem[d.eng][d.semi]
                        else:
                            if skip_same(d, ename):
                                continue
                            s = csem[d.eng]
                        if waits.get(s, 0) < d.semv:
                            waits[s] = d.semv
                    if o.dma and o.semv > 16:
                        s = dsem[ename][o.semi]
                        if waits.get(s, 0) < o.semv - 16:
                            waits[s] = o.semv - 16
                    for s, v in waits.items():
                        if waited.get(s, 0) >= v:
                            continue
                        waited[s] = v
                        eng.wait_ge(s, v)
                        nw += 1
                    if o.fn is None:
                        continue
                    ins = o.fn(eng)
                    if o.dma:
                        ins.then_inc(dsem[ename][o.semi], 16)
                    elif o.sig:
                        ins.then_inc(csem[ename], 1)
                self.stats[ename] = (len(self.q[ename]), nw)

            with nc.Block() as block:
                @block.tensor
                def _(eng):
                    run("pe", eng)

                @block.scalar
                def _(eng):
                    run("act", eng)

                @block.vector
                def _(eng):
                    run("dve", eng)

                @block.gpsimd
                def _(eng):
                    run("pool", eng)

                @block.sync
                def _(eng):
                    run("sp", eng)


def dap(t, offset, ap):
    return bass.AP(t.tensor, offset, [list(x) for x in ap])


class _Cut(Exception):
    pass


def build_program(nphase=99, debug=False, cut=None, mini=False):
    nc = bass.Bass("TRN2", target_bir_lowering=False)
    p = Prog(nc)
    S5IN = ("s5_lam_re", "s5_lam_im", "s5_log_step", "s5_b_re", "s5_b_im", "s5_c_re", "s5_c_im", "s5_d")

    def din(name, shape, dt=F32):
        if mini and not (name in S5IN or name.startswith("c_")):
            shape = [1, 2]
        return nc.dram_tensor(name, list(shape), dt, kind="ExternalInput").ap()

    def cutpoint(n):
        if cut is not None and n >= cut:
            raise _Cut()

    dbg = debug if isinstance(debug, (set, list, tuple)) else (("xa", "xb", "xc", "xe", "ye") if debug else ())

    def dscr(name, shape, dt=F32):
        return nc.dram_tensor(name, list(shape), dt, kind=("ExternalOutput" if name in dbg else "Internal")).ap()

    x_in = din("x", [4, 128, 8, D])
    dbgo = nc.dram_tensor("dbgo", [128, 4096], F32, kind="ExternalOutput").ap() if cut is not None else None
    lam_re = din("s5_lam_re", [64, 64]); lam_im = din("s5_lam_im", [64, 64]); log_step = din("s5_log_step", [1, 64])
    b_re = din("s5_b_re", [64, 64, 16]); b_im = din("s5_b_im", [64, 64, 16])
    c_re = din("s5_c_re", [64, 16, 64]); c_im = din("s5_c_im", [64, 16, 64])
    s5_d = din("s5_d", [1, D]); w_glu = din("s5_w_glu", [D, 2 * D])
    q_w_a = din("mla_q_w_a", [D, 256]); q_norm = din("mla_q_norm", [256, 1]); q_w_b = din("mla_q_w_b", [256, 1536])
    o_w = din("mla_o_w", [D, D]); kv_w_a = din("kv_w_a", [D, 192]); kv_norm = din("kv_norm", [128, 1]); kv_w_b = din("kv_w_b", [128, 2048])
    ffn_w_in = din("ffn_w_in", [D, 2 * FFN]); ffn_w_out = din("ffn_w_out", [FFN, D])
    moe_router = din("moe_router", [D, NEXP]); moe_w_in = din("moe_w_in", [NEXP, D, 2 * EDIM] if nphase >= 5 else [1, 1, 2]); moe_w_out = din("moe_w_out", [NEXP, EDIM, D] if nphase >= 5 else [1, 1, 2])
    ln_g = din("ln_g", [4, D]); ln_b = din("ln_b", [4, D])
    c_ident = din("c_ident", [128, 128]); c_evec = din("c_evec", [128, 24]); c_cmask = din("c_cmask", [128, 256])
    c_iota = din("c_iota", [128, 2, 128]); c_triu = din("c_triu", [128, 128]); c_ecol = din("c_ecol", [128, NEXP])
    c_dmask = din("c_dmask", [128, 128]); c_pos = din("c_pos", [64, 2048]); c_invf = din("c_invf", [64, 2])

    out = nc.dram_tensor("out", [4, 128, 8, D], F32, kind="ExternalOutput").ap()
    xa = dscr("xa", [4, 128, 8, D]); xb = dscr("xb", [4, 128, 8, D]); xc = dscr("xc", [4, 128, 8, D])
    XE = dscr("xe", [NEXP * CAP + 128, D], BF16)
    YE = dscr("ye", [NEXP * CAP + 128, D])

    top = ExitStack()
    with top:
        ar = {"peak": 0, "n": 0}

        def sbt(es, name, shape, dt=F32):
            ar["n"] += 1
            return es.enter_context(nc.sbuf_tensor("%s_%d" % (name, ar["n"]), list(shape), dt))

        PS = [top.enter_context(nc.psum_tensor("ps%d" % i, [128, 1024], F32)) for i in range(4)]

        def bank(i):
            return PS[i // 2][:, (i % 2) * 512:(i % 2) * 512 + 512]

        def bankb(i):
            return PS[i // 2][:, (i % 2) * 512:(i % 2) * 512 + 512].bitcast(BF16)

        identf = sbt(top, "identf", [128, 128]); identb = sbt(top, "identb", [128, 128], BF16)
        negpi = sbt(top, "negpi", [128, 1]); epsln = sbt(top, "epsln", [128, 1])
        p.dma("sp", identf[:], c_ident, w=["identf"])

        def load_ln(es, lnidx):
            g_ = sbt(es, "lng%d" % lnidx, [128, D]); b_ = sbt(es, "lnb%d" % lnidx, [128, D])
            p.dma("sp", g_[:], dap(ln_g, lnidx * D, [[0, 128], [1, D]]), w=[("lng", lnidx)])
            p.dma("sp", b_[:], dap(ln_b, lnidx * D, [[0, 128], [1, D]]), w=[("lnb", lnidx)])
            return (g_, b_, lnidx)
        p.cp("pool", identb[:], identf[:], r=["identf"], w=["identb"])
        p.op("pool", lambda e: e.memset(negpi[:], -PI), w=["negpi"])
        p.op("pool", lambda e: e.memset(epsln[:], LN_EPS), w=["epsln"])
        halfpi = sbt(top, "halfpi", [128, 1])
        p.op("pool", lambda e: e.memset(halfpi[:], PI / 2), w=["halfpi"])
        MAGIC = 12582912.0

        def sincos(eng, sin_out, cos_out, y, tmp, rk, wk, npart=128):
            p.ts(eng, tmp, y, MAGIC, MAGIC, ALU.add, ALU.subtract, r=rk, w=wk)
            p.tt(eng, tmp, y, tmp, ALU.subtract, r=rk + wk, w=wk)
            p.act(sin_out, tmp, AF.Sin, r=wk, w=wk, scale=TWO_PI)
            p.stt(eng, tmp, tmp, -1.0, tmp, ALU.mult, ALU.max, r=wk, w=wk)
            p.act(cos_out, tmp, AF.Sin, r=wk + ["halfpi"], w=wk, scale=-TWO_PI, bias=halfpi[0:npart, 0:1])


        def layer_norm(blk, lnp, keyblk, stat, aff="pool"):
            st6, mv, rstd = stat
            lng_, lnb_, lnidx = lnp
            for h in range(2):
                p.op("dve", (lambda hh: lambda e: e.bn_stats(out=st6[:, hh, :], in_=blk[:, hh * 512:(hh + 1) * 512]))(h), r=[keyblk], w=["st6"])
            p.op("dve", lambda e: e.bn_aggr(out=mv[:], in_=st6[:].rearrange("p a b -> p (a b)")), r=["st6"], w=["mv"])
            p.act(rstd[:], mv[:, 1:2], AF.Sqrt, r=["mv", "epsln"], w=["rstd"], bias=epsln[:, 0:1])
            p.op("dve", lambda e: e.reciprocal(out=rstd[:], in_=rstd[:]), r=["rstd"], w=["rstd"])
            p.ts("dve", blk, blk, mv[:, 0:1], rstd[:, 0:1], ALU.subtract, ALU.mult, r=[keyblk, "mv", "rstd"], w=[keyblk])
            p.tt(aff, blk, blk, lng_[:], ALU.mult, r=[keyblk, ("lng", lnidx)], w=[keyblk])
            p.tt(aff, blk, blk, lnb_[:], ALU.add, r=[keyblk, ("lnb", lnidx)], w=[keyblk])

        try:
            s5 = ExitStack()
            with s5:
                CtrlRe = sbt(s5, "CtrlRe", [128, 32, 128], BF16); CtrlIm = sbt(s5, "CtrlIm", [128, 32, 128], BF16)
                Toep = sbt(s5, "Toep", [128, 64, 128], BF16)
                ObRe = sbt(s5, "ObRe", [128, 64, 128], BF16); ObIm = sbt(s5, "ObIm", [128, 64, 128], BF16)
                rho = sbt(s5, "rho", [128, 32]); phi = sbt(s5, "phi", [128, 32])
                p0 = ExitStack()
                with p0:
                    lr = sbt(p0, "lr", [128, 32]); li = sbt(p0, "li", [128, 32]); ls = sbt(p0, "ls", [128, 32])
                    Bre = sbt(p0, "Bre", [128, 32, 16]); Bim = sbt(p0, "Bim", [128, 32, 16])
                    Cre = sbt(p0, "Cre", [128, 32, 16]); Cim = sbt(p0, "Cim", [128, 32, 16])
                    evec = sbt(p0, "evec", [128, 24]); cmask = sbt(p0, "cmask", [128, 256])
                    dt = sbt(p0, "dt", [128, 32]); lrdt = sbt(p0, "lrdt", [128, 32]); lidt = sbt(p0, "lidt", [128, 32])
                    PWre = sbt(p0, "PWre", [128, 32, 24]); PWim = sbt(p0, "PWim", [128, 32, 24])
                    sm = [sbt(p0, "sm%d" % i, [128, 32]) for i in range(6)]
                    fre = sbt(p0, "fre", [128, 32]); fim = sbt(p0, "fim", [128, 32])
                    Bbre = sbt(p0, "Bbre", [128, 32, 16]); Bbim = sbt(p0, "Bbim", [128, 32, 16]); tb = sbt(p0, "tb", [128, 32, 16])
                    XBre = sbt(p0, "XBre", [128, 32, 8, 16]); XBim = sbt(p0, "XBim", [128, 32, 8, 16])
                    Zre = sbt(p0, "Zre", [128, 32, 8, 16]); Zim = sbt(p0, "Zim", [128, 32, 8, 16])
                    p0b = ExitStack()
                    A = sbt(p0b, "A", [128, 32, 24]); ANG = sbt(p0b, "ANG", [128, 32, 24]); XS = sbt(p0b, "XS", [128, 32, 24])
                    T1 = sbt(p0b, "T1", [128, 32, 8, 16]); T2 = sbt(p0b, "T2", [128, 32, 8, 16])
                    for a in range(2):
                        sl = slice(a * 64, (a + 1) * 64)
                        p.dma("sp", lr[sl, :], dap(lam_re, a * 64, [[1, 64], [128, 32]]), w=["lr"])
                        p.dma("sp", li[sl, :], dap(lam_im, a * 64, [[1, 64], [128, 32]]), w=["li"])
                        p.dma("sp", ls[sl, :], dap(log_step, a, [[0, 64], [2, 32]]), w=["ls"])
                        p.dma("sp", Bre[sl], dap(b_re, a * 1024, [[16, 64], [2048, 32], [1, 16]]), w=["Bre"])
                        p.dma("sp", Bim[sl], dap(b_im, a * 1024, [[16, 64], [2048, 32], [1, 16]]), w=["Bim"])
                        for c_ in range(16):
                            p.dma("sp", Cre[sl, :, c_], dap(c_re, a * 1024 + c_ * 64, [[1, 64], [2048, 32]]), w=["Cre"])
                            p.dma("sp", Cim[sl, :, c_], dap(c_im, a * 1024 + c_ * 64, [[1, 64], [2048, 32]]), w=["Cim"])
                    p.dma("sp", evec[:], c_evec, w=["evec"])
                    p.dma("sp", cmask[:], c_cmask, w=["cmask"])
                    cutpoint(1)
                    V, M, Sb, AD = "dve", ALU.mult, ALU.subtract, ALU.add
                    p.act(dt[:], ls[:], AF.Exp, r=["ls"], w=["dt"])
                    p.tt(V, lrdt[:], lr[:], dt[:], M, r=["lr", "dt"], w=["lrdt"])
                    p.tt(V, lidt[:], li[:], dt[:], M, r=["li", "dt"], w=["lidt"])
                    b3 = lambda t2: t2[:].unsqueeze(2).to_broadcast([128, 32, 24])
                    ev3 = evec[:].unsqueeze(1).to_broadcast([128, 32, 24])
                    p.tt(V, A[:], b3(lrdt), ev3, M, r=["lrdt", "evec"], w=["A"])
                    p.act(A[:], A[:], AF.Exp, r=["A"], w=["A"])
                    p.ts(V, sm[0][:], lidt[:], 1.0 / TWO_PI, None, M, None, r=["lidt"], w=["sm0"])
                    p.tt(V, ANG[:], b3(sm[0]), ev3, M, r=["sm0", "evec"], w=["ANG"])
                    sincos(V, PWim[:], PWre[:], ANG[:], XS[:], ["ANG"], ["XS", "PWre", "PWim"])
                    p.tt(V, PWim[:], PWim[:], A[:], M, r=["A", "PWim", "XS"], w=["PWim"])
                    p.tt(V, PWre[:], PWre[:], A[:], M, r=["A", "PWre", "XS"], w=["PWre"])
                    if cut == 2:
                        p.dma("sp", dbgo[:, 0:768], PWre[:].rearrange("p a b -> p (a b)"), r=["PWre"], w=["dbgo"])
                        p.dma("sp", dbgo[:, 768:1536], PWim[:].rearrange("p a b -> p (a b)"), r=["PWim"], w=["dbgo"])
                    cutpoint(2)
                    lbre = PWre[:, :, 16]; lbim = PWim[:, :, 16]
                    p.tt(V, sm[0][:], lr[:], lr[:], M, r=["lr"], w=["sm0"])
                    p.tt(V, sm[1][:], li[:], li[:], M, r=["li"], w=["sm1"])
                    p.tt(V, sm[0][:], sm[0][:], sm[1][:], AD, r=["sm0", "sm1"], w=["sm0"])
                    p.op(V, lambda e: e.reciprocal(out=sm[0][:], in_=sm[0][:]), r=["sm0"], w=["sm0"])
                    p.ts(V, sm[1][:], lbre, -1.0, None, AD, None, r=["PWre", "sm0"], w=["sm1"])
                    p.tt(V, sm[2][:], sm[1][:], lr[:], M, r=["sm1", "lr"], w=["sm2"])
                    p.tt(V, sm[3][:], lbim, li[:], M, r=["PWim", "li"], w=["sm3"])
                    p.tt(V, sm[2][:], sm[2][:], sm[3][:], AD, r=["sm2", "sm3"], w=["sm2"])
                    p.tt(V, fre[:], sm[2][:], sm[0][:], M, r=["sm2", "sm0"], w=["fre"])
                    p.tt(V, sm[4][:], lbim, lr[:], M, r=["PWim", "lr"], w=["sm4"])
                    p.tt(V, sm[5][:], sm[1][:], li[:], M, r=["sm1", "li"], w=["sm5"])
                    p.tt(V, sm[4][:], sm[4][:], sm[5][:], Sb, r=["sm4", "sm5"], w=["sm4"])
                    p.tt(V, fim[:], sm[4][:], sm[0][:], M, r=["sm4", "sm0"], w=["fim"])
                    f3 = lambda t2: t2[:].unsqueeze(2).to_broadcast([128, 32, 16])
                    p.tt(V, Bbre[:], Bre[:], f3(fre), M, r=["Bre", "fre"], w=["Bbre"])
                    p.tt(V, tb[:], Bim[:], f3(fim), M, r=["Bim", "fim"], w=["tb"])
                    p.tt(V, Bbre[:], Bbre[:], tb[:], Sb, r=["Bbre", "tb"], w=["Bbre"])
                    p.tt(V, Bbim[:], Bim[:], f3(fre), M, r=["Bim", "fre"], w=["Bbim"])
                    p.tt(V, tb[:], Bre[:], f3(fim), M, r=["Bre", "fim", "Bbre"], w=["tb"])
                    p.tt(V, Bbim[:], Bbim[:], tb[:], AD, r=["Bbim", "tb"], w=["Bbim"])
                    pw4 = lambda t3, lo: t3[:, :, lo:lo + 8].unsqueeze(3).to_broadcast([128, 32, 8, 16])
                    v4 = lambda t3: t3[:].unsqueeze(2).to_broadcast([128, 32, 8, 16])

                    def cmul(outre, outim, pre, pim, lo, vre, vim, kre, kim, negim, obf=None):
                        p.tt(V, T1[:], pw4(pre, lo), v4(vre), M, r=["PWre", kre], w=["T1"])
                        p.tt(V, T2[:], pw4(pim, lo), v4(vim), M, r=["PWim", kim], w=["T2"])
                        p.tt(V, outre[0], T1[:], T2[:], Sb, r=["T1", "T2"], w=[outre[1]])
                        p.tt(V, T1[:], pw4(pre, lo), v4(vim), M, r=["PWre", kim, outre[1]], w=["T1"])
                        p.tt(V, T2[:], pw4(pim, lo), v4(vre), M, r=["PWim", kre, outre[1]], w=["T2"])
                        if negim:
                            p.stt(V, outim[0], T1[:], -1.0, T2[:], M, Sb, r=["T1", "T2"], w=[outim[1]])
                        else:
                            p.tt(V, outim[0], T1[:], T2[:], AD, r=["T1", "T2"], w=[outim[1]])

                    cmul((XBre[:], "XBre"), (XBim[:], "XBim"), PWre, PWim, 0, Bbre, Bbim, "Bbre", "Bbim", False)
                    cmul((Zre[:], "Zre"), (Zim[:], "Zim"), PWre, PWim, 8, Cre, Cim, "Cre", "Cim", True)
                    p.op("pool", lambda e: e.memset(ObRe[:], 0.0), w=["ObRe"])
                    p.op("pool", lambda e: e.memset(ObIm[:], 0.0), w=["ObIm"])
                    pw4 = lambda t3, lo: t3[:, :, lo:lo + 8].unsqueeze(3).to_broadcast([128, 32, 8, 16])
                    p.tt(V, T1[:], pw4(PWre, 16), v4(Cre), M, r=["PWre", "Cre"], w=["T1"])
                    p.tt(V, T2[:], pw4(PWim, 16), v4(Cim), M, r=["PWim", "Cim"], w=["T2"])
                    for a in range(2):
                        sl = slice(a * 64, (a + 1) * 64)
                        ov = ObRe[sl, :, :].rearrange("p (k a) (j c) -> p k a j c", a=2, c=16)[:, :, a, :, :]
                        p.tt(V, ov, T1[sl], T2[sl], Sb, r=["T1", "T2"], w=["ObRe"])
                    p.tt(V, T1[:], pw4(PWre, 16), v4(Cim), M, r=["PWre", "Cim", "ObRe"], w=["T1"])
                    p.tt(V, T2[:], pw4(PWim, 16), v4(Cre), M, r=["PWim", "Cre", "ObRe"], w=["T2"])
                    for a in range(2):
                        sl = slice(a * 64, (a + 1) * 64)
                        ov = ObIm[sl, :, :].rearrange("p (k a) (j c) -> p k a j c", a=2, c=16)[:, :, a, :, :]
                        p.stt(V, ov, T1[sl], -1.0, T2[sl], M, Sb, r=["T1", "T2"], w=["ObIm"])
                    p.act(rho[:], lrdt[:], AF.Exp, r=["lrdt"], w=["rho"], scale=8.0)
                    p.ts(V, phi[:], lidt[:], 8.0 / TWO_PI, None, M, None, r=["lidt"], w=["phi"])
                    if cut == 3:
                        p.dma("sp", dbgo[:, 0:4096], XBre[:].rearrange("p a b c -> p (a b c)"), r=["XBre"], w=["dbgo"])
                    cutpoint(3)
                    p0b.close()
                    HL = {}
                    for nm, src in (("XBre", XBre), ("XBim", XBim)):
                        hi = sbt(p0, nm + "_h", [128, 32, 128], BF16)
                        p.cp("pool", hi[:], src[:].rearrange("p k i c -> p k (i c)"), r=[nm], w=["HL", "T1", "T2", "A", "ANG", "XS"])
                        HL[nm] = hi
                    for nm, src in (("Zre", Zre), ("Zim", Zim)):
                        hi = sbt(p0, nm + "_bd", [128, 32, 2, 128], BF16)
                        p.op("pool", (lambda h_: lambda e: e.memset(h_[:], 0.0))(hi), w=["HL", "T1", "T2", "A", "ANG", "XS"])
                        for a in range(2):
                            sl = slice(a * 64, (a + 1) * 64)
                            p.cp("pool", hi[sl, :, a, :], src[sl].rearrange("p k i c -> p k (i c)"), r=[nm], w=["HL"])
                        HL[nm] = hi
                    for k in range(32):
                        bk = 2 * (k % 2)
                        xr = XBre[:, k, :, :].rearrange("p i c -> p (i c)"); xi = XBim[:, k, :, :].rearrange("p i c -> p (i c)")
                        kb0 = ("bank", bk); kb1 = ("bank", bk + 1)
                        import os
                        PEVAR = int(os.environ.get("PEVAR", "0"))
                        if PEVAR in (0, 1):
                            p.tr(bank(bk)[:, 0:128], xr, identf[:], r=["XBre", "identf"], w=[kb0])
                            p.tr(bank(bk)[:, 128:256], xi, identf[:], r=["XBim", "identf"], w=[kb0])
                            p.cp("act", CtrlRe[:, k, :], bank(bk)[:, 0:128], r=[kb0], w=[("Ctrl", k)])
                            p.cp("act", CtrlIm[:, k, :], bank(bk)[:, 128:256], r=[kb0], w=[("Ctrl", k)])
                        o_ = bank(bk + 1)[:, 0:256]
                        p.mm(o_, HL["XBre"][:, k, :], HL["Zre"][:, k, :, :].rearrange("p a m -> p (a m)"), True, False, r=["HL"], w=[kb1])
                        p.mm(o_, HL["XBim"][:, k, :], HL["Zim"][:, k, :, :].rearrange("p a m -> p (a m)"), False, True, r=["HL"], w=[kb1])
                        p.tt(V, Toep[:, 2 * k:2 * k + 2, :].rearrange("p g m -> p (g m)"), bank(bk + 1)[:, 0:256], cmask[:], M, r=[kb1, "cmask"], w=[("Toep", k)])
                    if cut == 4:
                        p.dma("sp", dbgo[:, 0:2048], Toep[:, 0:32, :].rearrange("p a b -> p (a b)").bitcast(F32), r=[("Toep", k) for k in range(32)], w=["dbgo"])
                    cutpoint(4)
                p.barrier()
                if nphase < 1:
                    pass
                p1 = ExitStack()
                with p1:
                    Wglu = sbt(p1, "Wglu", [128, 8, 2 * D], BF16)
                    Xc = sbt(p1, "Xc", [128, 8, D])
                    bfA = sbt(p1, "bfA", [128, 8192], BF16)
                    bfB = sbt(p1, "bfB", [128, 8192], BF16)
                    Ere = sbt(p1, "Ere", [128, 32, 129], BF16); Eim = sbt(p1, "Eim", [128, 32, 129], BF16)
                    Vc = sbt(p1, "Vc", [128, 32, 2])
                    iota = sbt(p1, "iota", [128, 2, 128]); Dt = sbt(p1, "Dt", [128, D])
                    sg = sbt(p1, "sg", [128, D])
                    st6 = sbt(p1, "st6", [128, 2, 6]); mv = sbt(p1, "mv", [128, 2]); rstd = sbt(p1, "rstd", [128, 1])
                    NB = 2
                    tabs = [[sbt(p1, "tab%d_%d" % (i, j), [128, 128]) for j in range(8)] for i in range(NB)]
                    du = [sbt(p1, "du%d" % i, [128, 4, 8, 16]) for i in range(2)]
                    ln0 = load_ln(p1, 0)
                    p.dma("sp", iota[:], c_iota, w=["iota"])
                    p.dma("sp", Dt[:], dap(s5_d, 0, [[0, 128], [1, D]]), w=["Dt"])
                    for kc in range(8):
                        p.dma("pool", Wglu[:, kc, :], w_glu[kc * 128:(kc + 1) * 128, :], w=[("Wglu", kc)])
                    Xg = bfA[:].rearrange("p (g i c) -> p g i c", g=64, i=8)
                    Ycb = bfA[:].rearrange("p (j f) -> p j f", j=8)
                    Ub = bfB[:].rearrange("p (g n) -> p g n", g=64)
                    actT = bfB[:].rearrange("p (fc j n) -> p fc j n", fc=8, j=8)
                    for t in range(4 if nphase >= 1 else 0):
                        tis = t % 2
                        p.dma("sp", Xc[:], x_in[t], w=["Xc"] + [("Xc", j) for j in range(8)])
                        p.cp("pool", Xg, Xc[:].rearrange("p i (g c) -> p g i c", c=16), r=["Xc"], w=["bfA"] + [("Ycb", gq) for gq in range(16)])
                        if tis == 0:
                            p.op("pool", lambda e: e.memset(Ere[:, :, 0:1], 0.0), w=["Ecar"])
                            p.op("pool", lambda e: e.memset(Eim[:, :, 0:1], 0.0), w=["Ecar"])
                            p.op("pool", lambda e: e.memset(Vc[:], 0.0), w=[("Vc", k) for k in range(32)])
                        else:
                            p.cp("pool", Ere[:, :, 0:1], Ere[:, :, 128:129], r=[("E", k) for k in range(32)], w=["Ecar"])
                            p.cp("pool", Eim[:, :, 0:1], Eim[:, :, 128:129], r=[("E", k) for k in range(32)], w=["Ecar"])
                        for gq in range(8):
                            bk = gq % 2
                            kb = ("bank", bk)
                            for gg in range(8):
                                g = gq * 8 + gg
                                p.tr(bankb(bk)[:, gg * 128:(gg + 1) * 128], Xg[:, g, :, :].rearrange("p i c -> p (i c)"), identb[:], r=["bfA", "identb"], w=[kb])
                            p.cp("act", Ub[:, gq * 8:(gq + 1) * 8, :].rearrange("p g n -> p (g n)"), bankb(bk), r=[kb], w=[("Ub", gq)] + [("actT", j) for j in range(8)])
                        for k in range(32):
                            bk = 2 + (k % 3)
                            kb = ("bank", bk)
                            T = tabs[k % NB]
                            tk = ("tab", k % NB)
                            for a in range(2):
                                g = 2 * k + a
                                p.mm(bank(bk)[:, a * 128:(a + 1) * 128], CtrlRe[:, k, :], Ub[:, g, :], True, True, r=[("Ctrl", k), ("Ub", g // 8)], w=[kb])
                                p.mm(bank(bk)[:, 256 + a * 128:256 + (a + 1) * 128], CtrlIm[:, k, :], Ub[:, g, :], True, True, r=[("Ctrl", k), ("Ub", g // 8)], w=[kb])
                            h0 = slice(0, 64); h1 = slice(64, 128)
                            p.ts("dve", T[2][:], iota[:, tis, :], phi[:, k:k + 1], None, ALU.mult, None, r=["iota", "phi"], w=[tk])
                            sincos("dve", T[0][:], T[1][:], T[2][:], T[3][:], [tk], [tk])
                            sn, cn = T[0], T[1]
                            kw_ = ("tabw", k % NB)
                            for hs, ro in ((h0, 0), (h1, 128)):
                                p.cp("act", T[6][hs], bank(bk)[hs, ro:ro + 128], r=[kb, tk], w=[tk])
                                p.cp("act", T[7][hs], bank(bk)[hs, 256 + ro:256 + ro + 128], r=[kb, tk], w=[tk])
                            p.tt("dve", T[2][:], cn[:], T[6][:], ALU.mult, r=[tk], w=[tk])
                            p.tt("dve", T[3][:], sn[:], T[7][:], ALU.mult, r=[tk], w=[tk])
                            p.tt("dve", T[4][:], cn[:], T[7][:], ALU.mult, r=[tk], w=[tk])
                            p.tt("dve", T[5][:], sn[:], T[6][:], ALU.mult, r=[tk], w=[tk])
                            p.tt("dve", T[2][:], T[2][:], T[3][:], ALU.add, r=[tk], w=[tk])
                            p.tt("dve", T[4][:], T[4][:], T[5][:], ALU.subtract, r=[tk], w=[tk])
                            rb = rho[:, k:k + 1].to_broadcast([128, 128])
                            p.op("dve", (lambda o_, d1, ini: lambda e: e.tensor_tensor_scan(out=o_, data0=rb, data1=d1, initial=ini, op0=ALU.mult, op1=ALU.add))(T[6][:], T[2][:], Vc[:, k, 0:1]), r=[tk, "rho", ("Vc", k)], w=[tk])
                            p.op("dve", (lambda o_, d1, ini: lambda e: e.tensor_tensor_scan(out=o_, data0=rb, data1=d1, initial=ini, op0=ALU.mult, op1=ALU.add))(T[7][:], T[4][:], Vc[:, k, 1:2]), r=[tk, "rho", ("Vc", k)], w=[tk])
                            p.cp("dve", Vc[:, k, 0:1], T[6][:, 127:128], r=[tk], w=[("Vc", k)])
                            p.cp("dve", Vc[:, k, 1:2], T[7][:, 127:128], r=[tk], w=[("Vc", k)])
                            p.tt("pool", T[3][:], cn[:], T[6][:], ALU.mult, r=[tk], w=[tk])
                            p.tt("pool", T[5][:], sn[:], T[7][:], ALU.mult, r=[tk], w=[tk])
                            p.tt("pool", Ere[:, k, 1:129], T[3][:], T[5][:], ALU.subtract, r=[tk], w=[("E", k)])
                            p.tt("pool", T[3][:], sn[:], T[6][:], ALU.mult, r=[tk], w=[tk])
                            p.tt("pool", T[5][:], cn[:], T[7][:], ALU.mult, r=[tk], w=[tk])
                            p.tt("pool", Eim[:, k, 1:129], T[3][:], T[5][:], ALU.add, r=[tk], w=[("E", k)])
                        for gq in range(16):
                            bk = 5 + (gq % 2)
                            kb = ("bank", bk)
                            for gg in range(4):
                                g = gq * 4 + gg
                                k = g // 2; a = g % 2
                                sl = slice(a * 64, (a + 1) * 64)
                                o_ = bank(bk)[:, gg * 128:(gg + 1) * 128]
                                p.mm(o_, Ub[:, g, :], Toep[:, g, :], True, False, r=[("Ub", g // 8), ("Toep", k)], w=[kb])
                                p.mm(o_, Ere[:, k, 0:128], ObRe[:, g, :], False, False, r=[("E", k), "Ecar", "ObRe"], w=[kb])
                                p.mm(o_, Eim[:, k, 0:128], ObIm[:, g, :], False, True, r=[("E", k), "Ecar", "ObIm"], w=[kb])
                            dd = du[gq % 2]
                            kd = ("du", gq % 2)
                            xv = Xc[:, :, gq * 64:(gq + 1) * 64].rearrange("p i (g c) -> p g i c", c=16)
                            dv = Dt[:, gq * 64:(gq + 1) * 64].rearrange("p (g c) -> p g c", c=16).unsqueeze(2).to_broadcast([128, 4, 8, 16])
                            p.tt("pool", dd[:], xv, dv, ALU.mult, r=["Xc", "Dt"], w=[kd])
                            p.tt("dve", dd[:], dd[:], bank(bk).rearrange("p (g j c) -> p g j c", g=4, j=8), ALU.add, r=[kd, kb], w=[kd])
                            yv = Ycb[:, :, gq * 64:(gq + 1) * 64].rearrange("p j (g c) -> p g j c", c=16)
                            p.act(yv, dd[:], AF.Gelu_apprx_tanh, r=[kd, ("Ub", 0), ("Ub", 7)], w=[("Ycb", gq)])
                        ykeys = [("Ycb", gq) for gq in range(16)]
                        for j in range(8):
                            bk = j % 2
                            kb = ("bank", bk)
                            for fc in range(8):
                                p.tr(bankb(bk)[:, fc * 128:(fc + 1) * 128], Ycb[:, j, fc * 128:(fc + 1) * 128], identb[:], r=ykeys + ["identb"], w=[kb])
                            p.cp("act", actT[:, :, j, :], bankb(bk).rearrange("p (fc n) -> p fc n", fc=8), r=[kb], w=[("Ub", gq2) for gq2 in range(8)] + [("actT", j)])
                        for j in range(8):
                            if j % 2 == 0:
                                pv, pg, kv, kg = PS[1], PS[2], [("bank", 2), ("bank", 3)], [("bank", 4), ("bank", 5)]
                            else:
                                pv, pg, kv, kg = PS[3], PS[0], [("bank", 6), ("bank", 7)], [("bank", 0), ("bank", 1)]
                            for half in range(4):
                                tgt = (pv if half < 2 else pg)[:, (half % 2) * 512:(half % 2) * 512 + 512]
                                kk = (kv if half < 2 else kg)[half % 2]
                                for fc in range(8):
                                    p.mm(tgt, actT[:, fc, j, :], Wglu[:, fc, half * 512:(half + 1) * 512], fc == 0, fc == 7,
                                         r=[("actT", j), ("Wglu", fc)], w=[kk])
                            p.act(sg[:], pg[:], AF.Sigmoid, r=kg, w=["sg"])
                            p.tt("dve", sg[:], sg[:], pv[:], ALU.mult, r=["sg"] + kv, w=["sg"])
                            blk = Xc[:, j, :]
                            kx = ("Xc", j)
                            p.stt("dve", blk, blk, ALPHA, sg[:], ALU.mult, ALU.add, r=["Xc", kx, "sg"] + [("du", 0), ("du", 1)], w=[kx])
                            layer_norm(blk, ln0, kx, (st6, mv, rstd))
                        p.dma("sp", xa[t], Xc[:], r=["Xc"] + [("Xc", j) for j in range(8)], w=[("xa", t)])
                p.barrier()
            if nphase >= 2:
                phase2(nc, p, top, sbt, PS, bank, bankb, identf, layer_norm, load_ln, xa, xb, ffn_w_in, ffn_w_out)
        except _Cut:
            pass
        if nphase >= 3:
            phase_attn(nc, p, sbt, PS, bank, bankb, identf, identb, layer_norm, load_ln, sincos, xb, xc,
                       (q_w_a, q_norm, q_w_b, o_w, kv_w_a, kv_norm, kv_w_b), (c_pos, c_invf, c_dmask))
        if nphase >= 5:
            if ROUTED:
                phase_moe_routed(nc, p, sbt, PS, bank, bankb, identf, identb, layer_norm, load_ln, xc, out, XE, YE, moe_router, moe_w_in, moe_w_out, (c_triu, c_ecol))
            else:
                phase_moe(nc, p, sbt, PS, bank, identf, layer_norm, load_ln, xc, out, moe_router, moe_w_in, moe_w_out)
        final = [o for o in p.dmaops["sp"][-RING:]] + [o for o in p.dmaops["pool"][-RING:]]
        p.op("sp", None, extra=final)
        with nc.allow_non_contiguous_dma(reason="tiny strided parameter loads"):
            p.emit()
        p.peak = ar["peak"]
    return nc, p


def phase2(nc, p, top, sbt, PS, bank, bankb, identf, layer_norm, load_ln, xa, xb, ffn_w_in, ffn_w_out):
    es = ExitStack()
    with es:
        Win = sbt(es, "Win", [128, 8, 2 * FFN], BF16)
        Wout = sbt(es, "Wout", [128, 22, D], BF16)
        Xh = sbt(es, "Xh", [128, 4, D])
        xT = sbt(es, "xT", [128, 8, 4, 128], BF16)
        hT = sbt(es, "hT", [128, 22, 512], BF16)
        slt = [sbt(es, "slt%d" % i, [128, 512], BF16) for i in range(2)]
        st6 = sbt(es, "st6b", [128, 2, 6]); mv = sbt(es, "mvb", [128, 2]); rstd = sbt(es, "rstdb", [128, 1])
        ln1 = load_ln(es, 1)
        for kc in range(8):
            p.dma("pool", Win[:, kc, :], ffn_w_in[kc * 128:(kc + 1) * 128, :], w=[("Win", kc)])
        for fc in range(22):
            p.dma("pool", Wout[:, fc, :], ffn_w_out[fc * 128:(fc + 1) * 128, :], w=[("Wout", fc)])
        for t in range(4):
            for jh in range(2):
                p.dma("sp", Xh[:], xa[t][:, jh * 4:(jh + 1) * 4, :], r=[("xa", t)], w=["Xh"] + [("Xh", j) for j in range(4)])
                for j in range(4):
                    for dq in range(2):
                        bk = (j * 2 + dq) % 2
                        kb = ("bank", bk)
                        for dd in range(4):
                            dc = dq * 4 + dd
                            p.tr(bank(bk)[:, dd * 128:(dd + 1) * 128], Xh[:, j, dc * 128:(dc + 1) * 128], identf[:], r=["Xh", "identf"], w=[kb])
                        p.cp("act", xT[:, dq * 4:(dq + 1) * 4, j, :], bank(bk).rearrange("p (d n) -> p d n", d=4), r=[kb], w=[("xT", j)])
                xkeys = [("xT", j) for j in range(4)]
                for fc in range(22):
                    bg = 2 + 2 * (fc % 3); bu = bg + 1
                    kg = ("bank", bg); ku = ("bank", bu)
                    for dc in range(8):
                        p.mm(bank(bg), Win[:, dc, fc * 128:(fc + 1) * 128], xT[:, dc, :, :].rearrange("p j n -> p (j n)"), dc == 0, dc == 7, r=xkeys + [("Win", dc)], w=[kg])
                    for dc in range(8):
                        p.mm(bank(bu), Win[:, dc, FFN + fc * 128:FFN + (fc + 1) * 128], xT[:, dc, :, :].rearrange("p j n -> p (j n)"), dc == 0, dc == 7, r=xkeys + [("Win", dc)], w=[ku])
                    s_ = slt[fc % 2]
                    ks = ("slt", fc % 2)
                    p.act(s_[:], bank(bg), AF.Silu, r=[kg], w=[ks])
                    p.tt("dve", hT[:, fc, :], s_[:], bank(bu), ALU.mult, r=[ks, ku], w=[("hT", fc)])
                for j in range(4):
                    po = PS[0]
                    ko = [("bank", 0), ("bank", 1)]
                    for nh in range(2):
                        for fc in range(22):
                            p.mm(po[:, nh * 512:(nh + 1) * 512], hT[:, fc, j * 128:(j + 1) * 128], Wout[:, fc, nh * 512:(nh + 1) * 512], fc == 0, fc == 21,
                                 r=[("hT", fc), ("Wout", fc)], w=[ko[nh]])
                    blk = Xh[:, j, :]
                    kx = ("Xh", j)
                    p.stt("dve", blk, blk, ALPHA, po[:], ALU.mult, ALU.add, r=["Xh", kx] + ko, w=[kx])
                    layer_norm(blk, ln1, kx, (st6, mv, rstd))
                p.dma("sp", xb[t][:, jh * 4:(jh + 1) * 4, :], Xh[:], r=["Xh"] + [("Xh", j) for j in range(4)], w=[("xb", t)])
    p.barrier()


def make_consts():
    c = {}
    c["c_ident"] = np.eye(128, dtype=np.float32)
    ev = np.array([7, 6, 5, 4, 3, 2, 1, 0, -7, -6, -5, -4, -3, -2, -1, 0, 1, 2, 3, 4, 5, 6, 7, 8], np.float32)
    c["c_evec"] = np.tile(ev[None, :], (128, 1))
    i_idx = np.arange(128) // 16
    cm = (i_idx[None, :] >= i_idx[:, None]).astype(np.float32)
    c["c_cmask"] = np.concatenate([cm, cm], axis=1)
    io = np.arange(256, dtype=np.float32).reshape(2, 128)
    c["c_iota"] = np.tile(io[None], (128, 1, 1))
    c["c_triu"] = (np.arange(128)[:, None] < np.arange(128)[None, :]).astype(np.float32)
    c["c_ecol"] = np.tile((np.arange(NEXP, dtype=np.float32) * CAP)[None, :], (128, 1))
    c["c_dmask"] = (np.arange(128)[:, None] <= np.arange(128)[None, :]).astype(np.float32)
    c["c_pos"] = np.tile(np.arange(2048, dtype=np.float32)[None, :], (64, 1))
    invf = ((10000.0 ** (-np.arange(0, 64, 2, dtype=np.float64) / 64)) / (2 * np.pi)).astype(np.float32)
    sgn = np.concatenate([-np.ones(32, np.float32), np.ones(32, np.float32)])
    c["c_invf"] = np.stack([np.concatenate([invf, invf]), sgn], axis=1).astype(np.float32)
    return c


_CACHE = {}


def kernel(**inputs):
    x = np.ascontiguousarray(inputs["x"], dtype=np.float32)
    if "nc" not in _CACHE:
        _CACHE["nc"] = build_program()[0]
    nc = _CACHE["nc"]
    consts = make_consts()
    shared = dict(consts)
    f = lambda k: np.ascontiguousarray(inputs[k], dtype=np.float32)
    shared["s5_lam_re"] = f("s5_lam_re")[0]; shared["s5_lam_im"] = f("s5_lam_im")[0]; shared["s5_log_step"] = f("s5_log_step")[0].reshape(1, 64)
    shared["s5_b_re"] = f("s5_b_re")[0]; shared["s5_b_im"] = f("s5_b_im")[0]; shared["s5_c_re"] = f("s5_c_re")[0]; shared["s5_c_im"] = f("s5_c_im")[0]
    shared["s5_d"] = f("s5_d")[0].reshape(1, D); shared["s5_w_glu"] = f("s5_w_glu")[0]
    shared["mla_q_w_a"] = f("mla_q_w_a")[0]; shared["mla_q_norm"] = f("mla_q_norm")[0].reshape(256, 1); shared["mla_q_w_b"] = f("mla_q_w_b")[0]
    shared["mla_o_w"] = f("mla_o_w")[0]; shared["kv_w_a"] = f("kv_w_a"); shared["kv_norm"] = f("kv_norm").reshape(128, 1); shared["kv_w_b"] = f("kv_w_b")
    shared["ffn_w_in"] = f("ffn_w_in")[0]; shared["ffn_w_out"] = f("ffn_w_out")[0]
    shared["moe_router"] = f("moe_router")[0]; shared["moe_w_in"] = f("moe_w_in")[0]; shared["moe_w_out"] = f("moe_w_out")[0]
    if _CACHE.get("small_moe"):
        shared["moe_w_in"] = np.zeros((1, 1, 2), np.float32); shared["moe_w_out"] = np.zeros((1, 1, 2), np.float32)
    shared["ln_g"] = f("ln_g").reshape(4, D); shared["ln_b"] = f("ln_b").reshape(4, D)
    in_maps = []
    for c in range(NCORES):
        m = dict(shared)
        m["x"] = x[2 * c:2 * c + 2].reshape(4, 128, 8, D)
        in_maps.append(m)
    res = run_bass_kernel_spmd(nc, in_maps, core_ids=list(range(NCORES)))
    outs = [np.asarray(r["out"]).reshape(2, 2048, D) for r in res.results]
    return np.concatenate(outs, axis=0).astype(np.float32)


def phase_moe(nc, p, sbt, PS, bank, identf, layer_norm, load_ln, xc, out, moe_router, moe_w_in, moe_w_out):
    es = ExitStack()
    with es:
        G = sbt(es, "G", [128, 32, NEXP])
        g1 = ExitStack()
        with g1:
            wrb = sbt(g1, "wrb", [128, D, NEXP]); Xg_ = sbt(g1, "Xcg", [128, 8, D]); junk = sbt(g1, "junk", [128, D])
            lg = sbt(g1, "lg", [128, NEXP]); m8 = sbt(g1, "m8", [128, 8]); dd = sbt(g1, "dd", [128, 2])
            m1 = sbt(g1, "m1", [128, NEXP]); m2 = sbt(g1, "m2", [128, NEXP])
            p.dma("sp", wrb[:].rearrange("p d e -> p (d e)"), dap(moe_router, 0, [[0, 128], [1, D * NEXP]]), w=["wrb"])
            for t in range(4):
                p.dma("sp", Xg_[:], xc[t], r=[("xc", t)], w=["Xcg"])
                for j in range(8):
                    for e in range(NEXP):
                        p.tt("dve", junk[:], Xg_[:, j, :], wrb[:, :, e], ALU.mult, r=["Xcg", "wrb"], w=["junk"])
                        p.op("dve", (lambda e_: lambda en: en.reduce_sum(out=lg[:, e_:e_ + 1], in_=junk[:], axis=AX.X))(e), r=["junk"], w=["lg"])
                    p.op("dve", lambda en: en.max(out=m8[:], in_=lg[:]), r=["lg"], w=["m8"])
                    p.tt("dve", dd[:, 0:1], m8[:, 1:2], m8[:, 0:1], ALU.subtract, r=["m8"], w=["dd"])
                    p.act(dd[:, 1:2], dd[:, 0:1], AF.Sigmoid, r=["dd"], w=["dd"])
                    p.ts("dve", dd[:, 0:1], dd[:, 1:2], -1.0, 1.0, ALU.mult, ALU.add, r=["dd"], w=["dd"])
                    p.ts("dve", m1[:], lg[:], m8[:, 0:1], dd[:, 0:1], ALU.is_equal, ALU.mult, r=["lg", "m8", "dd"], w=["m1"])
                    p.ts("dve", m2[:], lg[:], m8[:, 1:2], dd[:, 1:2], ALU.is_equal, ALU.mult, r=["lg", "m8", "dd"], w=["m2"])
                    p.tt("dve", G[:, t * 8 + j, :], m1[:], m2[:], ALU.add, r=["m1", "m2"], w=[("G", t)])
        p.barrier()
        Xc = sbt(es, "Xcm", [128, 8, D]); acc = sbt(es, "acc", [128, 8, D]); xT = sbt(es, "xTm", [128, 8, 8, 128], BF16)
        Wi = [sbt(es, "Wi%d" % i, [128, 8, 2, 896], BF16) for i in range(2)]
        Wo2 = [sbt(es, "Wo%d" % i, [128, 7, D], BF16) for i in range(2)]
        hT = sbt(es, "hTm", [128, 7, 512], BF16)
        slt = [sbt(es, "sltm%d" % i, [128, 512], BF16) for i in range(2)]
        st6 = sbt(es, "st6m", [128, 2, 6]); mv = sbt(es, "mvm", [128, 2]); rstd = sbt(es, "rstdm", [128, 1])
        ln3 = load_ln(es, 3)
        idx = 0
        for t in range(4):
            p.dma("sp", Xc[:], xc[t], r=[("xc", t)], w=["Xcm"] + [("Xcm", j) for j in range(8)])
            p.op("pool", lambda en: en.memset(acc[:], 0.0), w=[("acc", j) for j in range(8)])
            for j in range(8):
                for dq in range(2):
                    bk = dq
                    kb = ("bank", bk)
                    for d4 in range(4):
                        dc = dq * 4 + d4
                        p.tr(bank(bk)[:, d4 * 128:(d4 + 1) * 128], Xc[:, j, dc * 128:(dc + 1) * 128], identf[:], r=["Xcm", "identf"], w=[kb])
                    p.cp("act", xT[:, dq * 4:(dq + 1) * 4, j, :], bank(bk).rearrange("p (d n) -> p d n", d=4), r=[kb], w=[("xTm", j // 4)])
            for e in range(NEXP):
                for qh in range(4):
                    wi = Wi[idx % 2]; wo = Wo2[idx % 2]
                    kwi = ("Wi", idx % 2); kwo = ("Wo", idx % 2)
                    idx += 1
                    for gu in range(2):
                        p.dma("pool", wi[:, :, gu, :], dap(moe_w_in, e * D * 2 * EDIM + gu * EDIM + qh * 896, [[2 * EDIM, 128], [2 * EDIM * 128, 8], [1, 896]]), w=[kwi])
                    p.dma("pool", wo[:], dap(moe_w_out, (e * EDIM + qh * 896) * D, [[D, 128], [128 * D, 7], [1, D]]), w=[kwo])
                    for half in range(2):
                        xk = [("xTm", half)]
                        for fc in range(7):
                            bg = 2 + 2 * (fc % 3); bu = bg + 1
                            kg = ("bank", bg); ku = ("bank", bu)
                            rhs_ = lambda dc: xT[:, dc, half * 4:(half + 1) * 4, :].rearrange("p j n -> p (j n)")
                            for dc in range(8):
                                p.mm(bank(bg), wi[:, dc, 0, fc * 128:(fc + 1) * 128], rhs_(dc), dc == 0, dc == 7, r=xk + [kwi], w=[kg])
                            for dc in range(8):
                                p.mm(bank(bu), wi[:, dc, 1, fc * 128:(fc + 1) * 128], rhs_(dc), dc == 0, dc == 7, r=xk + [kwi], w=[ku])
                            s_ = slt[fc % 2]; ks = ("sltm", fc % 2)
                            p.act(s_[:], bank(bg), AF.Silu, r=[kg], w=[ks])
                            p.tt("dve", hT[:, fc, :], s_[:], bank(bu), ALU.mult, r=[ks, ku], w=[("hTm", fc)])
                        for j in range(4):
                            jj = half * 4 + j
                            ko = [("bank", 0), ("bank", 1)]
                            for nh in range(2):
                                for fc in range(7):
                                    p.mm(PS[0][:, nh * 512:(nh + 1) * 512], hT[:, fc, j * 128:(j + 1) * 128], wo[:, fc, nh * 512:(nh + 1) * 512], fc == 0, fc == 6,
                                         r=[("hTm", fc), kwo], w=[ko[nh]])
                            p.stt("dve", acc[:, jj, :], PS[0][:], G[:, t * 8 + jj, e:e + 1], acc[:, jj, :], ALU.mult, ALU.add, r=ko + [("G", t), ("acc", jj)], w=[("acc", jj)])
            for j in range(8):
                blk = Xc[:, j, :]; kx = ("Xcm", j)
                p.stt("dve", blk, blk, ALPHA, acc[:, j, :], ALU.mult, ALU.add, r=["Xcm", kx, ("acc", j), ("xTm", 0), ("xTm", 1)], w=[kx])
                layer_norm(blk, ln3, kx, (st6, mv, rstd))
            p.dma("sp", out[t], Xc[:], r=["Xcm"] + [("Xcm", j) for j in range(8)], w=[("out", t)])
    p.barrier()


def phase_attn(nc, p, sbt, PS, bank, bankb, identf, identb, layer_norm, load_ln, sincos, xb, xc, W, consts):
    q_w_a, q_norm, q_w_b, o_w, kv_w_a, kv_norm, kv_w_b = W
    c_pos, c_invf, c_dmask = consts
    SCALE = 1.0 / math.sqrt(192.0)
    es = ExitStack()
    with es:
        Wqa = sbt(es, "Wqa", [128, 8, 256], BF16); Wkva = sbt(es, "Wkva", [128, 8, 192], BF16); WkvaB = sbt(es, "WkvaB", [128, 8, 64], BF16)
        Wqb = sbt(es, "Wqb", [128, 2, 1536], BF16); WqbB = sbt(es, "WqbB", [128, 2, 8, 64], BF16)
        Wkvb = sbt(es, "Wkvb", [128, 2, 8, 128], BF16); Wo = sbt(es, "Wo", [128, 8, D], BF16)
        qn = sbt(es, "qn", [128, 2]); kvn = sbt(es, "kvn", [128, 1]); invf = sbt(es, "invf", [128, 2])
        CC = sbt(es, "CC", [128, 2048]); SS = sbt(es, "SS", [128, 2048])
        dmask = sbt(es, "dmask", [128, 128], BF16); onesb = sbt(es, "onesb", [128, 128], BF16)
        epsr = sbt(es, "epsr", [128, 1])
        cqT = sbt(es, "cqT", [128, 2, 2048], BF16); ckvT = sbt(es, "ckvT", [128, 2048], BF16); krT = sbt(es, "krT", [128, 2048], BF16)
        OT = sbt(es, "OT", [128, 8, 2048], BF16)
        Xh = sbt(es, "Xha", [128, 4, D]); xT = sbt(es, "xTa", [128, 8, 4, 128], BF16)
        kTh = sbt(es, "kTh", [128, 2048], BF16); Vh = sbt(es, "Vh", [128, 16, 128], BF16)
        qTh = sbt(es, "qTh", [128, 2048], BF16); qrT = sbt(es, "qrT", [128, 2048], BF16)
        PT = [sbt(es, "PT%d" % i, [128, 512], BF16) for i in range(3)]
        rec = sbt(es, "rec", [128, 512]); t1 = sbt(es, "t1a", [128, 512]); t2 = sbt(es, "t2a", [128, 512])
        cqn = sbt(es, "cqn", [128, 256], BF16); ckvn = sbt(es, "ckvn", [128, 128], BF16)
        ss = sbt(es, "ss", [128, 4]); junk = sbt(es, "junka", [128, 256])
        st6 = sbt(es, "st6a", [128, 2, 6]); mv = sbt(es, "mva", [128, 2]); rstd = sbt(es, "rstda", [128, 1])
        ln2 = load_ln(es, 2)
        for dc in range(8):
            p.dma("pool", Wqa[:, dc, :], q_w_a[dc * 128:(dc + 1) * 128, :], w=["Wqa"])
            p.dma("pool", Wkva[:, dc, :], kv_w_a[dc * 128:(dc + 1) * 128, :], w=["Wkva"])
        p.dma("pool", WkvaB[:, :, 0:32], dap(kv_w_a, 160, [[192, 128], [192 * 128, 8], [1, 32]]), w=["WkvaB"])
        p.dma("pool", WkvaB[:, :, 32:64], dap(kv_w_a, 128, [[192, 128], [192 * 128, 8], [1, 32]]), w=["WkvaB"])
        for cc in range(2):
            p.dma("pool", Wqb[:, cc, :], q_w_b[cc * 128:(cc + 1) * 128, :], w=["Wqb"])
            p.dma("pool", WqbB[:, cc, :, 0:32], dap(q_w_b, cc * 128 * 1536 + 160, [[1536, 128], [192, 8], [1, 32]]), w=["WqbB"])
            p.dma("pool", WqbB[:, cc, :, 32:64], dap(q_w_b, cc * 128 * 1536 + 128, [[1536, 128], [192, 8], [1, 32]]), w=["WqbB"])
        for t_ in range(2):
            p.dma("pool", Wkvb[:, t_, :, :], dap(kv_w_b, t_ * 128, [[2048, 128], [256, 8], [1, 128]]), w=["Wkvb"])
        for h in range(8):
            p.dma("pool", Wo[:, h, :], o_w[h * 128:(h + 1) * 128, :], w=["Wo"])
        p.dma("sp", qn[:], dap(q_norm, 0, [[1, 128], [128, 2]]), w=["qn"])
        p.dma("sp", kvn[:], kv_norm, w=["kvn"])
        p.dma("sp", invf[0:64, :], c_invf, w=["invf"])
        p.dma("sp", CC[0:64, :], c_pos, w=["CC"])
        p.dma("pool", dmask[:], c_dmask, w=["dmask"])
        p.op("pool", lambda e: e.memset(onesb[:], 1.0), w=["onesb"])
        p.op("pool", lambda e: e.memset(epsr[:], RMS_EPS), w=["epsr"])
        p.op("pool", lambda e: e.memset(krT[:], 0.0), w=["krT"])
        p.op("pool", lambda e: e.memset(qrT[:], 0.0), w=["qrT"])
        for cc in range(2):
            p.ts("dve", Wqb[:, cc, :], Wqb[:, cc, :], qn[:, cc:cc + 1], None, ALU.mult, None, r=["Wqb", "qn"], w=["Wqb"])
            p.ts("dve", WqbB[:, cc, :, :], WqbB[:, cc, :, :], qn[:, cc:cc + 1], None, ALU.mult, None, r=["WqbB", "qn"], w=["WqbB"])
        p.ts("dve", Wkvb[:], Wkvb[:], kvn[:, 0:1], None, ALU.mult, None, r=["Wkvb", "kvn"], w=["Wkvb"])
        p.ts("dve", CC[0:64, :], CC[0:64, :], invf[0:64, 0:1], None, ALU.mult, None, r=["CC", "invf"], w=["CC"])
        p.cp("dve", SS[0:64, :], CC[0:64, :], r=["CC"], w=["SS"])
        for c0 in range(0, 2048, 512):
            sincos("dve", SS[0:64, c0:c0 + 512], CC[0:64, c0:c0 + 512], SS[0:64, c0:c0 + 512], t1[0:64, :], ["SS"], ["SS", "CC", "t1a"], npart=64)
        p.ts("dve", SS[0:64, :], SS[0:64, :], invf[0:64, 1:2], None, ALU.mult, None, r=["SS", "invf"], w=["SS"])
        for s_ in range(2):
            for tis in range(2):
                t = 2 * s_ + tis
                for jh in range(2):
                    p.dma("sp", Xh[:], xb[t][:, jh * 4:(jh + 1) * 4, :], r=[("xb", t)], w=["Xha"])
                    for j in range(4):
                        for dq in range(2):
                            bk = dq; kb = ("bank", bk)
                            for d4 in range(4):
                                dc = dq * 4 + d4
                                p.tr(bank(bk)[:, d4 * 128:(d4 + 1) * 128], Xh[:, j, dc * 128:(dc + 1) * 128], identf[:], r=["Xha", "identf"], w=[kb])
                            p.cp("act", xT[:, dq * 4:(dq + 1) * 4, j, :], bank(bk).rearrange("p (d n) -> p d n", d=4), r=[kb], w=["xTa"])
                    for j in range(4):
                        jg = jh * 4 + j
                        kb = ("bank", 2)
                        for dc in range(8):
                            p.mm(bank(2)[:, 0:256], xT[:, dc, j, :], Wqa[:, dc, :], dc == 0, dc == 7, r=["xTa", "Wqa"], w=[kb])
                        for dc in range(8):
                            p.mm(bank(2)[:, 256:384], xT[:, dc, j, :], Wkva[:, dc, 0:128], dc == 0, dc == 7, r=["xTa", "Wkva"], w=[kb])
                        p.act(junk[:], bank(2)[:, 0:256], AF.Square, r=[kb], w=["junka"], accum_out=ss[:, 0:1])
                        p.act(junk[:, 0:128], bank(2)[:, 256:384], AF.Square, r=[kb], w=["junka"], accum_out=ss[:, 1:2])
                        p.act(ss[:, 2:3], ss[:, 0:1], AF.Sqrt, r=["junka", "epsr"], w=["ss"], scale=1.0 / 256, bias=epsr[:, 0:1])
                        p.act(ss[:, 3:4], ss[:, 1:2], AF.Sqrt, r=["junka", "epsr"], w=["ss"], scale=1.0 / 128, bias=epsr[:, 0:1])
                        p.op("dve", lambda e: e.reciprocal(out=ss[:, 2:4], in_=ss[:, 2:4]), r=["ss"], w=["ss"])
                        p.ts("dve", cqn[:], bank(2)[:, 0:256], ss[:, 2:3], None, ALU.mult, None, r=[kb, "ss"], w=["cqn"])
                        p.ts("dve", ckvn[:], bank(2)[:, 256:384], ss[:, 3:4], None, ALU.mult, None, r=[kb, "ss"], w=["ckvn"])
                        kb3 = ("bank", 3)
                        for cc in range(2):
                            p.tr(bankb(3)[:, cc * 128:(cc + 1) * 128], cqn[:, cc * 128:(cc + 1) * 128], identb[:], r=["cqn", "identb"], w=[kb3])
                        p.tr(bankb(3)[:, 256:384], ckvn[:], identb[:], r=["ckvn", "identb"], w=[kb3])
                        for cc in range(2):
                            dst = cqT[:, cc, tis * 1024:(tis + 1) * 1024].rearrange("p (n j) -> p n j", j=8)[:, :, jg]
                            p.cp("act", dst, bankb(3)[:, cc * 128:(cc + 1) * 128], r=[kb3], w=["cqT"])
                        dst = ckvT[:, tis * 1024:(tis + 1) * 1024].rearrange("p (n j) -> p n j", j=8)[:, :, jg]
                        p.cp("act", dst, bankb(3)[:, 256:384], r=[kb3], w=["ckvT"])
                    kb4 = ("bank", 4); kb5 = ("bank", 5)
                    for dc in range(8):
                        p.mm(bank(4)[0:64, :], Wkva[:, dc, 128:192], xT[:, dc, :, :].rearrange("p j n -> p (j n)"), dc == 0, dc == 7, r=["xTa", "Wkva"], w=[kb4])
                    for dc in range(8):
                        p.mm(bank(5)[0:64, :], WkvaB[:, dc, :], xT[:, dc, :, :].rearrange("p j n -> p (j n)"), dc == 0, dc == 7, r=["xTa", "WkvaB"], w=[kb5])
                    vw = lambda T_: T_[0:64, tis * 1024:(tis + 1) * 1024].rearrange("p (n j) -> p j n", j=8)[:, jh * 4:(jh + 1) * 4, :]
                    p4 = lambda b_: b_[0:64, :].rearrange("p (j n) -> p j n", j=4)
                    t1v = t1[0:64, :].rearrange("p (j n) -> p j n", j=4); t2v = t2[0:64, :].rearrange("p (j n) -> p j n", j=4)
                    p.tt("dve", t1v, p4(bank(4)), vw(CC), ALU.mult, r=[kb4, "CC"], w=["t1a"])
                    p.tt("dve", t2v, p4(bank(5)), vw(SS), ALU.mult, r=[kb5, "SS"], w=["t2a"])
                    p.tt("dve", vw(krT), t1v, t2v, ALU.add, r=["t1a", "t2a"], w=["krT"])
            for h in range(8):
                for ch in range(4):
                    cs = slice(ch * 512, (ch + 1) * 512)
                    kb = ("bank", ch % 2)
                    p.mm(bank(ch % 2), Wkvb[:, 0, h, :], ckvT[:, cs], True, True, r=["Wkvb", "ckvT"], w=[kb])
                    p.cp("act", kTh[:, cs], bank(ch % 2), r=[kb], w=["kTh"])
                for pq in range(4):
                    kb = ("bank", 2 + pq % 2)
                    for pb in range(4):
                        pbk = pq * 4 + pb
                        p.mm(bank(2 + pq % 2)[:, pb * 128:(pb + 1) * 128], ckvT[:, pbk * 128:(pbk + 1) * 128], Wkvb[:, 1, h, :], True, True, r=["Wkvb", "ckvT"], w=[kb])
                    p.cp("act", Vh[:, pq * 4:(pq + 1) * 4, :].rearrange("p a d -> p (a d)"), bank(2 + pq % 2), r=[kb], w=["Vh"])
                for ch in range(4):
                    cs = slice(ch * 512, (ch + 1) * 512)
                    kb = ("bank", 4 + ch % 2)
                    for cc in range(2):
                        p.mm(bank(4 + ch % 2), Wqb[:, cc, h * 192:h * 192 + 128], cqT[:, cc, cs], cc == 0, cc == 1, r=["Wqb", "cqT"], w=[kb])
                    p.act(qTh[:, cs], bank(4 + ch % 2), AF.Copy, r=[kb], w=["qTh"], scale=SCALE)
                    kb6 = ("bank", 6); kb7 = ("bank", 7)
                    for cc in range(2):
                        p.mm(bank(6)[0:64, :], Wqb[:, cc, h * 192 + 128:h * 192 + 192], cqT[:, cc, cs], cc == 0, cc == 1, r=["Wqb", "cqT"], w=[kb6])
                    for cc in range(2):
                        p.mm(bank(7)[0:64, :], WqbB[:, cc, h, :], cqT[:, cc, cs], cc == 0, cc == 1, r=["WqbB", "cqT"], w=[kb7])
                    p.stt("dve", t1[0:64, :], bank(6)[0:64, :], SCALE, CC[0:64, cs], ALU.mult, ALU.mult, r=[kb6, "CC"], w=["t1a"])
                    p.stt("dve", t2[0:64, :], bank(7)[0:64, :], SCALE, SS[0:64, cs], ALU.mult, ALU.mult, r=[kb7, "SS"], w=["t2a"])
                    p.tt("dve", qrT[0:64, cs], t1[0:64, :], t2[0:64, :], ALU.add, r=["t1a", "t2a"], w=["qrT"])
                for qc in range(4):
                    nkb = 4 * (qc + 1)
                    ko = ("bank", 0); kd = ("bank", 1)
                    def geom(kbi):
                        q0 = max(kbi * 128, qc * 512)
                        return q0, (qc + 1) * 512 - q0, q0 - qc * 512

                    def emit_S(kbi):
                        q0, N, off = geom(kbi)
                        sb_ = 2 + (kbi % 4)
                        ksb = ("bank", sb_)
                        ks_ = slice(kbi * 128, (kbi + 1) * 128)
                        p.mm(bank(sb_)[:, 0:N], kTh[:, ks_], qTh[:, q0:q0 + N], True, False, r=["kTh", "qTh"], w=[ksb])
                        p.mm(bank(sb_)[:, 0:N], krT[:, ks_], qrT[:, q0:q0 + N], False, True, r=["krT", "qrT"], w=[ksb])

                    def emit_P(kbi):
                        q0, N, off = geom(kbi)
                        sb_ = 2 + (kbi % 4)
                        ksb = ("bank", sb_)
                        pt = PT[kbi % 3]; kpt = ("PT", kbi % 3)
                        p.act(pt[:, 0:N], bank(sb_)[:, 0:N], AF.Exp, r=[ksb], w=[kpt])
                        if kbi * 128 >= qc * 512:
                            p.tt("pool", pt[:, 0:128], pt[:, 0:128], dmask[:], ALU.mult, r=[kpt, "dmask"], w=[kpt])

                    def emit_V(kbi):
                        q0, N, off = geom(kbi)
                        pt = PT[kbi % 3]; kpt = ("PT", kbi % 3)
                        p.mm(bank(0)[:, off:off + N], Vh[:, kbi, :], pt[:, 0:N], kbi == 0, kbi == nkb - 1, r=["Vh", kpt], w=[ko])
                        p.mm(bank(1)[:, off:off + N], onesb[:], pt[:, 0:N], kbi == 0, kbi == nkb - 1, r=["onesb", kpt], w=[kd])

                    emit_S(0)
                    if nkb > 1:
                        emit_S(1)
                    for kbi in range(nkb):
                        emit_P(kbi)
                        if kbi + 2 < nkb:
                            emit_S(kbi + 2)
                        emit_V(kbi)
                    p.op("dve", lambda e: e.reciprocal(out=rec[:], in_=bank(1)), r=[kd], w=["rec"])
                    p.tt("dve", OT[:, h, qc * 512:(qc + 1) * 512], bank(0), rec[:], ALU.mult, r=[ko, "rec"], w=["OT"])
            for tis in range(2):
                t = 2 * s_ + tis
                for jh in range(2):
                    p.dma("sp", Xh[:], xb[t][:, jh * 4:(jh + 1) * 4, :], r=[("xb", t)], w=["Xha"] + [("Xha", j) for j in range(4)])
                    for j in range(4):
                        jg = jh * 4 + j
                        ko = [("bank", 6), ("bank", 7)]
                        for nh in range(2):
                            for h in range(8):
                                lh = OT[:, h, tis * 1024:(tis + 1) * 1024].rearrange("p (n j) -> p n j", j=8)[:, :, jg]
                                p.mm(PS[3][:, nh * 512:(nh + 1) * 512], lh, Wo[:, h, nh * 512:(nh + 1) * 512], h == 0, h == 7, r=["OT", "Wo"], w=[ko[nh]])
                        blk = Xh[:, j, :]; kx = ("Xha", j)
                        p.stt("dve", blk, blk, ALPHA, PS[3][:], ALU.mult, ALU.add, r=["Xha", kx] + ko, w=[kx])
                        layer_norm(blk, ln2, kx, (st6, mv, rstd))
                    p.dma("sp", xc[t][:, jh * 4:(jh + 1) * 4, :], Xh[:], r=["Xha"] + [("Xha", j) for j in range(4)], w=[("xc", t)])
    p.barrier()


def phase_moe_routed(nc, p, sbt, PS, bank, bankb, identf, identb, layer_norm, load_ln, xc, out, XE, YE, moe_router, moe_w_in, moe_w_out, consts):
    c_triu, c_ecol = consts
    NT = CAP // 128
    es = ExitStack()
    with es:
        S1 = [sbt(es, "S1_%d" % i, [128, 1], I32) for i in range(32)]; S2 = [sbt(es, "S2_%d" % i, [128, 1], I32) for i in range(32)]
        G1 = sbt(es, "G1", [128, 32]); G2 = sbt(es, "G2", [128, 32])
        g1 = ExitStack()
        with g1:
            wrb = sbt(g1, "wrb", [128, D, NEXP]); Xg_ = sbt(g1, "Xcg", [128, 8, D]); junk = sbt(g1, "junk", [128, D])
            Xb = [sbt(g1, "Xb%d" % i, [128, D], BF16) for i in range(2)]
            zt = sbt(g1, "zt", [128, NT, D], BF16)
            lg = sbt(g1, "lg", [128, NEXP]); m8 = sbt(g1, "m8", [128, 8]); dd = sbt(g1, "dd", [128, 1])
            mk = sbt(g1, "mk", [128, NEXP]); mkb = sbt(g1, "mkb", [128, NEXP], BF16)
            m1 = sbt(g1, "m1", [128, NEXP]); m2 = sbt(g1, "m2", [128, NEXP]); dest = sbt(g1, "dest", [128, NEXP]); ovf = sbt(g1, "ovf", [128, NEXP])
            base = sbt(g1, "base", [128, NEXP]); ecol = sbt(g1, "ecol", [128, NEXP]); sf = sbt(g1, "sf", [128, 2])
            triu = sbt(g1, "triu", [128, 128], BF16); onesb = sbt(g1, "onesb2", [128, 128], BF16)
            p.dma("sp", wrb[:].rearrange("p d e -> p (d e)"), dap(moe_router, 0, [[0, 128], [1, D * NEXP]]), w=["wrb"])
            p.dma("sp", ecol[:], c_ecol, w=["ecol"])
            p.dma("pool", triu[:], c_triu, w=["triu"])
            p.op("pool", lambda e: e.memset(onesb[:], 1.0), w=["onesb2"])
            p.op("pool", lambda e: e.memset(base[:], 0.0), w=["base"])
            p.op("pool", lambda e: e.memset(zt[:], 0.0), w=["zt"])
            for e in range(NEXP):
                p.dma("sp", XE[e * CAP:(e + 1) * CAP, :].rearrange("(t p) d -> p t d", p=128), zt[:], r=["zt"], w=["XE"])
            p.op("pool", lambda e: e.memset(junk[:], 0.0), w=["junk"])
            p.dma("sp", YE[NEXP * CAP:NEXP * CAP + 128, :], junk[:], r=["junk"], w=["YE"])
            for t in range(4):
                p.dma("sp", Xg_[:], xc[t], r=[("xc", t)], w=["Xcg"])
                for j in range(8):
                    b = t * 8 + j
                    xb_ = Xb[b % 2]; kxb = ("Xb", b % 2)
                    p.cp("act", xb_[:], Xg_[:, j, :], r=["Xcg"], w=[kxb])
                    for e in range(NEXP):
                        p.op("dve", (lambda e_, j_: lambda en: en.scalar_tensor_tensor(out=junk[:], in0=Xg_[:, j_, :], scalar=1.0, in1=wrb[:, :, e_],
                                                                                 op0=ALU.mult, op1=ALU.mult, accum_out=lg[:, e_:e_ + 1]))(e, j),
                             r=["Xcg", "wrb"], w=["junk", "lg"])
                    p.op("dve", lambda en: en.max(out=m8[:], in_=lg[:]), r=["lg"], w=["m8"])
                    p.tt("dve", dd[:], m8[:, 1:2], m8[:, 0:1], ALU.subtract, r=["m8"], w=["dd"])
                    p.act(G2[:, b:b + 1], dd[:], AF.Sigmoid, r=["dd"], w=[("G", b)])
                    p.ts("dve", G1[:, b:b + 1], G2[:, b:b + 1], -1.0, 1.0, ALU.mult, ALU.add, r=[("G", b)], w=[("G", b)])
                    p.ts("dve", mk[:], lg[:], m8[:, 1:2], None, ALU.is_ge, None, r=["lg", "m8"], w=["mk"])
                    p.cp("dve", mkb[:], mk[:], r=["mk"], w=["mkb"])
                    p.ts("dve", m1[:], lg[:], m8[:, 0:1], None, ALU.is_equal, None, r=["lg", "m8"], w=["m1"])
                    p.tt("dve", m2[:], mk[:], m1[:], ALU.subtract, r=["mk", "m1"], w=["m2"])
                    kb = ("bank", 2 + b % 2)
                    pb_ = bank(2 + b % 2)
                    p.mm(pb_[:, 0:NEXP], triu[:], mkb[:], True, True, r=["triu", "mkb"], w=[kb])
                    p.mm(pb_[:, 8:8 + NEXP], onesb[:], mkb[:], True, True, r=["onesb2", "mkb"], w=[kb])
                    p.tt("dve", dest[:], pb_[:, 0:NEXP], base[:], ALU.add, r=[kb, "base"], w=["dest"])
                    p.ts("dve", ovf[:], dest[:], float(CAP), 1.0e6, ALU.is_ge, ALU.mult, r=["dest"], w=["ovf"])
                    p.tt("dve", dest[:], dest[:], ecol[:], ALU.add, r=["dest", "ecol"], w=["dest"])
                    p.tt("dve", dest[:], dest[:], ovf[:], ALU.add, r=["dest", "ovf"], w=["dest"])
                    p.tt("dve", base[:], base[:], pb_[:, 8:8 + NEXP], ALU.add, r=[kb, "base", "dest"], w=["base"])
                    p.tt("dve", m1[:], m1[:], dest[:], ALU.mult, r=["m1", "dest"], w=["m1"])
                    p.tt("dve", m2[:], m2[:], dest[:], ALU.mult, r=["m2", "dest"], w=["m2"])
                    p.op("dve", lambda en: en.reduce_sum(out=sf[:, 0:1], in_=m1[:], axis=AX.X), r=["m1"], w=["sf"])
                    p.op("dve", lambda en: en.reduce_sum(out=sf[:, 1:2], in_=m2[:], axis=AX.X), r=["m2"], w=["sf"])
                    p.ts("dve", sf[:], sf[:], float(NEXP * CAP), None, ALU.min, None, r=["sf"], w=["sf"])
                    p.cp("dve", S1[b][:], sf[:, 0:1], r=["sf"], w=[("S", b)])
                    p.cp("dve", S2[b][:], sf[:, 1:2], r=["sf"], w=[("S", b)])
                    for S_ in (S1, S2):
                        p.op("pool", (lambda S__, b_, x_: lambda en: en.indirect_dma_start(
                            out=XE[:, :], out_offset=bass.IndirectOffsetOnAxis(ap=S__[b_][:, :], axis=0),
                            in_=x_[:, :], in_offset=None, oob_is_err=False))(S_, b, xb_),
                            r=[kxb, ("S", b), "XE"], w=[("XEs", b)], dma=True)
        p.barrier()
        ex = ExitStack()
        with ex:
            XEe = sbt(ex, "XEe", [128, 2, D], BF16); xeT = sbt(ex, "xeT", [128, 8, CAP], BF16)
            SCW = 512
            CHUNKS = [(c0, min(512, CAP - c0)) for c0 in range(0, CAP, 512)]
            Wi = [sbt(ex, "Wi%d" % i, [128, 8, 2, 896], BF16) for i in range(2)]
            Wo2 = [sbt(ex, "Wo%d" % i, [128, 7, D], BF16) for i in range(2)]
            hT = sbt(ex, "hTm", [128, 7, CAP], BF16)
            slt = [sbt(ex, "sltm%d" % i, [128, SCW], BF16) for i in range(2)]
            yacc = sbt(ex, "yacc", [128, NT, D])
            idx = 0
            for e in range(NEXP):
                for st in range(NT):
                    bk = st % 2; kb = ("bank", bk)
                    kxe = ("XEe", st % 2)
                    p.dma("sp", XEe[:, st % 2, :], XE[e * CAP + st * 128:e * CAP + (st + 1) * 128, :], r=[("XEs", b_) for b_ in range(32)] if (e == 0 and st < 2) else [], w=[kxe])
                    for dc in range(8):
                        p.tr(bankb(bk)[:, dc * 128:(dc + 1) * 128], XEe[:, st % 2, dc * 128:(dc + 1) * 128], identb[:], r=[kxe, "identb"], w=[kb])
                    p.cp("act", xeT[:, :, st * 128:(st + 1) * 128], bankb(bk).rearrange("p (d n) -> p d n", d=8), r=[kb], w=["xeT"])
                for qh in range(4):
                    wi = Wi[idx % 2]; wo = Wo2[idx % 2]
                    kwi = ("Wi", idx % 2); kwo = ("Wo", idx % 2)
                    idx += 1
                    for gu in range(2):
                        p.dma("pool", wi[:, :, gu, :], dap(moe_w_in, e * D * 2 * EDIM + gu * EDIM + qh * 896, [[2 * EDIM, 128], [2 * EDIM * 128, 8], [1, 896]]), w=[kwi])
                    p.dma("pool", wo[:], dap(moe_w_out, (e * EDIM + qh * 896) * D, [[D, 128], [128 * D, 7], [1, D]]), w=[kwo])
                    for fc in range(7):
                        for sc, (c0_, cw_) in enumerate(CHUNKS):
                            ci = fc * len(CHUNKS) + sc
                            bg = 2 + 2 * (ci % 2); bu = bg + 1
                            kg = ("bank", bg); ku = ("bank", bu)
                            cs = slice(c0_, c0_ + cw_)
                            for dc in range(8):
                                p.mm(bank(bg)[:, 0:cw_], wi[:, dc, 0, fc * 128:(fc + 1) * 128], xeT[:, dc, cs], dc == 0, dc == 7, r=["xeT", kwi], w=[kg])
                            for dc in range(8):
                                p.mm(bank(bu)[:, 0:cw_], wi[:, dc, 1, fc * 128:(fc + 1) * 128], xeT[:, dc, cs], dc == 0, dc == 7, r=["xeT", kwi], w=[ku])
                            s_ = slt[ci % 2]; ks = ("sltm", ci % 2)
                            p.act(s_[:, 0:cw_], bank(bg)[:, 0:cw_], AF.Silu, r=[kg], w=[ks])
                            p.tt("dve", hT[:, fc, cs], s_[:, 0:cw_], bank(bu)[:, 0:cw_], ALU.mult, r=[ks, ku], w=[("hTm", fc)])
                    for st in range(NT):
                        if st % 2 == 0:
                            po_, ko = PS[0], [("bank", 0), ("bank", 1)]
                        else:
                            po_, ko = PS[3], [("bank", 6), ("bank", 7)]
                        for nh in range(2):
                            for fc in range(7):
                                p.mm(po_[:, nh * 512:(nh + 1) * 512], hT[:, fc, st * 128:(st + 1) * 128], wo[:, fc, nh * 512:(nh + 1) * 512], fc == 0, fc == 6,
                                     r=[("hTm", fc), kwo], w=[ko[nh]])
                        if qh == 0:
                            p.cp("act", yacc[:, st, :], po_[:], r=ko, w=[("yacc", st)])
                        else:
                            p.tt("dve", yacc[:, st, :], yacc[:, st, :], po_[:], ALU.add, r=ko + [("yacc", st)], w=[("yacc", st)])
                p.dma("sp", YE[e * CAP:(e + 1) * CAP, :].rearrange("(t p) d -> p t d", p=128), yacc[:], r=[("yacc", st) for st in range(NT)], w=["YE"])
        p.barrier()
        cb = ExitStack()
        with cb:
            Xc = sbt(cb, "Xcm", [128, 8, D])
            Y1 = [sbt(cb, "Y1_%d" % i, [128, D]) for i in range(16)]; Y2 = [sbt(cb, "Y2_%d" % i, [128, D]) for i in range(16)]
            st6 = sbt(cb, "st6m", [128, 2, 6]); mv = sbt(cb, "mvm", [128, 2]); rstd = sbt(cb, "rstdm", [128, 1])
            ln3 = load_ln(cb, 3)

            def gathers(t):
                for j in range(8):
                    b = t * 8 + j
                    ky = ("Y", b % 16)
                    for S_, y_ in ((S1, Y1[b % 16]), (S2, Y2[b % 16])):
                        p.op("pool", (lambda S__, b_, yy: lambda en: en.indirect_dma_start(
                            out=yy[:, :], out_offset=None, in_=YE[:, :],
                            in_offset=bass.IndirectOffsetOnAxis(ap=S__[b_][:, :], axis=0),
                            oob_is_err=False))(S_, b, y_),
                            r=["YE", ("S", b)], w=[ky], dma=True)

            gathers(0)
            for t in range(4):
                if t + 1 < 4:
                    gathers(t + 1)
                p.dma("sp", Xc[:], xc[t], r=[("xc", t)], w=["Xcm"] + [("Xcm", j) for j in range(8)])
                for j in range(8):
                    b = t * 8 + j
                    y1 = Y1[b % 16]; y2 = Y2[b % 16]; ky = ("Y", b % 16)
                    p.ts("dve", y1[:], y1[:], G1[:, b:b + 1], None, ALU.mult, None, r=[ky, ("G", b)], w=[ky])
                    p.stt("dve", y1[:], y2[:], G2[:, b:b + 1], y1[:], ALU.mult, ALU.add, r=[ky, ("G", b)], w=[ky])
                    blk = Xc[:, j, :]; kx = ("Xcm", j)
                    p.stt("dve", blk, blk, ALPHA, y1[:], ALU.mult, ALU.add, r=["Xcm", kx, ky], w=[kx])
                    layer_norm(blk, ln3, kx, (st6, mv, rstd), aff="dve")
                p.dma("sp", out[t], Xc[:], r=["Xcm"] + [("Xcm", j) for j in range(8)], w=[("out", t)])
    p.barrier()
```
